# Optimizing a Trainium2 kernel written in Bass

```python
import math
import jax, jax.numpy as jnp
from jax import lax
import numpy as np

D_MODEL = 1024
BATCH = 16
SEQ = 4096
DEPTH = 2

D_MIX = 2 * D_MODEL
D_SSD = D_MIX // 2
SSD_HEAD_DIM = 64
SSD_HEADS = D_SSD // SSD_HEAD_DIM
SSD_GROUPS = 4
SSD_STATE = 128
CONV_WIDTH = 4
CHUNK = 128
CONV_CH = D_SSD + 2 * SSD_GROUPS * SSD_STATE
D_FOX = D_MIX - D_SSD
FOX_HEAD_DIM = 64
FOX_HEADS = D_FOX // FOX_HEAD_DIM
Q_BLOCK = 128
PROJ_SIZES = (D_SSD, CONV_CH, SSD_HEADS, D_FOX, D_FOX, D_FOX, FOX_HEADS)
D_PROJ = sum(PROJ_SIZES)
PROJ_SPLITS = tuple(int(s) for s in np.cumsum(PROJ_SIZES)[:-1])
D_FF = 2816
N_EXPERTS = 8
TOP_K = 2
N_DENSE = (DEPTH + 1) // 2
N_MOE = DEPTH // 2
EPS = 1e-5

kernel_name = 'hymba_ssd_fox_moe_trunk'


def rms_norm(x, g):
    xf = x.astype(jnp.float32)
    y = xf * lax.rsqrt(jnp.mean(xf * xf, axis=-1, keepdims=True) + EPS)
    return y.astype(x.dtype) * g


def swiglu(h, w_gate, w_up, w_down):
    return (jax.nn.silu(h @ w_gate) * (h @ w_up)) @ w_down


def causal_depthwise_conv(u, w, b):
    out = lax.conv_general_dilated(
        u, w[:, None, :], window_strides=(1,), padding=[(CONV_WIDTH - 1, 0)],
        dimension_numbers=('NWC', 'WIO', 'NWC'), feature_group_count=u.shape[-1])
    return out + b


def ssd_chunked(x, dt, a, b_mat, c_mat):
    bsz, seq = x.shape[0], x.shape[1]
    nc = seq // CHUNK
    r = SSD_HEADS // SSD_GROUPS
    xc = (x * dt[..., None]).reshape(bsz, nc, CHUNK, SSD_GROUPS, r, SSD_HEAD_DIM)
    a_dt = (dt * a).reshape(bsz, nc, CHUNK, SSD_GROUPS, r)
    bc = b_mat.reshape(bsz, nc, CHUNK, SSD_GROUPS, SSD_STATE)
    cc = c_mat.reshape(bsz, nc, CHUNK, SSD_GROUPS, SSD_STATE)
    a_cs = jnp.cumsum(a_dt, axis=2)
    causal = jnp.tril(jnp.ones((CHUNK, CHUNK), dtype=bool))[:, :, None, None]
    seg = a_cs[:, :, :, None] - a_cs[:, :, None, :]
    decay_in = jnp.where(causal, jnp.exp(jnp.where(causal, seg, 0.0)), 0.0)
    cb = jnp.einsum('bclgn,bcsgn->bclsg', cc, bc)
    y_diag = jnp.einsum('bclsgr,bcsgrp->bclgrp', cb[..., None] * decay_in, xc)
    decay_out = jnp.exp(a_cs[:, :, -1:] - a_cs)
    chunk_states = jnp.einsum('bclgn,bclgrp->bcgrpn', bc, xc * decay_out[..., None])
    chunk_decay = jnp.exp(a_cs[:, :, -1])

    def step(h, inp):
        st, dec = inp
        return h * dec[..., None, None] + st, h

    init = jnp.zeros((bsz, SSD_GROUPS, r, SSD_HEAD_DIM, SSD_STATE), chunk_states.dtype)
    _, prev = lax.scan(step, init, (jnp.moveaxis(chunk_states, 1, 0), jnp.moveaxis(chunk_decay, 1, 0)))
    prev = jnp.moveaxis(prev, 0, 1)
    y_off = jnp.einsum('bclgn,bcgrpn->bclgrp', cc, prev) * jnp.exp(a_cs)[..., None]
    return (y_diag + y_off).reshape(bsz, seq, SSD_HEADS, SSD_HEAD_DIM)


def forgetting_attention(q, k, v, log_f):
    bsz, seq = q.shape[0], q.shape[1]
    nb = seq // Q_BLOCK
    scale = FOX_HEAD_DIM ** -0.5
    cum_f = jnp.cumsum(log_f, axis=1)
    f_k = jnp.transpose(cum_f, (0, 2, 1))
    q_blocks = jnp.moveaxis(q.reshape(bsz, nb, Q_BLOCK, FOX_HEADS, FOX_HEAD_DIM), 1, 0)
    f_blocks = jnp.moveaxis(cum_f.reshape(bsz, nb, Q_BLOCK, FOX_HEADS), 1, 0)
    pos_blocks = jnp.arange(seq, dtype=jnp.int32).reshape(nb, Q_BLOCK)
    k_pos = jnp.arange(seq, dtype=jnp.int32)

    def block(args):
        qb, fq, qpos = args
        s = jnp.einsum('bqhd,bkhd->bhqk', qb, k).astype(jnp.float32) * scale
        s = s + jnp.transpose(fq, (0, 2, 1))[..., None] - f_k[:, :, None, :]
        s = jnp.where(k_pos[None, :] <= qpos[:, None], s, -jnp.inf)
        p = jax.nn.softmax(s, axis=-1).astype(v.dtype)
        return jnp.einsum('bhqk,bkhd->bqhd', p, v)

    out = lax.map(block, (q_blocks, f_blocks, pos_blocks))
    return jnp.moveaxis(out, 0, 1).reshape(bsz, seq, D_FOX)


def hybrid_mixer(h, w_in, conv_w, conv_b, dt_bias, a_log, d_skip, ssd_norm, fox_f_bias, fox_norm, w_out):
    bsz, seq, _ = h.shape
    proj = h @ w_in
    z, xbc, dt_raw, q, k, v, f_raw = jnp.split(proj, PROJ_SPLITS, axis=-1)
    xbc = jax.nn.silu(causal_depthwise_conv(xbc, conv_w, conv_b))
    xs, b_mat, c_mat = jnp.split(xbc, (D_SSD, D_SSD + SSD_GROUPS * SSD_STATE), axis=-1)
    xs = xs.reshape(bsz, seq, SSD_HEADS, SSD_HEAD_DIM)
    b_mat = b_mat.reshape(bsz, seq, SSD_GROUPS, SSD_STATE)
    c_mat = c_mat.reshape(bsz, seq, SSD_GROUPS, SSD_STATE)
    dt = jax.nn.softplus(dt_raw.astype(jnp.float32) + dt_bias.astype(jnp.float32))
    a = -jnp.exp(a_log.astype(jnp.float32))
    y = ssd_chunked(xs, dt, a, b_mat, c_mat) + d_skip[:, None] * xs
    y = y.reshape(bsz, seq, D_SSD).astype(h.dtype)
    y_ssd = rms_norm(y * jax.nn.silu(z), ssd_norm)
    log_f = jax.nn.log_sigmoid(f_raw.astype(jnp.float32) + fox_f_bias.astype(jnp.float32))
    y_fox = forgetting_attention(
        q.reshape(bsz, seq, FOX_HEADS, FOX_HEAD_DIM),
        k.reshape(bsz, seq, FOX_HEADS, FOX_HEAD_DIM),
        v.reshape(bsz, seq, FOX_HEADS, FOX_HEAD_DIM), log_f)
    y_fox = rms_norm(y_fox.astype(h.dtype), fox_norm)
    return jnp.concatenate([y_ssd, y_fox], axis=-1) @ w_out


def moe_swiglu(h, router_w, w_gate, w_up, w_down):
    bsz, seq, d = h.shape
    t = h.reshape(-1, d)
    logits = (t @ router_w).astype(jnp.float32)
    top_val, top_idx = lax.top_k(logits, TOP_K)
    top_w = jax.nn.softmax(top_val, axis=-1)
    combine = jnp.sum(jax.nn.one_hot(top_idx, N_EXPERTS, dtype=jnp.float32) * top_w[..., None], axis=1)
    combine = combine.astype(h.dtype)
    out = jnp.zeros_like(t)
    for e in range(N_EXPERTS):
        out = out + combine[:, e:e + 1] * swiglu(t, w_gate[e], w_up[e], w_down[e])
    return out.reshape(bsz, seq, d)


def setup_inputs(seed: int = 0) -> dict:
    key = jax.random.key(seed)
    ks = jax.random.split(key, 24)
    f32 = jnp.float32
    nrm = lambda k, shape, s: jax.random.normal(k, shape, f32) * s
    gain = lambda k, shape: 1.0 + 0.02 * jax.random.normal(k, shape, f32)
    u = jax.random.uniform(ks[5], (DEPTH, SSD_HEADS), f32)
    dt0 = jnp.exp(u * (math.log(0.1) - math.log(0.001)) + math.log(0.001))
    return {
        'x': jax.random.normal(ks[0], (BATCH, SEQ, D_MODEL), f32),
        'mix_norm': gain(ks[1], (DEPTH, D_MODEL)),
        'w_in': nrm(ks[2], (DEPTH, D_MODEL, D_PROJ), D_MODEL ** -0.5),
        'conv_w': nrm(ks[3], (DEPTH, CONV_WIDTH, CONV_CH), CONV_WIDTH ** -0.5),
        'conv_b': nrm(ks[4], (DEPTH, CONV_CH), 0.02),
        'dt_bias': dt0 + jnp.log(-jnp.expm1(-dt0)),
        'a_log': jnp.log(jax.random.uniform(ks[6], (DEPTH, SSD_HEADS), f32, 1.0, 16.0)),
        'd_skip': gain(ks[7], (DEPTH, SSD_HEADS)),
        'ssd_norm': gain(ks[8], (DEPTH, D_SSD)),
        'fox_f_bias': jax.random.uniform(ks[9], (DEPTH, FOX_HEADS), f32, 1.0, 4.0),
        'fox_norm': gain(ks[10], (DEPTH, D_FOX)),
        'w_out': nrm(ks[11], (DEPTH, D_MIX, D_MODEL), D_MIX ** -0.5),
        'ffn_norm': gain(ks[12], (DEPTH, D_MODEL)),
        'ffn_w_gate': nrm(ks[13], (N_DENSE, D_MODEL, D_FF), D_MODEL ** -0.5),
        'ffn_w_up': nrm(ks[14], (N_DENSE, D_MODEL, D_FF), D_MODEL ** -0.5),
        'ffn_w_down': nrm(ks[15], (N_DENSE, D_FF, D_MODEL), D_FF ** -0.5),
        'router_w': nrm(ks[16], (N_MOE, D_MODEL, N_EXPERTS), D_MODEL ** -0.5),
        'moe_w_gate': nrm(ks[17], (N_MOE, N_EXPERTS, D_MODEL, D_FF), D_MODEL ** -0.5),
        'moe_w_up': nrm(ks[18], (N_MOE, N_EXPERTS, D_MODEL, D_FF), D_MODEL ** -0.5),
        'moe_w_down': nrm(ks[19], (N_MOE, N_EXPERTS, D_FF, D_MODEL), D_FF ** -0.5),
        'final_norm': gain(ks[20], (D_MODEL,)),
    }


def reference(x, mix_norm, w_in, conv_w, conv_b, dt_bias, a_log, d_skip, ssd_norm, fox_f_bias,
              fox_norm, w_out, ffn_norm, ffn_w_gate, ffn_w_up, ffn_w_down, router_w,
              moe_w_gate, moe_w_up, moe_w_down, final_norm):
    for i in range(DEPTH):
        x = x + hybrid_mixer(rms_norm(x, mix_norm[i]), w_in[i], conv_w[i], conv_b[i], dt_bias[i],
                             a_log[i], d_skip[i], ssd_norm[i], fox_f_bias[i], fox_norm[i], w_out[i])
        hn = rms_norm(x, ffn_norm[i])
        j = i // 2
        if i % 2 == 0:
            x = x + swiglu(hn, ffn_w_gate[j], ffn_w_up[j], ffn_w_down[j])
        else:
            x = x + moe_swiglu(hn, router_w[j], moe_w_gate[j], moe_w_up[j], moe_w_down[j])
    return rms_norm(x, final_norm)
```

```python
import numpy as np
from contextlib import ExitStack
import ml_dtypes
import concourse.bass as bass
import concourse.mybir as mybir
from concourse.bass_utils import run_bass_kernel_spmd

F32 = mybir.dt.float32
BF16 = mybir.dt.bfloat16
AF = mybir.ActivationFunctionType
ALU = mybir.AluOpType
AX = mybir.AxisListType

D = 1024
DP = 6176
DFF = 2816
NFC = DFF // 128
NE = 8
NH = 16
EPS = 1e-5
NEG = -30000.0
NDS = 88


class Sem:
    def __init__(self, h):
        self.h = h
        self.cnt = 0


class Buf:
    def __init__(self, t, dram=False):
        self.t = t
        self.dram = dram
        self.w = {}
        self.r = {}
        self.sem = None

    def __getitem__(self, k):
        return self.t[k]


class Alias:
    def __init__(self, parent, ap):
        self.p = parent
        self.ap_ = ap
        self.dram = False

    def __getitem__(self, k):
        return self.ap_[k]

    w = property(lambda s: s.p.w, lambda s, v: setattr(s.p, "w", v))
    r = property(lambda s: s.p.r, lambda s, v: setattr(s.p, "r", v))
    sem = property(lambda s: s.p.sem, lambda s, v: setattr(s.p, "sem", v))


class KB:
    def __init__(self, nc, stack):
        self.nc = nc
        self.stack = stack
        self.eng = dict(pe=nc.tensor, act=nc.scalar, dve=nc.vector, pool=nc.gpsimd, sp=nc.sync)
        self.esem = {e: Sem(stack.enter_context(nc.semaphore("se_" + e))) for e in self.eng}
        self.seen = {e: {} for e in self.eng}
        self.dsems = [Sem(stack.enter_context(nc.semaphore("sd%d" % i))) for i in range(NDS)]
        self.free = list(self.dsems)
        self.bar = Sem(stack.enter_context(nc.semaphore("sbar")))
        self.uid = 0

    def _wait(self, e, toks, skip=None):
        for s, val in list(toks.items()):
            if s is skip:
                continue
            if self.seen[e].get(s, 0) < val:
                self.eng[e].wait_ge(s.h, val)
                self.seen[e][s] = val

    def op(self, e, fn, reads=(), writes=(), sreads=(), fence=False):
        own = self.esem[e]
        for b in reads:
            self._wait(e, b.w, own if e == "pe" else None)
        for b in sreads:
            self._wait(e, b.w)
        for b in writes:
            self._wait(e, b.w, own)
            self._wait(e, b.r, own)
        ins = fn(self.eng[e])
        own.cnt += 1
        ins.then_inc(own.h, 1)
        for b in list(reads) + list(sreads):
            if b not in writes:
                b.r[own] = own.cnt
        for b in writes:
            b.w = {own: own.cnt}
            b.r = {}
        if fence:
            self._wait(e, {own: own.cnt})
        return ins

    def dma(self, q, out, in_, sb, reads=(), writes=()):
        if sb.sem is None:
            sb.sem = self.free.pop()
        s = sb.sem
        for b in reads:
            if not b.dram:
                self._wait(q, b.w)
        for b in writes:
            if not b.dram:
                self._wait(q, b.w, s)
                self._wait(q, b.r)
        ins = self.eng[q].dma_start(out=out, in_=in_)
        s.cnt += 16
        ins.then_inc(s.h, 16)
        for b in reads:
            if not b.dram:
                b.r[s] = s.cnt
        for b in writes:
            if b.dram:
                pass
            else:
                b.w = {s: s.cnt}
                b.r = {}
        return ins

    def release(self, bufs):
        for b in bufs:
            if b.sem is not None:
                self.free.append(b.sem)
                b.sem = None

    def barrier(self):
        for e, s in self.esem.items():
            if e != "sp" and s.cnt:
                self._wait("sp", {s: s.cnt})
        for s in self.dsems:
            if s.cnt:
                self._wait("sp", {s: s.cnt})
        self.nc.sync.sem_inc(self.bar.h, 1)
        self.bar.cnt += 1
        for e in self.eng:
            if e != "sp":
                self._wait(e, {self.bar: self.bar.cnt})


class Phase:
    def __init__(self, kb, name):
        self.kb = kb
        self.name = name
        self.stack = ExitStack()
        self.bufs = []

    def sb(self, name, shape, dt):
        self.kb.uid += 1
        t = self.stack.enter_context(self.kb.nc.sbuf_tensor("%s_%s_%d" % (self.name, name, self.kb.uid), list(shape), dt))
        b = Buf(t)
        self.bufs.append(b)
        return b

    def ps(self, name, shape, dt):
        self.kb.uid += 1
        t = self.stack.enter_context(self.kb.nc.psum_tensor("%s_%s_%d" % (self.name, name, self.kb.uid), list(shape), dt))
        b = Buf(t)
        self.bufs.append(b)
        return b

    def __enter__(self):
        return self

    def __exit__(self, *a):
        if a[0] is None:
            self.kb.barrier()
            self.kb.release(self.bufs)
        self.stack.close()
        return False


def bc(ap, shape):
    return ap.broadcast_to(list(shape))


class _Stop(Exception):
    pass


def build(L, NB, depth=2, dbg=(), stop=None):
    T = NB * L
    NT = T // 128
    NS = T // 512
    TPS = L // 128
    nc = bass.Bass("TRN2", target_bir_lowering=False)

    def din(name, shape, dt=F32):
        return nc.dram_tensor(name, list(shape), dt, kind="ExternalInput").ap()

    def dscr(name, shape, dt):
        kind = "ExternalOutput" if name in dbg else "Internal"
        return nc.dram_tensor(name, list(shape), dt, kind=kind).ap()

    x_in = din("x", [T, D])
    w_in = din("w_in", [2, D, DP])
    w_out = din("w_out", [2, 2 * D, D])
    ffn_wg = din("ffn_w_gate", [1, D, DFF])
    ffn_wu = din("ffn_w_up", [1, D, DFF])
    ffn_wd = din("ffn_w_down", [1, DFF, D])
    moe_wg = din("moe_w_gate", [1, NE, D, DFF])
    moe_wu = din("moe_w_up", [1, NE, D, DFF])
    moe_wd = din("moe_w_down", [1, NE, DFF, D])
    g_mix = din("g_mix", [2, 128, 8])
    g_ffn = din("g_ffn", [2, 128, 8])
    g_out = din("g_out", [2, 128, 16])
    cw = din("cw", [2, 128, 16, 4])
    cb = din("cb", [2, 128, 16])
    hp = din("hp", [2, 128, 64])
    nfb = din("nfb", [2, 16, 1])
    rw = din("rw", [128, 8, 8])
    g_fin = din("g_fin", [128, D])
    c_id_b = din("c_id_b", [128, 128], BF16)
    c_id_f = din("c_id_f", [128, 128])
    c_tri = din("c_tri", [128, 128])
    c_ones = din("c_ones", [128, 128])
    c_mb4 = din("c_mb4", [128, 512])
    c_tri01 = din("c_tri01", [128, 128], BF16)
    out_d = nc.dram_tensor("out", [T, D], F32, kind="ExternalOutput").ap()

    xa = dscr("xa", [T, D], F32)
    xb = dscr("xb", [T, D], F32)
    hT = dscr("hT", [D, T], BF16)
    zs = dscr("zs", [T, D], BF16)
    xbcT = dscr("xbcT", [2 * D, T], BF16)
    qa = dscr("qa", [NB, NH, 70, L], BF16)
    ka = dscr("ka", [NB, NH, 70, L], BF16)
    va = dscr("va", [T, NH, 65], BF16)
    oT = dscr("oT", [D, T], F32)
    rsum = dscr("rsum", [NH, T], F32)
    yT = dscr("yT", [D, T], BF16)
    dts = dscr("dts", [T, NH], F32)
    cmb = dscr("cmb", [T, NE], F32)

    stack = ExitStack()
    kb = KB(nc, stack)
    B_xa, B_xb, B_hT, B_zs, B_xbcT, B_qa, B_ka, B_va, B_oT, B_rs, B_yT, B_dts, B_cmb, B_out = [
        Buf(None, dram=True) for _ in range(14)]
    B_in = Buf(None, dram=True)

    def sbp(name, shape, dt):
        return Buf(stack.enter_context(nc.sbuf_tensor(name, list(shape), dt)))

    idb = sbp("idb", [128, 128], BF16)
    idf = sbp("idf", [128, 128], F32)
    tri = sbp("tri", [128, 128], F32)
    ones = sbp("ones", [128, 128], F32)
    mb4 = sbp("mb4", [128, 512], F32)
    tri01 = sbp("tri01", [128, 128], BF16)
    onesb = sbp("onesb", [128, 128], BF16)
    for b, src in ((idb, c_id_b), (idf, c_id_f), (tri, c_tri), (ones, c_ones), (mb4, c_mb4), (tri01, c_tri01)):
        kb.dma("sp", b[:], src, b, reads=[B_in], writes=[b])
    kb.op("dve", lambda e: e.tensor_copy(out=onesb[:], in_=ones[:]), reads=[ones], writes=[onesb])
    kb.barrier()

    def load_w(ph, dst, kcs, cols, src_fn, gain, stg, col0=0):
        i = 0
        CH = stg[0].t.shape[1]
        for kc in range(kcs):
            for c0 in range(0, cols, CH):
                cn = min(CH, cols - c0)
                s = stg[i % len(stg)]
                q = "sp" if i % 2 == 0 else "act"
                kb.dma(q, s[:, 0:cn], src_fn(kc)[:, c0:c0 + cn], s, reads=[B_in], writes=[s])
                e = ("dve", "pool")[i % 2]
                if gain is None:
                    kb.op(e, lambda en, s=s, kc=kc, c0=c0, cn=cn: en.tensor_copy(
                        out=dst[:, kc, col0 + c0:col0 + c0 + cn], in_=s[:, 0:cn]), reads=[s], writes=[dst])
                else:
                    kb.op(e, lambda en, s=s, kc=kc, c0=c0, cn=cn: en.tensor_scalar(
                        out=dst[:, kc, col0 + c0:col0 + c0 + cn], in0=s[:, 0:cn], scalar1=gain[:, kc:kc + 1],
                        scalar2=None, op0=ALU.mult), reads=[s], writes=[dst], sreads=[gain])
                i += 1

    def rstd_gen(ph_tmp, src, ssq, rs, junk, n=D):
        kb.op("act", lambda e: e.activation(out=junk[:, 0:n], in_=src, func=AF.Square, accum_out=ssq[:, 0:1]),
              reads=[ph_tmp], writes=[junk, ssq])
        yield
        kb.op("act", lambda e: e.activation(out=rs[:, 0:1], in_=ssq[:, 0:1], func=AF.Sqrt, scale=1.0 / n, bias=EPS),
              reads=[ssq], writes=[rs])
        yield
        kb.op("dve", lambda e: e.reciprocal(out=rs[:, 0:1], in_=rs[:, 0:1]), reads=[rs], writes=[rs])
        yield

    def rstd_of(*a, **k):
        for _ in rstd_gen(*a, **k):
            pass

    class NormT:
        def __init__(self, ph, nhs=2, pT=None, hs=None):
            self.ph = ph
            self.nhs = nhs if hs is None else len(hs)
            self.ssq = [ph.sb("n_ssq%d" % i, [128, 1], F32) for i in range(2)]
            self.rs = [ph.sb("n_rs%d" % i, [128, 1], F32) for i in range(2)]
            self.junk = ph.sb("n_junk", [128, D], BF16)
            self.hb = [ph.sb("n_hb%d" % i, [128, D], BF16) for i in range(2)]
            self.pT = ph.ps("n_pT", [128, D], BF16) if pT is None else pT
            self.hs = [ph.sb("n_hs%d" % i, [128, 8, 512], BF16) for i in range(nhs)] if hs is None else hs
            self.n = 0

        def emit(self, xt, tile):
            for _ in self.emit_gen(xt, tile):
                pass
            return self.last_rs

        def emit_gen(self, xt, tile):
            i = self.n % 2
            self.n += 1
            ssq, rs, hb = self.ssq[i], self.rs[i], self.hb[i]
            self.last_rs = rs
            yield from rstd_gen(xt, xt[:, :], ssq, rs, self.junk)
            kb.op("dve", lambda e: e.tensor_scalar(out=hb[:, :], in0=xt[:, :], scalar1=rs[:, 0:1], scalar2=None,
                                                   op0=ALU.mult), reads=[xt], writes=[hb], sreads=[rs])
            yield
            pT = self.pT

            def tr(e):
                ins = None
                for kc in range(8):
                    ins = e.transpose(pT[:, kc * 128:(kc + 1) * 128], hb[:, kc * 128:(kc + 1) * 128], idb[:, :])
                return ins
            kb.op("pe", tr, reads=[hb, idb], writes=[pT])
            yield
            s, tt = tile // 4, tile % 4
            hs = self.hs[s % self.nhs]
            kb.op("act", lambda e: e.copy(out=hs[:, :, tt * 128:(tt + 1) * 128],
                                          in_=pT[:, :].rearrange("p (k t) -> p k t", k=8)), reads=[pT], writes=[hs])
            yield
            if tt == 3:
                kb.dma("pool", hT.rearrange("(k p) t -> p k t", p=128)[:, :, s * 512:(s + 1) * 512], hs[:, :, :], hs,
                       reads=[hs], writes=[B_hT])
            yield

    def chk(name):
        if stop == name:
            raise _Stop()

    try:
      for layer in range(depth):
          x_src, B_xsrc = (x_in, B_in) if layer == 0 else (xb, B_xb)

          if layer == 0:
              with Phase(kb, "n1") as ph:
                  nt = NormT(ph)
                  xts = [ph.sb("xt%d" % i, [128, D], F32) for i in range(3)]
                  for t in range(NT):
                      xt = xts[t % 3]
                      kb.dma("sp", xt[:, :], x_src[t * 128:(t + 1) * 128, :], xt, reads=[B_xsrc], writes=[xt])
                      nt.emit(xt, t)

          chk("n1")
          with Phase(kb, "a1") as ph:
              W = ph.sb("W", [128, 8, 3072], BF16)
              gm = ph.sb("gm", [128, 8], F32)
              cwt = ph.sb("cwt", [128, 16, 4], F32)
              cbt = ph.sb("cbt", [128, 16], F32)
              stg = [ph.sb("stg%d" % i, [128, 1536], F32) for i in range(3)]
              kb.dma("sp", gm[:, :], g_mix[layer], gm, reads=[B_in], writes=[gm])
              kb.dma("sp", cwt[:, :, :], cw[layer], cwt, reads=[B_in], writes=[cwt])
              kb.dma("sp", cbt[:, :], cb[layer], cbt, reads=[B_in], writes=[cbt])
              wv = w_in[layer].rearrange("(k p) n -> k p n", p=128)
              load_w(ph, W, 8, 3072, lambda kc: wv[kc, :, 0:3072], gm, stg)
              hts = [ph.sb("hts%d" % i, [128, 8, 512], BF16) for i in range(2)]
              pss = [ph.ps("ps%d" % i, [128, 512], F32) for i in range(6)]
              zst = [ph.sb("zst%d" % i, [128, D], BF16) for i in range(2)]
              xr = [ph.sb("xr%d" % i, [128, 515], F32) for i in range(4)]
              halos = [ph.sb("halo%d" % i, [128, 8, 3], F32) for i in range(2)]
              acc = [ph.sb("acc%d" % i, [128, 512], F32) for i in range(4)]
              xst = [ph.sb("xst%d" % i, [128, 16, 512], BF16) for i in range(2)]
              pi = 0
              for s in range(NS):
                  ht = hts[s % 2]
                  kb.dma("sp", ht[:, :, :], hT.rearrange("(k p) t -> p k t", p=128)[:, :, s * 512:(s + 1) * 512], ht,
                         reads=[B_hT], writes=[ht])
                  if (s * 512) % L == 0:
                      for hi_, he_ in enumerate(("dve", "pool")):
                          kb.op(he_, lambda e, hi_=hi_: e.memset(halos[hi_][:, :, :], 0.0), writes=[halos[hi_]], fence=True)
                  for tt in range(4):
                      zt = zst[tt % 2]
                      for half in range(2):
                          p = pss[pi % 6]
                          pi += 1

                          def mm(e, p=p, tt=tt, half=half):
                              ins = None
                              for kc in range(8):
                                  ins = e.matmul(p[:, :], lhsT=ht[:, kc, tt * 128:(tt + 1) * 128],
                                                 rhs=W[:, kc, half * 512:(half + 1) * 512], start=(kc == 0), stop=(kc == 7))
                              return ins
                          kb.op("pe", mm, reads=[ht, W], writes=[p])
                          kb.op("act", lambda e, p=p, half=half, zt=zt: e.copy(out=zt[:, half * 512:(half + 1) * 512], in_=p[:, :]),
                                reads=[p], writes=[zt])
                      tile = s * 4 + tt
                      kb.dma("pool", zs[tile * 128:(tile + 1) * 128, :], zt[:, :], zt, reads=[zt], writes=[B_zs])
                  xs_t = xst[s % 2]
                  for c in range(16):
                      p = pss[pi % 6]
                      pi += 1

                      def mm(e, p=p, c=c):
                          ins = None
                          for kc in range(8):
                              ins = e.matmul(p[:, :], lhsT=W[:, kc, 1024 + c * 128:1024 + (c + 1) * 128], rhs=ht[:, kc, :],
                                             start=(kc == 0), stop=(kc == 7))
                          return ins
                      kb.op("pe", mm, reads=[ht, W], writes=[p])
                      r = xr[c % 4]
                      a = acc[c % 4]
                      ce = "dve"
                      halo = halos[c % 2]
                      kb.op("act", lambda e, p=p, r=r: e.copy(out=r[:, 3:515], in_=p[:, :]), reads=[p], writes=[r])
                      kb.op(ce, lambda e, r=r, c=c, halo=halo: e.tensor_copy(out=r[:, 0:3], in_=halo[:, c // 2, :]), reads=[halo], writes=[r])
                      kb.op(ce, lambda e, r=r, a=a, c=c: e.tensor_scalar(out=a[:, :], in0=r[:, 0:512], scalar1=cwt[:, c, 0:1],
                                                                     scalar2=None, op0=ALU.mult),
                            reads=[r], writes=[a], sreads=[cwt])
                      for k in range(1, 4):
                          kb.op(ce, lambda e, r=r, a=a, c=c, k=k: e.scalar_tensor_tensor(
                              out=a[:, :], in0=r[:, k:k + 512], scalar=cwt[:, c, k:k + 1], in1=a[:, :], op0=ALU.mult, op1=ALU.add),
                              reads=[r, a], writes=[a], sreads=[cwt])
                      kb.op(ce, lambda e, r=r, c=c, halo=halo: e.tensor_copy(out=halo[:, c // 2, :], in_=r[:, 512:515]), reads=[r], writes=[halo])
                      kb.op("act", lambda e, a=a, c=c, xs_t=xs_t: e.activation(out=xs_t[:, c, :], in_=a[:, :], func=AF.Silu,
                                                                            bias=cbt[:, c:c + 1]),
                            reads=[a], writes=[xs_t], sreads=[cbt])
                  kb.dma("pool", xbcT.rearrange("(c p) t -> p c t", p=128)[:, :, s * 512:(s + 1) * 512], xs_t[:, :, :], xs_t,
                         reads=[xs_t], writes=[B_xbcT])

          chk("a1")
          with Phase(kb, "a2") as ph:
              NW = DP - 3072
              W = ph.sb("W", [128, 8, NW], BF16)
              gm = ph.sb("gm", [128, 8], F32)
              hpt = ph.sb("hpt", [128, 64], F32)
              nfbt = ph.sb("nfbt", [16, 1], F32)
              stg = [ph.sb("stg%d" % i, [128, 1552], F32) for i in range(3)]
              kb.dma("sp", gm[:, :], g_mix[layer], gm, reads=[B_in], writes=[gm])
              kb.dma("sp", hpt[:, :], hp[layer], hpt, reads=[B_in], writes=[hpt])
              kb.dma("sp", nfbt[:, :], nfb[layer], nfbt, reads=[B_in], writes=[nfbt])
              wv = w_in[layer].rearrange("(k p) n -> k p n", p=128)
              load_w(ph, W, 8, NW, lambda kc: wv[kc, :, 3072:DP], gm, stg)
              hts = [ph.sb("hts%d" % i, [128, 8, 512], BF16) for i in range(2)]
              pss = [ph.ps("ps%d" % i, [128, 512], F32) for i in range(6)]
              psd = ph.ps("psd", [128, 512], F32)
              psf = ph.ps("psf", [128, 512], F32)
              qst = [ph.sb("qst%d" % i, [128, 8, 512], BF16) for i in range(2)]
              kst = [ph.sb("kst%d" % i, [128, 8, 512], BF16) for i in range(2)]
              vst = [ph.sb("vst%d" % i, [128, NH, 65], BF16) for i in range(2)]
              dtt = [ph.sb("dtt%d" % i, [128, 16], F32) for i in range(2)]
              Gs = [ph.sb("G%d" % i, [16, 512], F32) for i in range(2)]
              spf = [ph.sb("spf%d" % i, [16, 512], F32) for i in range(2)]
              g8 = ph.sb("g8", [16, 512], F32)
              gh = [ph.sb("gh%d" % i, [16, 512], BF16) for i in range(6)]
              ngh = [ph.sb("ngh%d" % i, [16, 512], BF16) for i in range(6)]
              g32 = ph.sb("g32", [16, 512], F32)
              onl = ph.sb("onl", [16, 512], BF16)
              onf = ph.sb("onf", [16, 512], F32)
              kb.op("pool", lambda e: e.memset(onl[:, :], 1.0), writes=[onl], fence=True)
              kb.op("pool", lambda e: e.memset(onf[:, :], 1.0), writes=[onf], fence=True)
              kb.op("dve", lambda e: e.tensor_scalar(out=nfbt[:, :], in0=nfbt[:, :], scalar1=-1.0, scalar2=None, op0=ALU.mult),
                    reads=[nfbt], writes=[nfbt])
              for v_ in vst:
                  kb.op("pool", lambda e, v_=v_: e.memset(v_[:, :, :], 1.0), writes=[v_], fence=True)
              pi = 0
              for s in range(NS):
                  b_, t0 = (s * 512) // L, (s * 512) % L
                  ht = hts[s % 2]
                  kb.dma("sp", ht[:, :, :], hT.rearrange("(k p) t -> p k t", p=128)[:, :, s * 512:(s + 1) * 512], ht,
                         reads=[B_hT], writes=[ht])
                  for which, st_, dd, Bd, off in (("q", qst[s % 2], qa, B_qa, 16), ("k", kst[s % 2], ka, B_ka, 16 + 1024)):
                      for c in range(8):
                          p = pss[pi % 6]
                          pi += 1

                          def mm(e, p=p, c=c, off=off):
                              ins = None
                              for kc in range(8):
                                  ins = e.matmul(p[:, :], lhsT=W[:, kc, off + c * 128:off + (c + 1) * 128], rhs=ht[:, kc, :],
                                                 start=(kc == 0), stop=(kc == 7))
                              return ins
                          kb.op("pe", mm, reads=[ht, W], writes=[p])
                          ce = ("act", "dve")[c % 2]
                          if ce == "act":
                              kb.op("act", lambda e, p=p, c=c, st_=st_: e.copy(out=st_[:, c, :], in_=p[:, :]), reads=[p], writes=[st_])
                          else:
                              kb.op("dve", lambda e, p=p, c=c, st_=st_: e.tensor_copy(out=st_[:, c, :], in_=p[:, :]), reads=[p], writes=[st_])
                      for par in range(2):
                          dst = dd[b_, :, 0:64, t0:t0 + 512].rearrange("(c two) d t -> two d c t", two=2)[par]
                          kb.dma("pool", dst, st_[par * 64:(par + 1) * 64, :, :], st_, reads=[st_], writes=[Bd])
                  for tt in range(4):
                      tile = s * 4 + tt
                      vt = vst[tt % 2]
                      for half in range(2):
                          p = pss[pi % 6]
                          pi += 1

                          def mm(e, p=p, tt=tt, half=half):
                              ins = None
                              for kc in range(8):
                                  ins = e.matmul(p[:, :], lhsT=ht[:, kc, tt * 128:(tt + 1) * 128],
                                                 rhs=W[:, kc, 2064 + half * 512:2064 + (half + 1) * 512], start=(kc == 0), stop=(kc == 7))
                              return ins
                          kb.op("pe", mm, reads=[ht, W], writes=[p])
                          kb.op("act", lambda e, p=p, half=half, vt=vt: e.copy(
                              out=vt[:, half * 8:(half + 1) * 8, 0:64], in_=p[:, :].rearrange("p (h d) -> p h d", d=64)),
                              reads=[p], writes=[vt])
                      kb.dma("pool", va[tile * 128:(tile + 1) * 128, :, :], vt[:, :, :], vt, reads=[vt], writes=[B_va])

                      def mmd(e, tt=tt):
                          ins = None
                          for kc in range(8):
                              ins = e.matmul(psd[:, 0:16], lhsT=ht[:, kc, tt * 128:(tt + 1) * 128], rhs=W[:, kc, 0:16],
                                             start=(kc == 0), stop=(kc == 7))
                          return ins
                      kb.op("pe", mmd, reads=[ht, W], writes=[psd])
                      d_ = dtt[tt % 2]
                      kb.op("dve", lambda e, d_=d_: e.tensor_tensor(out=d_[:, :], in0=psd[:, 0:16], in1=hpt[:, 0:16], op=ALU.add),
                            reads=[psd, hpt], writes=[d_])
                      kb.op("act", lambda e, d_=d_: e.activation(out=d_[:, :], in_=d_[:, :], func=AF.Exp), reads=[d_], writes=[d_])
                      kb.op("act", lambda e, d_=d_: e.activation(out=d_[:, :], in_=d_[:, :], func=AF.Ln, bias=1.0), reads=[d_], writes=[d_])
                      kb.dma("pool", dts[tile * 128:(tile + 1) * 128, :], d_[:, :], d_, reads=[d_], writes=[B_dts])

                  def mmf(e):
                      ins = None
                      for kc in range(8):
                          ins = e.matmul(psf[0:16, :], lhsT=W[:, kc, 3088:3104], rhs=ht[:, kc, :], start=(kc == 0), stop=(kc == 7))
                      return ins
                  kb.op("pe", mmf, reads=[ht, W], writes=[psf])
                  sp_ = spf[s % 2]
                  kb.op("act", lambda e, sp_=sp_: e.activation(out=sp_[:, :], in_=psf[0:16, :], func=AF.Exp, scale=-1.0, bias=nfbt[:, 0:1]),
                        reads=[psf], writes=[sp_], sreads=[nfbt])
                  kb.op("act", lambda e, sp_=sp_: e.activation(out=sp_[:, :], in_=sp_[:, :], func=AF.Ln, bias=1.0), reads=[sp_], writes=[sp_])
                  G = Gs[s % 2]
                  Gp = Gs[(s + 1) % 2]
                  if t0 == 0:
                      kb.op("dve", lambda e, sp_=sp_, G=G: e.tensor_tensor_scan(out=G[:, :], data0=onf[:, :], data1=sp_[:, :],
                                                                               initial=0.0, op0=ALU.mult, op1=ALU.add),
                            reads=[sp_, onf], writes=[G])
                  else:
                      kb.op("dve", lambda e, sp_=sp_, G=G, Gp=Gp: e.tensor_tensor_scan(
                          out=G[:, :], data0=onf[:, :], data1=sp_[:, :], initial=Gp[:, 511:512],
                          op0=ALU.mult, op1=ALU.add), reads=[sp_, onf], writes=[G], sreads=[Gp])
                  kb.op("dve", lambda e, G=G: e.tensor_scalar(out=g8[:, :], in0=G[:, :], scalar1=8.0, scalar2=None, op0=ALU.mult),
                        reads=[G], writes=[g8])
                  for j in range(3):
                      gj, ngj = gh[(s % 2) * 3 + j], ngh[(s % 2) * 3 + j]
                      kb.op("dve", lambda e, gj=gj: e.tensor_copy(out=gj[:, :], in_=g8[:, :]), reads=[g8], writes=[gj])
                      kb.op("dve", lambda e, gj=gj: e.tensor_copy(out=g32[:, :], in_=gj[:, :]), reads=[gj], writes=[g32])
                      if j < 2:
                          kb.op("dve", lambda e: e.tensor_sub(out=g8[:, :], in0=g8[:, :], in1=g32[:, :]), reads=[g8, g32], writes=[g8])
                      kb.op("dve", lambda e, ngj=ngj: e.tensor_scalar(out=ngj[:, :], in0=g32[:, :], scalar1=-1.0, scalar2=None,
                                                                    op0=ALU.mult), reads=[g32], writes=[ngj])
                      kb.dma("pool", qa[b_, :, 64 + j, t0:t0 + 512], ngj[:, :], ngj, reads=[ngj], writes=[B_qa])
                      kb.dma("pool", qa[b_, :, 67 + j, t0:t0 + 512], onl[:, :], onl, reads=[onl], writes=[B_qa])
                      kb.dma("pool", ka[b_, :, 64 + j, t0:t0 + 512], onl[:, :], onl, reads=[onl], writes=[B_ka])
                      kb.dma("pool", ka[b_, :, 67 + j, t0:t0 + 512], gj[:, :], gj, reads=[gj], writes=[B_ka])

          chk("a2")
          with Phase(kb, "ssd") as ph:
              hpt = ph.sb("hpt", [128, 64], F32)
              abc = ph.sb("abc", [128, 16], F32)
              kb.dma("sp", hpt[:, :], hp[layer], hpt, reads=[B_in], writes=[hpt])
              kb.op("act", lambda e: e.activation(out=abc[:, :], in_=hpt[:, 16:32], func=AF.Exp), reads=[hpt], writes=[abc])
              kb.op("dve", lambda e: e.tensor_scalar(out=abc[:, :], in0=abc[:, :], scalar1=-1.0, scalar2=None, op0=ALU.mult),
                    reads=[abc], writes=[abc])
              def ssd_stream(si, slist):
                  sfx = "_%d" % si
                  xbt = [ph.sb("xbt%d" % i + sfx, [128, 16, 512], BF16) for i in range(1)]
                  zt_ = [ph.sb("zt%d" % i + sfx, [128, D], BF16) for i in range(2)]
                  dtb = [ph.sb("dtb%d" % i + sfx, [128, 16], F32) for i in range(2)]
                  b0 = ph.ps("b0" + sfx, [128, 512], F32)
                  b1 = ph.ps("b1" + sfx, [128, 512], F32)
                  b2 = ph.ps("b2" + sfx, [128, 512], F32)
                  b3 = ph.ps("b3" + sfx, [128, 512], F32)
                  pT = Alias(b0, b0.t[:, :].bitcast(BF16))
                  pR = b0
                  pA = b1
                  pG = b1
                  pY = b1
                  pB = Alias(b2, b2.t[:, :].bitcast(BF16))
                  pYo = b2
                  pS = b3
                  Gsb = ph.sb("Gsb" + sfx, [128, 512], F32)
                  xc = ph.sb("xc" + sfx, [128, NH, 64], BF16)
                  xcd = ph.sb("xcd" + sfx, [128, NH, 64], BF16)
                  xsd = ph.sb("xsd" + sfx, [128, NH, 64], F32)
                  btok = ph.sb("btok" + sfx, [128, 4, 128], BF16)
                  adt = ph.sb("adt" + sfx, [128, 16], F32)
                  acs = ph.sb("acs" + sfx, [128, 32], F32)
                  nacs = ph.sb("nacs" + sfx, [128, 16], F32)
                  dout = ph.sb("dout" + sfx, [128, 16], F32)
                  ea = ph.sb("ea" + sfx, [128, 16], F32)
                  cdec = ph.sb("cdec" + sfx, [128, 16], F32)
                  rr = [ph.sb("rr%d" % i + sfx, [128, 4, 128], F32) for i in range(2)]
                  Es = [ph.sb("Es%d" % i + sfx, [128, 4, 128], F32) for i in range(2)]
                  Mt = [ph.sb("Mt%d" % i + sfx, [128, 4, 128], BF16) for i in range(2)]
                  prev = ph.sb("prev" + sfx, [128, D], F32)
                  prevb = ph.sb("prevb" + sfx, [128, D], BF16)
                  tmp = ph.sb("tmp" + sfx, [128, 512], F32)
                  ysb = ph.sb("ysb" + sfx, [128, D], F32)
                  gz = ph.sb("gz" + sfx, [128, D], F32)
                  ssq = ph.sb("ssq" + sfx, [128, 1], F32)
                  rs = ph.sb("rs" + sfx, [128, 1], F32)
                  junk = ph.sb("junk" + sfx, [128, D], BF16)
                  yb = ph.sb("yb" + sfx, [128, D], BF16)
                  yst = [ph.sb("yst%d" % i + sfx, [128, 8, 512], BF16) for i in range(1)]
                  for s in slist:
                      xt_ = xbt[0]
                      kb.dma("sp", xt_[:, :, :], xbcT.rearrange("(c p) t -> p c t", p=128)[:, :, s * 512:(s + 1) * 512], xt_,
                             reads=[B_xbcT], writes=[xt_])
                      yield
                      ys_ = yst[0]
                      for tt in range(4):
                          tile = s * 4 + tt
                          tk = slice(tt * 128, (tt + 1) * 128)
                          z_ = zt_[tile % 2]
                          d_ = dtb[tile % 2]
                          kb.dma("sp", z_[:, :], zs[tile * 128:(tile + 1) * 128, :], z_, reads=[B_zs], writes=[z_])
                          yield
                          kb.dma("sp", d_[:, :], dts[tile * 128:(tile + 1) * 128, :], d_, reads=[B_dts], writes=[d_])
                          yield
                          if (tile * 128) % L == 0:
                              kb.op("dve", lambda e: e.memset(prev[:, :], 0.0), writes=[prev], fence=True)
                              yield
                              kb.op("pool", lambda e: e.memset(prevb[:, :], 0.0), writes=[prevb], fence=True)
                              yield
                          def trx(e, tk=tk):
                              ins = None
                              for c in range(8):
                                  ins = e.transpose(pT[:, c * 128:(c + 1) * 128], xt_[:, c, tk], idb[:, :])
                              return ins
                          kb.op("pe", trx, reads=[xt_, idb], writes=[pT])
                          yield

                          def trb(e, tk=tk):
                              ins = None
                              for c in range(4):
                                  ins = e.transpose(pB[:, c * 128:(c + 1) * 128], xt_[:, 8 + c, tk], idb[:, :])
                              return ins
                          kb.op("pe", trb, reads=[xt_, idb], writes=[pB])
                          yield
                          kb.op("act", lambda e: e.copy(out=btok[:, :, :], in_=pB[:, 0:512].rearrange("p (g n) -> p g n", g=4)),
                                reads=[pB], writes=[btok])
                          yield
                          kb.op("dve", lambda e, d_=d_: e.tensor_tensor(out=adt[:, :], in0=d_[:, :], in1=abc[:, :], op=ALU.mult),
                                reads=[d_, abc], writes=[adt])
                          yield

                          def mma(e):
                              e.matmul(pA[:, 0:16], lhsT=tri[:, :], rhs=adt[:, :], start=True, stop=True)
                              return e.matmul(pA[:, 16:32], lhsT=ones[:, :], rhs=adt[:, :], start=True, stop=True)
                          kb.op("pe", mma, reads=[tri, ones, adt], writes=[pA])
                          yield
                          kb.op("dve", lambda e: e.tensor_copy(out=acs[:, :], in_=pA[:, 0:32]), reads=[pA], writes=[acs])
                          yield
                          kb.op("dve", lambda e: e.tensor_scalar(out=nacs[:, :], in0=acs[:, 0:16], scalar1=-1.0, scalar2=None, op0=ALU.mult),
                                reads=[acs], writes=[nacs])
                          yield
                          kb.op("dve", lambda e: e.tensor_sub(out=dout[:, :], in0=acs[:, 16:32], in1=acs[:, 0:16]), reads=[acs], writes=[dout])
                          yield
                          kb.op("act", lambda e: e.activation(out=dout[:, :], in_=dout[:, :], func=AF.Exp), reads=[dout], writes=[dout])
                          yield
                          kb.op("act", lambda e: e.activation(out=ea[:, :], in_=acs[:, 0:16], func=AF.Exp), reads=[acs], writes=[ea])
                          yield
                          kb.op("act", lambda e: e.activation(out=cdec[:, :], in_=acs[:, 16:32], func=AF.Exp), reads=[acs], writes=[cdec])
                          yield
                          pT3 = pT[:, :].rearrange("p (h d) -> p h d", d=64)
                          kb.op("dve", lambda e, d_=d_: e.tensor_tensor(out=xc[:, :, :], in0=pT3, in1=bc(d_[:, 0:16].unsqueeze(2), [128, 16, 64]),
                                                                       op=ALU.mult), reads=[pT, d_], writes=[xc])
                          yield
                          kb.op("pool", lambda e: e.tensor_tensor(out=xcd[:, :, :], in0=xc[:, :, :], in1=bc(dout[:, 0:16].unsqueeze(2), [128, 16, 64]),
                                                                  op=ALU.mult), reads=[xc, dout], writes=[xcd])
                          yield
                          kb.op("dve", lambda e: e.tensor_tensor(out=xsd[:, :, :], in0=pT3, in1=bc(hpt[:, 32:48].unsqueeze(2), [128, 16, 64]),
                                                                 op=ALU.mult), reads=[pT, hpt], writes=[xsd])
                          yield
                          def mmg(e, tk=tk):
                              ins = None
                              for g in range(4):
                                  ins = e.matmul(pG[:, g * 128:(g + 1) * 128], lhsT=xt_[:, 8 + g, tk], rhs=xt_[:, 12 + g, tk], start=True, stop=True)
                              return ins
                          kb.op("pe", mmg, reads=[xt_], writes=[pG])
                          yield
                          kb.op("act", lambda e: e.copy(out=Gsb[:, :], in_=pG[:, :]), reads=[pG], writes=[Gsb])
                          yield
                          kb.op("act", lambda e, z_=z_: e.activation(out=gz[:, :], in_=z_[:, :], func=AF.Silu), reads=[z_], writes=[gz])
                          yield
                          for half in range(2):
                              for gg in range(2):
                                  g = half * 2 + gg
                                  r_ = rr[g % 2]
                                  E_ = Es[g % 2]
                                  M_ = Mt[g % 2]
                                  kb.op("pool", lambda e, r_=r_, g=g: e.tensor_tensor(
                                      out=r_[:, :, :], in0=bc(tri[:, :].unsqueeze(1), [128, 4, 128]),
                                      in1=bc(adt[:, 4 * g:4 * g + 4].unsqueeze(2), [128, 4, 128]), op=ALU.mult),
                                      reads=[tri, adt], writes=[r_])
                                  yield

                                  def mmr(e, r_=r_):
                                      e.matmul(pR[:, :], lhsT=ones[:, :], rhs=r_[:, :, :].rearrange("p a b -> p (a b)"), start=True, stop=False)
                                      return e.matmul(pR[:, :], lhsT=idf[:, :], rhs=mb4[:, :], start=False, stop=True)
                                  kb.op("pe", mmr, reads=[ones, idf, mb4, r_], writes=[pR])
                                  yield
                                  for r in range(4):
                                      kb.op("act", lambda e, E_=E_, r=r, g=g: e.activation(
                                          out=E_[:, r, :], in_=pR[:, r * 128:(r + 1) * 128], func=AF.Exp, bias=nacs[:, 4 * g + r:4 * g + r + 1]),
                                          reads=[pR], writes=[E_], sreads=[nacs])
                                      yield
                                  kb.op("dve", lambda e, E_=E_, M_=M_, g=g: e.tensor_tensor(
                                      out=M_[:, :, :], in0=E_[:, :, :], in1=bc(Gsb[:, g * 128:(g + 1) * 128].unsqueeze(1), [128, 4, 128]), op=ALU.mult),
                                      reads=[E_, Gsb], writes=[M_])
                                  yield

                                  def mmy(e, M_=M_, g=g, gg=gg):
                                      ins = None
                                      for r in range(4):
                                          h = 4 * g + r
                                          ins = e.matmul(pY[:, (gg * 4 + r) * 64:(gg * 4 + r + 1) * 64], lhsT=M_[:, r, :], rhs=xc[:, h, :],
                                                         start=True, stop=True)
                                      return ins
                                  kb.op("pe", mmy, reads=[M_, xc], writes=[pY])
                                  yield

                              def mmo(e, half=half, tk=tk):
                                  ins = None
                                  for gg in range(2):
                                      g = half * 2 + gg
                                      ins = e.matmul(pYo[:, gg * 256:(gg + 1) * 256], lhsT=xt_[:, 12 + g, tk], rhs=prevb[:, g * 256:(g + 1) * 256],
                                                     start=True, stop=True)
                                  return ins
                              kb.op("pe", mmo, reads=[xt_, prevb], writes=[pYo])
                              yield

                              def mms(e, half=half):
                                  ins = None
                                  for gg in range(2):
                                      g = half * 2 + gg
                                      ins = e.matmul(pS[:, gg * 256:(gg + 1) * 256], lhsT=btok[:, g, :],
                                                     rhs=xcd[:, 4 * g:4 * g + 4, :].rearrange("p h d -> p (h d)"), start=True, stop=True)
                                  return ins
                              kb.op("pe", mms, reads=[btok, xcd], writes=[pS])
                              yield
                              hs_ = slice(half * 512, (half + 1) * 512)
                              h8 = slice(half * 8, (half + 1) * 8)
                              kb.op("dve", lambda e, h8=h8: e.tensor_tensor(
                                  out=tmp[:, :].rearrange("p (h d) -> p h d", d=64), in0=pYo[:, :].rearrange("p (h d) -> p h d", d=64),
                                  in1=bc(ea[:, h8].unsqueeze(2), [128, 8, 64]), op=ALU.mult), reads=[pYo, ea], writes=[tmp])
                              yield
                              kb.op("dve", lambda e: e.tensor_tensor(out=tmp[:, :], in0=pY[:, :], in1=tmp[:, :], op=ALU.add),
                                    reads=[pY, tmp], writes=[tmp])
                              yield
                              kb.op("pool", lambda e, hs_=hs_, h8=h8: e.tensor_tensor(
                                  out=ysb[:, hs_], in0=tmp[:, :], in1=xsd[:, h8, :].rearrange("p h d -> p (h d)"), op=ALU.add),
                                  reads=[tmp, xsd], writes=[ysb])
                              yield
                              kb.op("dve", lambda e, hs_=hs_, h8=h8: e.tensor_tensor(
                                  out=prev[:, hs_].rearrange("p (h d) -> p h d", d=64), in0=prev[:, hs_].rearrange("p (h d) -> p h d", d=64),
                                  in1=bc(cdec[:, h8].unsqueeze(2), [128, 8, 64]), op=ALU.mult), reads=[prev, cdec], writes=[prev])
                              yield
                              kb.op("dve", lambda e, hs_=hs_: e.tensor_tensor(out=prev[:, hs_], in0=prev[:, hs_], in1=pS[:, :], op=ALU.add),
                                    reads=[prev, pS], writes=[prev])
                              yield
                              kb.op("pool", lambda e, hs_=hs_: e.tensor_copy(out=prevb[:, hs_], in_=prev[:, hs_]), reads=[prev], writes=[prevb])
                              yield
                          kb.op("dve", lambda e: e.tensor_tensor(out=ysb[:, :], in0=ysb[:, :], in1=gz[:, :], op=ALU.mult),
                                reads=[ysb, gz], writes=[ysb])
                          yield
                          rstd_of(ysb, ysb[:, :], ssq, rs, junk)
                          yield
                          kb.op("dve", lambda e: e.tensor_scalar(out=yb[:, :], in0=ysb[:, :], scalar1=rs[:, 0:1], scalar2=None, op0=ALU.mult),
                                reads=[ysb], writes=[yb], sreads=[rs])
                          yield

                          def try_(e):
                              ins = None
                              for c in range(8):
                                  ins = e.transpose(pT[:, c * 128:(c + 1) * 128], yb[:, c * 128:(c + 1) * 128], idb[:, :])
                              return ins
                          kb.op("pe", try_, reads=[yb, idb], writes=[pT])
                          yield
                          kb.op("act", lambda e, tt=tt, ys_=ys_: e.copy(out=ys_[:, :, tt * 128:(tt + 1) * 128],
                                                                      in_=pT[:, :].rearrange("p (k t) -> p k t", k=8)), reads=[pT], writes=[ys_])
                          yield
                      kb.dma("pool", yT.rearrange("(k p) t -> p k t", p=128)[:, :, s * 512:(s + 1) * 512], ys_[:, :, :], ys_,
                             reads=[ys_], writes=[B_yT])
                      yield

              if NB == 2:
                  gens = [ssd_stream(si, list(range(si * (L // 512), (si + 1) * (L // 512)))) for si in range(2)]
              else:
                  gens = [ssd_stream(0, list(range(NS)))]
              if len(gens) == 2:
                  for _ in range(36):
                      next(gens[0], None)
              while gens:
                  for g_ in list(gens):
                      try:
                          next(g_)
                      except StopIteration:
                          gens.remove(g_)
          chk("ssd")
          with Phase(kb, "fox") as ph:
              NQ = 3
              qt = [ph.sb("qt%d" % i, [70, L], BF16) for i in range(NQ)]
              kt = [ph.sb("kt%d" % i, [70, L], BF16) for i in range(NQ)]
              vt = [ph.sb("vt%d" % i, [128, TPS, 65], BF16) for i in range(NQ)]
              NR = 6
              LA = 3
              pS_ = [ph.ps("pS%d" % i, [128, 512], F32) for i in range(NR)]
              pO = [ph.ps("pO%d" % i, [65, 512], F32) for i in range(2)]
              Pt = [ph.sb("Pt%d" % i, [128, 512], BF16) for i in range(NR)]
              osb = [ph.sb("osb%d" % i, [65, 512], F32) for i in range(3)]
              heads = [(b_, h) for b_ in range(NB) for h in range(NH)]

              def load_head(n):
                  b_, h = heads[n]
                  q_, k_, v_ = qt[n % NQ], kt[n % NQ], vt[n % NQ]
                  kb.dma("sp", q_[:, :], qa[b_, h], q_, reads=[B_qa], writes=[q_])
                  kb.dma("sp", k_[:, :], ka[b_, h], k_, reads=[B_ka], writes=[k_])
                  kb.dma("sp", v_[:, :, :], va.rearrange("(b i p) h c -> b h p i c", b=NB, p=128)[b_, h], v_, reads=[B_va], writes=[v_])

              steps = []
              jn = 0
              for n, (b_, h) in enumerate(heads):
                  for j in range(L // 512):
                      nk = 4 * j + 4
                      for i in range(nk):
                          steps.append(dict(n=n, b=b_, h=h, j=j, i=i, nk=nk, jn=jn, first=(j == 0 and i == 0)))
                      jn += 1

              def front(t):
                  st = steps[t]
                  n, j, i = st["n"], st["j"], st["i"]
                  if st["first"]:
                      if n == 0:
                          load_head(0)
                      if n + 1 < len(heads):
                          load_head(n + 1)
                  q_, k_ = qt[n % NQ], kt[n % NQ]
                  dd = i - 4 * j
                  c0 = 128 * dd if dd > 0 else 0
                  S_, P_ = pS_[t % NR], Pt[t % NR]
                  kb.op("pe", lambda e: e.matmul(S_[:, c0:512], lhsT=k_[:, i * 128:(i + 1) * 128], rhs=q_[:, j * 512 + c0:(j + 1) * 512],
                                                 start=True, stop=True), reads=[k_, q_], writes=[S_])
                  kb.op("act", lambda e: e.activation(out=P_[:, c0:512], in_=S_[:, c0:512], func=AF.Exp, scale=0.125), reads=[S_], writes=[P_])
                  if dd >= 0:
                      me = ("dve", "pool")[dd % 2]
                      kb.op(me, lambda e: e.tensor_tensor(out=P_[:, c0:c0 + 128], in0=P_[:, c0:c0 + 128], in1=tri01[:, :], op=ALU.mult),
                            reads=[P_, tri01], writes=[P_])

              def back(t):
                  st = steps[t]
                  n, j, i, nk, b_, h = st["n"], st["j"], st["i"], st["nk"], st["b"], st["h"]
                  v_ = vt[n % NQ]
                  dd = i - 4 * j
                  c0 = 128 * dd if dd > 0 else 0
                  P_ = Pt[t % NR]
                  O = pO[st["jn"] % 2]
                  kb.op("pe", lambda e: e.matmul(O[:, c0:512], lhsT=v_[:, i, :], rhs=P_[:, c0:512], start=(i == 0), stop=(i == nk - 1),
                                                 skip_group_check=True), reads=[v_, P_], writes=[O])
                  if i == nk - 1:
                      o_ = osb[st["jn"] % 3]
                      kb.op("dve", lambda e: e.tensor_copy(out=o_[:, :], in_=O[:, :]), reads=[O], writes=[o_])
                      tcol = b_ * L + j * 512
                      kb.dma("pool", oT[h * 64:(h + 1) * 64, tcol:tcol + 512], o_[0:64, :], o_, reads=[o_], writes=[B_oT])
                      kb.dma("pool", rsum[h:h + 1, tcol:tcol + 512], o_[64:65, :], o_, reads=[o_], writes=[B_rs])

              for t in range(len(steps) + LA):
                  if t < len(steps):
                      front(t)
                  if t - LA >= 0:
                      back(t - LA)
          chk("fox")
          moe = (layer % 2 == 1)
          with Phase(kb, "e") as ph:
              W = ph.sb("W", [128, 16, D], BF16)
              go = ph.sb("go", [128, 16], F32)
              stg = [ph.sb("stg%d" % i, [128, 512], F32) for i in range(2)]
              kb.dma("sp", go[:, :], g_out[layer], go, reads=[B_in], writes=[go])
              wv = w_out[layer].rearrange("(k p) n -> k p n", p=128)
              load_w(ph, W, 16, D, lambda kc: wv[kc], go, stg)
              if moe:
                  rwt = ph.sb("rwt", [128, 8, 8], F32)
                  gf = ph.sb("gf", [128, 8], F32)
                  kb.dma("sp", rwt[:, :, :], rw, rwt, reads=[B_in], writes=[rwt])
                  kb.dma("sp", gf[:, :], g_ffn[layer], gf, reads=[B_in], writes=[gf])
                  kb.op("dve", lambda e: e.tensor_tensor(out=rwt[:, :, :], in0=rwt[:, :, :], in1=bc(gf[:, :].unsqueeze(2), [128, 8, 8]),
                                                         op=ALU.mult), reads=[rwt, gf], writes=[rwt])

              def e_stream(si, slist):
                  sfx = "_%d" % si
                  bA = ph.ps("bA" + sfx, [128, 512], F32)
                  bB = ph.ps("bB" + sfx, [128, 512], F32)
                  po = [ph.ps("po%d" % i + sfx, [128, 512], F32) for i in range(2)]
                  osq = ph.sb("osq" + sfx, [128, 8, 512], BF16)
                  nt = NormT(ph, pT=Alias(bA, bA.t[:, :].bitcast(BF16)), hs=[osq])
                  ot = [ph.sb("ot" + sfx, [128, 8, 512], F32)]
                  rt = [ph.sb("rt" + sfx, [128, 8, 512], F32)]
                  ysT = [ph.sb("ysT" + sfx, [128, 8, 512], BF16)]
                  yfT = ph.sb("yfT" + sfx, [128, 8, 512], BF16)
                  rstd = ph.sb("rstd" + sfx, [128, 512], F32)
                  pss = bB
                  xts = [ph.sb("xt%d" % i + sfx, [128, D], F32) for i in range(2)]
                  if moe:
                      pTf = bB
                      pl = bA
                      xTf = ph.sb("xTf" + sfx, [128, D], F32)
                      lg = ph.sb("lg" + sfx, [128, 8], F32)
                      lg2 = ph.sb("lg2" + sfx, [128, 8], F32)
                      m1 = ph.sb("m1" + sfx, [128, 4], F32)
                      mk1 = ph.sb("mk1" + sfx, [128, 8], F32)
                      mk2 = ph.sb("mk2" + sfx, [128, 8], F32)
                      cm = [ph.sb("cm%d" % i + sfx, [128, 8], F32) for i in range(2)]
                  pi = 0
                  xi = 0
                  for s in slist:
                      o_, r_, ys_ = ot[0], rt[0], ysT[0]
                      cs = slice(s * 512, (s + 1) * 512)
                      kb.dma("sp", o_[:, :, :], oT.rearrange("(k p) t -> p k t", p=128)[:, :, cs], o_, reads=[B_oT], writes=[o_])
                      yield
                      for par in range(2):
                          src = rsum.rearrange("(k two) t -> two k t", two=2)[par:par + 1, :, cs]
                          kb.dma("act", r_[par * 64:(par + 1) * 64, :, :], bc(src, [64, 8, 512]), r_, reads=[B_rs], writes=[r_])
                          yield
                      kb.dma("sp", ys_[:, :, :], yT.rearrange("(k p) t -> p k t", p=128)[:, :, cs], ys_, reads=[B_yT], writes=[ys_])
                      yield
                      kb.op("dve", lambda e, r_=r_: e.reciprocal(out=r_[:, :, :], in_=r_[:, :, :]), reads=[r_], writes=[r_])
                      yield
                      kb.op("dve", lambda e, r_=r_, o_=o_: e.tensor_tensor(out=o_[:, :, :], in0=o_[:, :, :], in1=r_[:, :, :], op=ALU.mult),
                            reads=[o_, r_], writes=[o_])
                      yield
                      kb.op("act", lambda e, o_=o_: e.activation(out=osq[:, :, :], in_=o_[:, :, :], func=AF.Square), reads=[o_], writes=[osq])
                      yield

                      def mss(e):
                          ins = None
                          for kc in range(8):
                              ins = e.matmul(pss[:, :], lhsT=onesb[:, :], rhs=osq[:, kc, :], start=(kc == 0), stop=(kc == 7))
                          return ins
                      kb.op("pe", mss, reads=[onesb, osq], writes=[pss])
                      yield
                      kb.op("act", lambda e: e.activation(out=rstd[:, :], in_=pss[:, :], func=AF.Sqrt, scale=1.0 / D, bias=EPS), reads=[pss], writes=[rstd])
                      yield
                      kb.op("dve", lambda e: e.reciprocal(out=rstd[:, :], in_=rstd[:, :]), reads=[rstd], writes=[rstd])
                      yield
                      kb.op("dve", lambda e, o_=o_: e.tensor_tensor(out=yfT[:, :, :], in0=o_[:, :, :], in1=bc(rstd[:, :].unsqueeze(1), [128, 8, 512]),
                                                                   op=ALU.mult), reads=[o_, rstd], writes=[yfT])
                      yield
                      for tt in range(4):
                          tile = s * 4 + tt
                          xt = xts[xi % 2]
                          xi += 1
                          kb.dma("sp", xt[:, :], x_src[tile * 128:(tile + 1) * 128, :], xt, reads=[B_xsrc], writes=[xt])
                          yield
                          for half in range(2):
                              p = po[pi % 2]
                              pi += 1

                              def mm(e, p=p, tt=tt, half=half, ys_=ys_):
                                  ins = None
                                  for kc in range(16):
                                      src = ys_ if kc < 8 else yfT
                                      ins = e.matmul(p[:, :], lhsT=src[:, kc % 8, tt * 128:(tt + 1) * 128], rhs=W[:, kc, half * 512:(half + 1) * 512],
                                                     start=(kc == 0), stop=(kc == 15))
                                  return ins
                              kb.op("pe", mm, reads=[ys_, yfT, W], writes=[p])
                              yield
                              kb.op("dve", lambda e, p=p, half=half, xt=xt: e.tensor_tensor(
                                  out=xt[:, half * 512:(half + 1) * 512], in0=p[:, :], in1=xt[:, half * 512:(half + 1) * 512], op=ALU.add),
                                  reads=[p, xt], writes=[xt])
                              yield
                          kb.dma("pool", xa[tile * 128:(tile + 1) * 128, :], xt[:, :], xt, reads=[xt], writes=[B_xa])
                          yield
                          yield from nt.emit_gen(xt, tile)
                          rs = nt.last_rs
                          if moe:
                              for hh in range(2):
                                  def trf(e, hh=hh, xt=xt):
                                      ins = None
                                      for c in range(4):
                                          kc = hh * 4 + c
                                          ins = e.transpose(pTf[:, c * 128:(c + 1) * 128], xt[:, kc * 128:(kc + 1) * 128], idf[:, :])
                                      return ins
                                  kb.op("pe", trf, reads=[xt, idf], writes=[pTf])
                                  yield
                                  kb.op("act", lambda e, hh=hh: e.copy(out=xTf[:, hh * 512:(hh + 1) * 512], in_=pTf[:, :]), reads=[pTf], writes=[xTf])
                                  yield

                              def mml(e):
                                  ins = None
                                  for kc in range(8):
                                      ins = e.matmul(pl[:, 0:8], lhsT=xTf[:, kc * 128:(kc + 1) * 128], rhs=rwt[:, kc, :], start=(kc == 0), stop=(kc == 7))
                                  return ins
                              kb.op("pe", mml, reads=[xTf, rwt], writes=[pl])
                              yield
                              c_ = cm[tile % 2]
                              kb.op("dve", lambda e, rs=rs: e.tensor_scalar(out=lg[:, :], in0=pl[:, 0:8], scalar1=rs[:, 0:1], scalar2=None, op0=ALU.mult),
                                    reads=[pl], writes=[lg], sreads=[rs])
                              yield
                              kb.op("dve", lambda e: e.reduce_max(out=m1[:, 0:1], in_=lg[:, :], axis=AX.X), reads=[lg], writes=[m1])
                              yield
                              kb.op("dve", lambda e: e.tensor_scalar(out=mk1[:, :], in0=lg[:, :], scalar1=m1[:, 0:1], scalar2=None, op0=ALU.is_ge),
                                    reads=[lg], writes=[mk1], sreads=[m1])
                              yield
                              kb.op("dve", lambda e: e.scalar_tensor_tensor(out=lg2[:, :], in0=mk1[:, :], scalar=NEG, in1=lg[:, :], op0=ALU.mult, op1=ALU.add),
                                    reads=[mk1, lg], writes=[lg2])
                              yield
                              kb.op("dve", lambda e: e.reduce_max(out=m1[:, 1:2], in_=lg2[:, :], axis=AX.X), reads=[lg2], writes=[m1])
                              yield
                              kb.op("dve", lambda e: e.tensor_scalar(out=mk2[:, :], in0=lg2[:, :], scalar1=m1[:, 1:2], scalar2=None, op0=ALU.is_ge),
                                    reads=[lg2], writes=[mk2], sreads=[m1])
                              yield
                              kb.op("dve", lambda e: e.tensor_sub(out=m1[:, 2:3], in0=m1[:, 1:2], in1=m1[:, 0:1]), reads=[m1], writes=[m1])
                              yield
                              kb.op("act", lambda e: e.activation(out=m1[:, 2:3], in_=m1[:, 2:3], func=AF.Sigmoid), reads=[m1], writes=[m1])
                              yield
                              kb.op("dve", lambda e: e.tensor_scalar(out=m1[:, 3:4], in0=m1[:, 2:3], scalar1=-1.0, scalar2=1.0, op0=ALU.mult, op1=ALU.add),
                                    reads=[m1], writes=[m1])
                              yield
                              kb.op("dve", lambda e, c_=c_: e.tensor_scalar(out=c_[:, :], in0=mk1[:, :], scalar1=m1[:, 3:4], scalar2=None, op0=ALU.mult),
                                    reads=[mk1], writes=[c_], sreads=[m1])
                              yield
                              kb.op("dve", lambda e, c_=c_: e.scalar_tensor_tensor(out=c_[:, :], in0=mk2[:, :], scalar=m1[:, 2:3], in1=c_[:, :],
                                                                                  op0=ALU.mult, op1=ALU.add), reads=[mk2, c_], writes=[c_], sreads=[m1])
                              yield
                              kb.dma("pool", cmb[tile * 128:(tile + 1) * 128, :], c_[:, :], c_, reads=[c_], writes=[B_cmb])
                              yield

              if NB == 2:
                  gens = [e_stream(si, list(range(si * (L // 512), (si + 1) * (L // 512)))) for si in range(2)]
              else:
                  gens = [e_stream(0, list(range(NS)))]
              if len(gens) == 2:
                  for _ in range(25 if moe else 17):
                      next(gens[0], None)
              while gens:
                  for g_ in list(gens):
                      try:
                          next(g_)
                      except StopIteration:
                          gens.remove(g_)
          chk("e")
          npass = NE if moe else 1
          last_layer = (layer == depth - 1)
          HF = DFF // 2
          NFH = NFC // 2
          with Phase(kb, "f") as ph:
              WG = [ph.sb("WG%d" % i, [128, 8, HF], BF16) for i in range(2)]
              WU = [ph.sb("WU%d" % i, [128, 8, HF], BF16) for i in range(2)]
              WD = [ph.sb("WD%d" % i, [128, NFH, D], BF16) for i in range(2)]
              gf = ph.sb("gf", [128, 8], F32)
              CHW = 352
              stg = [ph.sb("stg%d" % i, [128, CHW], F32) for i in range(4)]
              kb.dma("sp", gf[:, :], g_ffn[layer], gf, reads=[B_in], writes=[gf])
              hts = [ph.sb("hts%d" % i, [128, 8, 512], BF16) for i in range(2)]
              aT = ph.sb("aT", [128, NFH, 512], BF16)
              sg = [ph.sb("sg%d" % i, [128, 512], F32) for i in range(2)]
              pg = [ph.ps("pg%d" % i, [128, 512], F32) for i in range(2)]
              pu = [ph.ps("pu%d" % i, [128, 512], F32) for i in range(2)]
              po = [ph.ps("po%d" % i, [128, 512], F32) for i in range(3)]
              xts = [ph.sb("xt%d" % i, [128, D], F32) for i in range(2)]
              cmt = [ph.sb("cmt%d" % i, [128, 8], F32) for i in range(2)]
              if last_layer:
                  gfin = ph.sb("gfin", [128, D], F32)
                  kb.dma("sp", gfin[:, :], g_fin, gfin, reads=[B_in], writes=[gfin])
                  ssq = ph.sb("ssq", [128, 1], F32)
                  rsf = ph.sb("rsf", [128, 1], F32)
                  junk = ph.sb("junk", [128, D], BF16)
              else:
                  nt = NormT(ph, nhs=1)
              units = [(e_, hf) for e_ in range(npass) for hf in range(2)]
              lc = [0]

              def wchunks(u):
                  e_, hf = units[u]
                  if moe:
                      wgv = moe_wg[0, e_].rearrange("(k p) n -> k p n", p=128)
                      wuv = moe_wu[0, e_].rearrange("(k p) n -> k p n", p=128)
                      wdv = moe_wd[0, e_].rearrange("(k p) n -> k p n", p=128)
                  else:
                      wgv = ffn_wg[0].rearrange("(k p) n -> k p n", p=128)
                      wuv = ffn_wu[0].rearrange("(k p) n -> k p n", p=128)
                      wdv = ffn_wd[0].rearrange("(k p) n -> k p n", p=128)
                  out = []

                  def mk(dst, kc, c0, cn, src, gain):
                      def f():
                          s_ = stg[lc[0] % 4]
                          lc[0] += 1
                          kb.dma("sp", s_[:, 0:cn], src, s_, reads=[B_in], writes=[s_])
                          if gain:
                              kb.op("pool", lambda en: en.tensor_scalar(out=dst[:, kc, c0:c0 + cn], in0=s_[:, 0:cn], scalar1=gf[:, kc:kc + 1],
                                                                        scalar2=None, op0=ALU.mult), reads=[s_], writes=[dst], sreads=[gf])
                          else:
                              kb.op("pool", lambda en: en.tensor_copy(out=dst[:, kc, c0:c0 + cn], in_=s_[:, 0:cn]), reads=[s_], writes=[dst])
                      return f
                  for dst, wv_ in ((WG[u % 2], wgv), (WU[u % 2], wuv)):
                      for kc in range(8):
                          for c0 in range(0, HF, CHW):
                              out.append(mk(dst, kc, c0, CHW, wv_[kc][:, hf * HF + c0:hf * HF + c0 + CHW], True))
                  for fc in range(NFH):
                      for c0 in range(0, D, CHW):
                          cn = min(CHW, D - c0)
                          out.append(mk(WD[u % 2], fc, c0, cn, wdv[hf * NFH + fc][:, c0:c0 + cn], False))
                  return out

              for f_ in wchunks(0):
                  f_()
              tile_tok = {}
              pi = 0
              for u, (e_, hf) in enumerate(units):
                  Wg, Wu, Wd = WG[u % 2], WU[u % 2], WD[u % 2]
                  pend = wchunks(u + 1) if u + 1 < len(units) else []
                  final_unit = (u == len(units) - 1)
                  acc_src = xa if u == 0 else xb
                  gi = [0]
                  gu_of = {}

                  def load_ht(s):
                      ht = hts[s % 2]
                      kb.dma("sp", ht[:, :, :], hT.rearrange("(k p) t -> p k t", p=128)[:, :, s * 512:(s + 1) * 512], ht,
                             reads=[B_hT], writes=[ht])

                  def pe_part(s, fc):
                      ht = hts[s % 2]
                      k_ = gi[0] % 2
                      gi[0] += 1
                      gu_of[(s, fc)] = k_
                      g_, u_ = pg[k_], pu[k_]

                      def mmg(e):
                          ins = None
                          for kc in range(8):
                              ins = e.matmul(g_[:, :], lhsT=Wg[:, kc, fc * 128:(fc + 1) * 128], rhs=ht[:, kc, :], start=(kc == 0), stop=(kc == 7))
                          return ins

                      def mmu(e):
                          ins = None
                          for kc in range(8):
                              ins = e.matmul(u_[:, :], lhsT=Wu[:, kc, fc * 128:(fc + 1) * 128], rhs=ht[:, kc, :], start=(kc == 0), stop=(kc == 7))
                          return ins
                      kb.op("pe", mmg, reads=[Wg, ht], writes=[g_])
                      kb.op("pe", mmu, reads=[Wu, ht], writes=[u_])

                  def ew_part(s, fc):
                      k_ = gu_of.pop((s, fc))
                      g_, u_, s_ = pg[k_], pu[k_], sg[k_]
                      kb.op("act", lambda e: e.activation(out=s_[:, :], in_=g_[:, :], func=AF.Silu), reads=[g_], writes=[s_])
                      kb.op("dve", lambda e: e.tensor_tensor(out=aT[:, fc, :], in0=u_[:, :], in1=s_[:, :], op=ALU.mult),
                            reads=[u_, s_], writes=[aT])

                  FR = 2
                  load_ht(0)
                  for fc in range(FR):
                      pe_part(0, fc)
                  for s in range(NS):
                      for fc in range(NFH):
                          if fc >= FR:
                              pe_part(s, fc)
                          ew_part(s, fc)
                          if pend:
                              pend.pop(0)()
                      if s + 1 < NS:
                          load_ht(s + 1)
                          for fc in range(FR):
                              pe_part(s + 1, fc)
                      for tt in range(4):
                          tile = s * 4 + tt
                          xt = xts[tile % 2]
                          if tile in tile_tok:
                              kb._wait("sp", {tile_tok[tile][0]: tile_tok[tile][1]})
                          kb.dma("sp", xt[:, :], acc_src[tile * 128:(tile + 1) * 128, :], xt, reads=[B_in], writes=[xt])
                          if moe:
                              c_ = cmt[tile % 2]
                              kb.dma("sp", c_[:, :], cmb[tile * 128:(tile + 1) * 128, :], c_, reads=[B_cmb], writes=[c_])
                          for half in range(2):
                              p = po[pi % 3]
                              pi += 1

                              def mmd(e, p=p, tt=tt, half=half):
                                  ins = None
                                  for fc in range(NFH):
                                      ins = e.matmul(p[:, :], lhsT=aT[:, fc, tt * 128:(tt + 1) * 128], rhs=Wd[:, fc, half * 512:(half + 1) * 512],
                                                     start=(fc == 0), stop=(fc == NFH - 1))
                                  return ins
                              kb.op("pe", mmd, reads=[aT, Wd], writes=[p])
                              hs_ = slice(half * 512, (half + 1) * 512)
                              if moe:
                                  kb.op("dve", lambda e, p=p, xt=xt, hs_=hs_, c_=c_: e.scalar_tensor_tensor(
                                      out=xt[:, hs_], in0=p[:, :], scalar=c_[:, e_:e_ + 1], in1=xt[:, hs_], op0=ALU.mult, op1=ALU.add),
                                      reads=[p, xt], writes=[xt], sreads=[c_])
                              else:
                                  kb.op("dve", lambda e, p=p, xt=xt, hs_=hs_: e.tensor_tensor(out=xt[:, hs_], in0=p[:, :], in1=xt[:, hs_], op=ALU.add),
                                        reads=[p, xt], writes=[xt])
                          if final_unit and last_layer:
                              rstd_of(xt, xt[:, :], ssq, rsf, junk)
                              kb.op("dve", lambda e, xt=xt: e.scalar_tensor_tensor(out=xt[:, :], in0=xt[:, :], scalar=rsf[:, 0:1], in1=gfin[:, :],
                                                                                  op0=ALU.mult, op1=ALU.mult), reads=[xt, gfin], writes=[xt], sreads=[rsf])
                              kb.dma("pool", out_d[tile * 128:(tile + 1) * 128, :], xt[:, :], xt, reads=[xt], writes=[B_out])
                          else:
                              kb.dma("pool", xb[tile * 128:(tile + 1) * 128, :], xt[:, :], xt, reads=[xt], writes=[B_xb])
                              tile_tok[tile] = (xt.sem, xt.sem.cnt)
                              if final_unit:
                                  nt.emit(xt, tile)
                          if pend:
                              pend.pop(0)()
                  while pend:
                      pend.pop(0)()
    except _Stop:
        pass
    kb.barrier()
    stack.close()
    return nc


def _consts():
    i = np.arange(128)
    tri = (i[:, None] <= i[None, :]).astype(np.float32)
    mb = np.where(i[:, None] > i[None, :], NEG, 0.0).astype(np.float32)
    return dict(
        c_id_b=np.eye(128, dtype=np.float32).astype(ml_dtypes.bfloat16),
        c_id_f=np.eye(128, dtype=np.float32),
        c_tri=tri,
        c_ones=np.ones((128, 128), np.float32),
        c_mb4=np.ascontiguousarray(np.tile(mb, (1, 4))),
        c_tri01=tri.astype(ml_dtypes.bfloat16),
    )


def _layout_params(inp):
    f = np.float32
    pm = {}
    pm["g_mix"] = np.ascontiguousarray(inp["mix_norm"].reshape(2, 8, 128).transpose(0, 2, 1)).astype(f)
    pm["g_ffn"] = np.ascontiguousarray(inp["ffn_norm"].reshape(2, 8, 128).transpose(0, 2, 1)).astype(f)
    go = np.concatenate([inp["ssd_norm"], inp["fox_norm"]], axis=1)
    pm["g_out"] = np.ascontiguousarray(go.reshape(2, 16, 128).transpose(0, 2, 1)).astype(f)
    pm["cw"] = np.ascontiguousarray(inp["conv_w"].reshape(2, 4, 16, 128).transpose(0, 3, 2, 1)).astype(f)
    pm["cb"] = np.ascontiguousarray(inp["conv_b"].reshape(2, 16, 128).transpose(0, 2, 1)).astype(f)
    hp = np.concatenate([inp["dt_bias"], inp["a_log"], inp["d_skip"], inp["fox_f_bias"]], axis=1)
    pm["hp"] = np.ascontiguousarray(np.broadcast_to(hp[:, None, :], (2, 128, 64))).astype(f)
    pm["nfb"] = np.ascontiguousarray(inp["fox_f_bias"].reshape(2, 16, 1)).astype(f)
    pm["rw"] = np.ascontiguousarray(inp["router_w"][0].reshape(8, 128, 8).transpose(1, 0, 2)).astype(f)
    pm["g_fin"] = np.ascontiguousarray(np.broadcast_to(inp["final_norm"][None, :], (128, D))).astype(f)
    return pm


_CACHE = {}


def run(inp, L, NB, ncores=8, depth=2, dbg=(), stop=None):
    key = (L, NB, depth, tuple(dbg), stop)
    if key not in _CACHE:
        _CACHE[key] = build(L, NB, depth, dbg, stop)
    nc = _CACHE[key]
    x = np.ascontiguousarray(np.asarray(inp["x"], dtype=np.float32))
    T = NB * L
    xs = x.reshape(-1, T, D)
    shared = dict(_consts())
    shared.update(_layout_params({k: np.asarray(v) for k, v in inp.items()}))
    for k in ("w_in", "w_out", "ffn_w_gate", "ffn_w_up", "ffn_w_down", "moe_w_gate", "moe_w_up", "moe_w_down"):
        shared[k] = np.ascontiguousarray(np.asarray(inp[k], dtype=np.float32))
    in_maps = []
    for c in range(ncores):
        m = dict(shared)
        m["x"] = np.ascontiguousarray(xs[c])
        in_maps.append(m)
    res = run_bass_kernel_spmd(nc, in_maps, core_ids=list(range(ncores)))
    return res


def kernel(**inputs):
    x = np.asarray(inputs["x"])
    Bsz, L, _ = x.shape
    NB = Bsz // 8
    res = run(inputs, L, NB)
    out = np.stack([r["out"] for r in res.results], axis=0).reshape(Bsz, L, D)
    return out.astype(np.float32)
```

```python
import numpy as np
from contextlib import ExitStack
import ml_dtypes
import concourse.bass as bass
import concourse.mybir as mybir
from concourse.bass_utils import run_bass_kernel_spmd

F32 = mybir.dt.float32
BF16 = mybir.dt.bfloat16
AF = mybir.ActivationFunctionType
ALU = mybir.AluOpType
AX = mybir.AxisListType

D = 1024
DP = 6176
DFF = 2816
NFC = DFF // 128
NE = 8
NH = 16
EPS = 1e-5
NEG = -30000.0
NDS = 88


class Sem:
    def __init__(self, h):
        self.h = h
        self.cnt = 0


class Buf:
    def __init__(self, t, dram=False):
        self.t = t
        self.dram = dram
        self.w = {}
        self.r = {}
        self.sem = None

    def __getitem__(self, k):
        return self.t[k]


class Alias:
    def __init__(self, parent, ap):
        self.p = parent
        self.ap_ = ap
        self.dram = False

    def __getitem__(self, k):
        return self.ap_[k]

    w = property(lambda s: s.p.w, lambda s, v: setattr(s.p, "w", v))
    r = property(lambda s: s.p.r, lambda s, v: setattr(s.p, "r", v))
    sem = property(lambda s: s.p.sem, lambda s, v: setattr(s.p, "sem", v))


class KB:
    def __init__(self, nc, stack):
        self.nc = nc
        self.stack = stack
        self.eng = dict(pe=nc.tensor, act=nc.scalar, dve=nc.vector, pool=nc.gpsimd, sp=nc.sync)
        self.esem = {e: Sem(stack.enter_context(nc.semaphore("se_" + e))) for e in self.eng}
        self.seen = {e: {} for e in self.eng}
        self.dsems = [Sem(stack.enter_context(nc.semaphore("sd%d" % i))) for i in range(NDS)]
        self.free = list(self.dsems)
        self.bar = Sem(stack.enter_context(nc.semaphore("sbar")))
        self.uid = 0

    def _wait(self, e, toks, skip=None):
        for s, val in list(toks.items()):
            if s is skip:
                continue
            if self.seen[e].get(s, 0) < val:
                self.eng[e].wait_ge(s.h, val)
                self.seen[e][s] = val

    def op(self, e, fn, reads=(), writes=(), sreads=(), fence=False):
        own = self.esem[e]
        for b in reads:
            self._wait(e, b.w, own if e == "pe" else None)
        for b in sreads:
            self._wait(e, b.w)
        for b in writes:
            self._wait(e, b.w, own)
            self._wait(e, b.r, own)
        ins = fn(self.eng[e])
        own.cnt += 1
        ins.then_inc(own.h, 1)
        for b in list(reads) + list(sreads):
            if b not in writes:
                b.r[own] = own.cnt
        for b in writes:
            b.w = {own: own.cnt}
            b.r = {}
        if fence:
            self._wait(e, {own: own.cnt})
        return ins

    def dma(self, q, out, in_, sb, reads=(), writes=()):
        if sb.sem is None:
            sb.sem = self.free.pop()
        s = sb.sem
        for b in reads:
            if not b.dram:
                self._wait(q, b.w)
        for b in writes:
            if not b.dram:
                self._wait(q, b.w, s)
                self._wait(q, b.r)
        ins = self.eng[q].dma_start(out=out, in_=in_)
        s.cnt += 16
        ins.then_inc(s.h, 16)
        for b in reads:
            if not b.dram:
                b.r[s] = s.cnt
        for b in writes:
            if b.dram:
                pass
            else:
                b.w = {s: s.cnt}
                b.r = {}
        return ins

    def release(self, bufs):
        for b in bufs:
            if b.sem is not None:
                self.free.append(b.sem)
                b.sem = None

    def barrier(self):
        for e, s in self.esem.items():
            if e != "sp" and s.cnt:
                self._wait("sp", {s: s.cnt})
        for s in self.dsems:
            if s.cnt:
                self._wait("sp", {s: s.cnt})
        self.nc.sync.sem_inc(self.bar.h, 1)
        self.bar.cnt += 1
        for e in self.eng:
            if e != "sp":
                self._wait(e, {self.bar: self.bar.cnt})


class Phase:
    def __init__(self, kb, name):
        self.kb = kb
        self.name = name
        self.stack = ExitStack()
        self.bufs = []

    def sb(self, name, shape, dt):
        self.kb.uid += 1
        t = self.stack.enter_context(self.kb.nc.sbuf_tensor("%s_%s_%d" % (self.name, name, self.kb.uid), list(shape), dt))
        b = Buf(t)
        self.bufs.append(b)
        return b

    def ps(self, name, shape, dt):
        self.kb.uid += 1
        t = self.stack.enter_context(self.kb.nc.psum_tensor("%s_%s_%d" % (self.name, name, self.kb.uid), list(shape), dt))
        b = Buf(t)
        self.bufs.append(b)
        return b

    def __enter__(self):
        return self

    def __exit__(self, *a):
        if a[0] is None:
            self.kb.barrier()
            self.kb.release(self.bufs)
        self.stack.close()
        return False


def bc(ap, shape):
    return ap.broadcast_to(list(shape))


class _Stop(Exception):
    pass


def build(L, NB, depth=2, dbg=(), stop=None):
    T = NB * L
    NT = T // 128
    NS = T // 512
    TPS = L // 128
    nc = bass.Bass("TRN2", target_bir_lowering=False)

    def din(name, shape, dt=F32):
        return nc.dram_tensor(name, list(shape), dt, kind="ExternalInput").ap()

    def dscr(name, shape, dt):
        kind = "ExternalOutput" if name in dbg else "Internal"
        return nc.dram_tensor(name, list(shape), dt, kind=kind).ap()

    x_in = din("x", [T, D])
    w_in = din("w_in", [2, D, DP])
    w_out = din("w_out", [2, 2 * D, D])
    ffn_wg = din("ffn_w_gate", [1, D, DFF])
    ffn_wu = din("ffn_w_up", [1, D, DFF])
    ffn_wd = din("ffn_w_down", [1, DFF, D])
    moe_wg = din("moe_w_gate", [1, NE, D, DFF])
    moe_wu = din("moe_w_up", [1, NE, D, DFF])
    moe_wd = din("moe_w_down", [1, NE, DFF, D])
    g_mix = din("g_mix", [2, 128, 8])
    g_ffn = din("g_ffn", [2, 128, 8])
    g_out = din("g_out", [2, 128, 16])
    cw = din("cw", [2, 128, 16, 4])
    cb = din("cb", [2, 128, 16])
    hp = din("hp", [2, 128, 64])
    nfb = din("nfb", [2, 16, 1])
    rw = din("rw", [128, 8, 8])
    g_fin = din("g_fin", [128, D])
    c_id_b = din("c_id_b", [128, 128], BF16)
    c_id_f = din("c_id_f", [128, 128])
    c_tri = din("c_tri", [128, 128])
    c_ones = din("c_ones", [128, 128])
    c_mb4 = din("c_mb4", [128, 512])
    c_tri01 = din("c_tri01", [128, 128], BF16)
    out_d = nc.dram_tensor("out", [T, D], F32, kind="ExternalOutput").ap()

    xa = dscr("xa", [T, D], F32)
    xb = dscr("xb", [T, D], F32)
    hT = dscr("hT", [D, T], BF16)
    zs = dscr("zs", [T, D], BF16)
    xbcT = dscr("xbcT", [2 * D, T], BF16)
    qa = dscr("qa", [NB, NH, 70, L], BF16)
    ka = dscr("ka", [NB, NH, 70, L], BF16)
    va = dscr("va", [T, NH, 65], BF16)
    oT = dscr("oT", [D, T], F32)
    rsum = dscr("rsum", [NH, T], F32)
    yT = dscr("yT", [D, T], BF16)
    dts = dscr("dts", [T, NH], F32)
    cmb = dscr("cmb", [T, NE], F32)

    stack = ExitStack()
    kb = KB(nc, stack)
    B_xa, B_xb, B_hT, B_zs, B_xbcT, B_qa, B_ka, B_va, B_oT, B_rs, B_yT, B_dts, B_cmb, B_out = [
        Buf(None, dram=True) for _ in range(14)]
    B_in = Buf(None, dram=True)

    def sbp(name, shape, dt):
        return Buf(stack.enter_context(nc.sbuf_tensor(name, list(shape), dt)))

    idb = sbp("idb", [128, 128], BF16)
    idf = sbp("idf", [128, 128], F32)
    tri = sbp("tri", [128, 128], F32)
    ones = sbp("ones", [128, 128], F32)
    mb4 = sbp("mb4", [128, 512], F32)
    tri01 = sbp("tri01", [128, 128], BF16)
    onesb = sbp("onesb", [128, 128], BF16)
    for b, src in ((idb, c_id_b), (idf, c_id_f), (tri, c_tri), (ones, c_ones), (mb4, c_mb4), (tri01, c_tri01)):
        kb.dma("sp", b[:], src, b, reads=[B_in], writes=[b])
    kb.op("dve", lambda e: e.tensor_copy(out=onesb[:], in_=ones[:]), reads=[ones], writes=[onesb])
    kb.barrier()

    def load_w(ph, dst, kcs, cols, src_fn, gain, stg, col0=0):
        i = 0
        CH = stg[0].t.shape[1]
        for kc in range(kcs):
            for c0 in range(0, cols, CH):
                cn = min(CH, cols - c0)
                s = stg[i % len(stg)]
                q = "sp" if i % 2 == 0 else "act"
                kb.dma(q, s[:, 0:cn], src_fn(kc)[:, c0:c0 + cn], s, reads=[B_in], writes=[s])
                e = ("dve", "pool")[i % 2]
                if gain is None:
                    kb.op(e, lambda en, s=s, kc=kc, c0=c0, cn=cn: en.tensor_copy(
                        out=dst[:, kc, col0 + c0:col0 + c0 + cn], in_=s[:, 0:cn]), reads=[s], writes=[dst])
                else:
                    kb.op(e, lambda en, s=s, kc=kc, c0=c0, cn=cn: en.tensor_scalar(
                        out=dst[:, kc, col0 + c0:col0 + c0 + cn], in0=s[:, 0:cn], scalar1=gain[:, kc:kc + 1],
                        scalar2=None, op0=ALU.mult), reads=[s], writes=[dst], sreads=[gain])
                i += 1

    def w_chunks(dst, kcs, cols, src_fn, gain, stg, eng="pool", q="sp"):
        out = []
        st = [0]
        CH = stg[0].t.shape[1]
        for kc in range(kcs):
            for c0 in range(0, cols, CH):
                cn = min(CH, cols - c0)

                def f(kc=kc, c0=c0, cn=cn):
                    s_ = stg[st[0] % len(stg)]
                    st[0] += 1
                    kb.dma(q, s_[:, 0:cn], src_fn(kc)[:, c0:c0 + cn], s_, reads=[B_in], writes=[s_])
                    if gain is None:
                        kb.op(eng, lambda en: en.tensor_copy(out=dst[:, kc, c0:c0 + cn], in_=s_[:, 0:cn]), reads=[s_], writes=[dst])
                    else:
                        kb.op(eng, lambda en: en.tensor_scalar(out=dst[:, kc, c0:c0 + cn], in0=s_[:, 0:cn], scalar1=gain[:, kc:kc + 1],
                                                               scalar2=None, op0=ALU.mult), reads=[s_], writes=[dst], sreads=[gain])
                out.append(f)
        return out

    def outer_sb(st_, name, shape, dt):
        kb.uid += 1
        return Buf(st_.enter_context(nc.sbuf_tensor("%s_%d" % (name, kb.uid), list(shape), dt)))

    def rstd_gen(ph_tmp, src, ssq, rs, junk, n=D):
        kb.op("act", lambda e: e.activation(out=junk[:, 0:n], in_=src, func=AF.Square, accum_out=ssq[:, 0:1]),
              reads=[ph_tmp], writes=[junk, ssq])
        yield
        kb.op("act", lambda e: e.activation(out=rs[:, 0:1], in_=ssq[:, 0:1], func=AF.Sqrt, scale=1.0 / n, bias=EPS),
              reads=[ssq], writes=[rs])
        yield
        kb.op("dve", lambda e: e.reciprocal(out=rs[:, 0:1], in_=rs[:, 0:1]), reads=[rs], writes=[rs])
        yield

    def rstd_of(*a, **k):
        for _ in rstd_gen(*a, **k):
            pass

    class NormT:
        def __init__(self, ph, nhs=2, pT=None, hs=None):
            self.ph = ph
            self.nhs = nhs if hs is None else len(hs)
            self.ssq = [ph.sb("n_ssq%d" % i, [128, 1], F32) for i in range(2)]
            self.rs = [ph.sb("n_rs%d" % i, [128, 1], F32) for i in range(2)]
            self.junk = ph.sb("n_junk", [128, D], BF16)
            self.hb = [ph.sb("n_hb%d" % i, [128, D], BF16) for i in range(2)]
            self.pT = ph.ps("n_pT", [128, D], BF16) if pT is None else pT
            self.hs = [ph.sb("n_hs%d" % i, [128, 8, 512], BF16) for i in range(nhs)] if hs is None else hs
            self.n = 0

        def emit(self, xt, tile):
            for _ in self.emit_gen(xt, tile):
                pass
            return self.last_rs

        def emit_gen(self, xt, tile):
            i = self.n % 2
            self.n += 1
            ssq, rs, hb = self.ssq[i], self.rs[i], self.hb[i]
            self.last_rs = rs
            yield from rstd_gen(xt, xt[:, :], ssq, rs, self.junk)
            kb.op("dve", lambda e: e.tensor_scalar(out=hb[:, :], in0=xt[:, :], scalar1=rs[:, 0:1], scalar2=None,
                                                   op0=ALU.mult), reads=[xt], writes=[hb], sreads=[rs])
            yield
            pT = self.pT

            def tr(e):
                ins = None
                for kc in range(8):
                    ins = e.transpose(pT[:, kc * 128:(kc + 1) * 128], hb[:, kc * 128:(kc + 1) * 128], idb[:, :])
                return ins
            kb.op("pe", tr, reads=[hb, idb], writes=[pT])
            yield
            s, tt = tile // 4, tile % 4
            hs = self.hs[s % self.nhs]
            kb.op("act", lambda e: e.copy(out=hs[:, :, tt * 128:(tt + 1) * 128],
                                          in_=pT[:, :].rearrange("p (k t) -> p k t", k=8)), reads=[pT], writes=[hs])
            yield
            if tt == 3:
                kb.dma("pool", hT.rearrange("(k p) t -> p k t", p=128)[:, :, s * 512:(s + 1) * 512], hs[:, :, :], hs,
                       reads=[hs], writes=[B_hT])
            yield

    def chk(name):
        if stop == name:
            raise _Stop()

    try:
      for layer in range(depth):
          x_src, B_xsrc = (x_in, B_in) if layer == 0 else (xb, B_xb)

          wvin = w_in[layer].rearrange("(k p) n -> k p n", p=128)
          outer_in = ExitStack()
          preA1 = None
          if layer == 0:
              W_a1 = outer_sb(outer_in, "W_a1", [128, 8, 3072], BF16)
              gm_a1 = outer_sb(outer_in, "gm_a1", [128, 8], F32)
              kb.dma("sp", gm_a1[:, :], g_mix[layer], gm_a1, reads=[B_in], writes=[gm_a1])
              preA1 = (W_a1, gm_a1)
              with Phase(kb, "n1") as ph:
                  nt = NormT(ph)
                  xts = [ph.sb("xt%d" % i, [128, D], F32) for i in range(3)]
                  pstg = [ph.sb("pstg%d" % i, [128, 1536], F32) for i in range(2)]
                  pend = w_chunks(W_a1, 8, 3072, lambda kc: wvin[kc, :, 0:3072], gm_a1, pstg)
                  for t in range(NT):
                      xt = xts[t % 3]
                      kb.dma("sp", xt[:, :], x_src[t * 128:(t + 1) * 128, :], xt, reads=[B_xsrc], writes=[xt])
                      nt.emit(xt, t)
                      if pend and t % 2 == 1:
                          pend.pop(0)()
                  while pend:
                      pend.pop(0)()
          NW = DP - 3072
          W_a2 = outer_sb(outer_in, "W_a2", [128, 8, NW], BF16)
          gm_a2 = outer_sb(outer_in, "gm_a2", [128, 8], F32)
          kb.dma("sp", gm_a2[:, :], g_mix[layer], gm_a2, reads=[B_in], writes=[gm_a2])

          chk("n1")
          with Phase(kb, "a1") as ph:
              cwt = ph.sb("cwt", [128, 16, 4], F32)
              cbt = ph.sb("cbt", [128, 16], F32)
              kb.dma("sp", cwt[:, :, :], cw[layer], cwt, reads=[B_in], writes=[cwt])
              kb.dma("sp", cbt[:, :], cb[layer], cbt, reads=[B_in], writes=[cbt])
              if preA1 is not None:
                  W, gm = preA1
              else:
                  W = ph.sb("W", [128, 8, 3072], BF16)
                  gm = ph.sb("gm", [128, 8], F32)
                  stg = [ph.sb("stg%d" % i, [128, 1536], F32) for i in range(2)]
                  kb.dma("sp", gm[:, :], g_mix[layer], gm, reads=[B_in], writes=[gm])
                  load_w(ph, W, 8, 3072, lambda kc: wvin[kc, :, 0:3072], gm, stg)
              pstg = [ph.sb("pstg%d" % i, [128, 1552], F32) for i in range(2)]
              pend = w_chunks(W_a2, 8, NW, lambda kc: wvin[kc, :, 3072:DP], gm_a2, pstg)
              hts = [ph.sb("hts%d" % i, [128, 8, 512], BF16) for i in range(2)]
              pss = [ph.ps("ps%d" % i, [128, 512], F32) for i in range(6)]
              zst = [ph.sb("zst%d" % i, [128, D], BF16) for i in range(2)]
              xr = [ph.sb("xr%d" % i, [128, 515], F32) for i in range(4)]
              halos = [ph.sb("halo%d" % i, [128, 8, 3], F32) for i in range(2)]
              acc = [ph.sb("acc%d" % i, [128, 512], F32) for i in range(4)]
              xst = [ph.sb("xst%d" % i, [128, 16, 512], BF16) for i in range(2)]
              pi = 0
              for s in range(NS):
                  ht = hts[s % 2]
                  kb.dma("sp", ht[:, :, :], hT.rearrange("(k p) t -> p k t", p=128)[:, :, s * 512:(s + 1) * 512], ht,
                         reads=[B_hT], writes=[ht])
                  if (s * 512) % L == 0:
                      for hi_, he_ in enumerate(("dve", "pool")):
                          kb.op(he_, lambda e, hi_=hi_: e.memset(halos[hi_][:, :, :], 0.0), writes=[halos[hi_]], fence=True)
                  for tt in range(4):
                      zt = zst[tt % 2]
                      for half in range(2):
                          p = pss[pi % 6]
                          pi += 1

                          def mm(e, p=p, tt=tt, half=half):
                              ins = None
                              for kc in range(8):
                                  ins = e.matmul(p[:, :], lhsT=ht[:, kc, tt * 128:(tt + 1) * 128],
                                                 rhs=W[:, kc, half * 512:(half + 1) * 512], start=(kc == 0), stop=(kc == 7))
                              return ins
                          kb.op("pe", mm, reads=[ht, W], writes=[p])
                          kb.op("act", lambda e, p=p, half=half, zt=zt: e.copy(out=zt[:, half * 512:(half + 1) * 512], in_=p[:, :]),
                                reads=[p], writes=[zt])
                      tile = s * 4 + tt
                      kb.dma("pool", zs[tile * 128:(tile + 1) * 128, :], zt[:, :], zt, reads=[zt], writes=[B_zs])
                  xs_t = xst[s % 2]
                  for c in range(16):
                      p = pss[pi % 6]
                      pi += 1

                      def mm(e, p=p, c=c):
                          ins = None
                          for kc in range(8):
                              ins = e.matmul(p[:, :], lhsT=W[:, kc, 1024 + c * 128:1024 + (c + 1) * 128], rhs=ht[:, kc, :],
                                             start=(kc == 0), stop=(kc == 7))
                          return ins
                      kb.op("pe", mm, reads=[ht, W], writes=[p])
                      r = xr[c % 4]
                      a = acc[c % 4]
                      ce = "dve"
                      halo = halos[c % 2]
                      kb.op("act", lambda e, p=p, r=r: e.copy(out=r[:, 3:515], in_=p[:, :]), reads=[p], writes=[r])
                      kb.op(ce, lambda e, r=r, c=c, halo=halo: e.tensor_copy(out=r[:, 0:3], in_=halo[:, c // 2, :]), reads=[halo], writes=[r])
                      kb.op(ce, lambda e, r=r, a=a, c=c: e.tensor_scalar(out=a[:, :], in0=r[:, 0:512], scalar1=cwt[:, c, 0:1],
                                                                     scalar2=None, op0=ALU.mult),
                            reads=[r], writes=[a], sreads=[cwt])
                      for k in range(1, 4):
                          kb.op(ce, lambda e, r=r, a=a, c=c, k=k: e.scalar_tensor_tensor(
                              out=a[:, :], in0=r[:, k:k + 512], scalar=cwt[:, c, k:k + 1], in1=a[:, :], op0=ALU.mult, op1=ALU.add),
                              reads=[r, a], writes=[a], sreads=[cwt])
                      kb.op(ce, lambda e, r=r, c=c, halo=halo: e.tensor_copy(out=halo[:, c // 2, :], in_=r[:, 512:515]), reads=[r], writes=[halo])
                      kb.op("act", lambda e, a=a, c=c, xs_t=xs_t: e.activation(out=xs_t[:, c, :], in_=a[:, :], func=AF.Silu,
                                                                            bias=cbt[:, c:c + 1]),
                            reads=[a], writes=[xs_t], sreads=[cbt])
                  kb.dma("pool", xbcT.rearrange("(c p) t -> p c t", p=128)[:, :, s * 512:(s + 1) * 512], xs_t[:, :, :], xs_t,
                         reads=[xs_t], writes=[B_xbcT])
                  for _ in range(2):
                      if pend:
                          pend.pop(0)()
              while pend:
                  pend.pop(0)()

          chk("a1")
          with Phase(kb, "a2") as ph:
              W, gm = W_a2, gm_a2
              hpt = ph.sb("hpt", [128, 64], F32)
              nfbt = ph.sb("nfbt", [16, 1], F32)
              kb.dma("sp", hpt[:, :], hp[layer], hpt, reads=[B_in], writes=[hpt])
              kb.dma("sp", nfbt[:, :], nfb[layer], nfbt, reads=[B_in], writes=[nfbt])
              hts = [ph.sb("hts%d" % i, [128, 8, 512], BF16) for i in range(2)]
              pss = [ph.ps("ps%d" % i, [128, 512], F32) for i in range(6)]
              psd = ph.ps("psd", [128, 512], F32)
              psf = ph.ps("psf", [128, 512], F32)
              qst = [ph.sb("qst%d" % i, [128, 8, 512], BF16) for i in range(2)]
              kst = [ph.sb("kst%d" % i, [128, 8, 512], BF16) for i in range(2)]
              vst = [ph.sb("vst%d" % i, [128, NH, 65], BF16) for i in range(2)]
              dtt = [ph.sb("dtt%d" % i, [128, 16], F32) for i in range(2)]
              Gs = [ph.sb("G%d" % i, [16, 512], F32) for i in range(2)]
              spf = [ph.sb("spf%d" % i, [16, 512], F32) for i in range(2)]
              g8 = ph.sb("g8", [16, 512], F32)
              gh = [ph.sb("gh%d" % i, [16, 512], BF16) for i in range(6)]
              ngh = [ph.sb("ngh%d" % i, [16, 512], BF16) for i in range(6)]
              g32 = ph.sb("g32", [16, 512], F32)
              onl = ph.sb("onl", [16, 512], BF16)
              onf = ph.sb("onf", [16, 512], F32)
              kb.op("pool", lambda e: e.memset(onl[:, :], 1.0), writes=[onl], fence=True)
              kb.op("pool", lambda e: e.memset(onf[:, :], 1.0), writes=[onf], fence=True)
              kb.op("dve", lambda e: e.tensor_scalar(out=nfbt[:, :], in0=nfbt[:, :], scalar1=-1.0, scalar2=None, op0=ALU.mult),
                    reads=[nfbt], writes=[nfbt])
              for v_ in vst:
                  kb.op("pool", lambda e, v_=v_: e.memset(v_[:, :, :], 1.0), writes=[v_], fence=True)
              pi = 0
              for s in range(NS):
                  b_, t0 = (s * 512) // L, (s * 512) % L
                  ht = hts[s % 2]
                  kb.dma("sp", ht[:, :, :], hT.rearrange("(k p) t -> p k t", p=128)[:, :, s * 512:(s + 1) * 512], ht,
                         reads=[B_hT], writes=[ht])
                  for which, st_, dd, Bd, off in (("q", qst[s % 2], qa, B_qa, 16), ("k", kst[s % 2], ka, B_ka, 16 + 1024)):
                      for c in range(8):
                          p = pss[pi % 6]
                          pi += 1

                          def mm(e, p=p, c=c, off=off):
                              ins = None
                              for kc in range(8):
                                  ins = e.matmul(p[:, :], lhsT=W[:, kc, off + c * 128:off + (c + 1) * 128], rhs=ht[:, kc, :],
                                                 start=(kc == 0), stop=(kc == 7))
                              return ins
                          kb.op("pe", mm, reads=[ht, W], writes=[p])
                          ce = ("act", "dve")[c % 2]
                          if ce == "act":
                              kb.op("act", lambda e, p=p, c=c, st_=st_: e.copy(out=st_[:, c, :], in_=p[:, :]), reads=[p], writes=[st_])
                          else:
                              kb.op("dve", lambda e, p=p, c=c, st_=st_: e.tensor_copy(out=st_[:, c, :], in_=p[:, :]), reads=[p], writes=[st_])
                      for par in range(2):
                          dst = dd[b_, :, 0:64, t0:t0 + 512].rearrange("(c two) d t -> two d c t", two=2)[par]
                          kb.dma("pool", dst, st_[par * 64:(par + 1) * 64, :, :], st_, reads=[st_], writes=[Bd])
                  for tt in range(4):
                      tile = s * 4 + tt
                      vt = vst[tt % 2]
                      for half in range(2):
                          p = pss[pi % 6]
                          pi += 1

                          def mm(e, p=p, tt=tt, half=half):
                              ins = None
                              for kc in range(8):
                                  ins = e.matmul(p[:, :], lhsT=ht[:, kc, tt * 128:(tt + 1) * 128],
                                                 rhs=W[:, kc, 2064 + half * 512:2064 + (half + 1) * 512], start=(kc == 0), stop=(kc == 7))
                              return ins
                          kb.op("pe", mm, reads=[ht, W], writes=[p])
                          kb.op("act", lambda e, p=p, half=half, vt=vt: e.copy(
                              out=vt[:, half * 8:(half + 1) * 8, 0:64], in_=p[:, :].rearrange("p (h d) -> p h d", d=64)),
                              reads=[p], writes=[vt])
                      kb.dma("pool", va[tile * 128:(tile + 1) * 128, :, :], vt[:, :, :], vt, reads=[vt], writes=[B_va])

                      def mmd(e, tt=tt):
                          ins = None
                          for kc in range(8):
                              ins = e.matmul(psd[:, 0:16], lhsT=ht[:, kc, tt * 128:(tt + 1) * 128], rhs=W[:, kc, 0:16],
                                             start=(kc == 0), stop=(kc == 7))
                          return ins
                      kb.op("pe", mmd, reads=[ht, W], writes=[psd])
                      d_ = dtt[tt % 2]
                      kb.op("dve", lambda e, d_=d_: e.tensor_tensor(out=d_[:, :], in0=psd[:, 0:16], in1=hpt[:, 0:16], op=ALU.add),
                            reads=[psd, hpt], writes=[d_])
                      kb.op("act", lambda e, d_=d_: e.activation(out=d_[:, :], in_=d_[:, :], func=AF.Exp), reads=[d_], writes=[d_])
                      kb.op("act", lambda e, d_=d_: e.activation(out=d_[:, :], in_=d_[:, :], func=AF.Ln, bias=1.0), reads=[d_], writes=[d_])
                      kb.dma("pool", dts[tile * 128:(tile + 1) * 128, :], d_[:, :], d_, reads=[d_], writes=[B_dts])

                  def mmf(e):
                      ins = None
                      for kc in range(8):
                          ins = e.matmul(psf[0:16, :], lhsT=W[:, kc, 3088:3104], rhs=ht[:, kc, :], start=(kc == 0), stop=(kc == 7))
                      return ins
                  kb.op("pe", mmf, reads=[ht, W], writes=[psf])
                  sp_ = spf[s % 2]
                  kb.op("act", lambda e, sp_=sp_: e.activation(out=sp_[:, :], in_=psf[0:16, :], func=AF.Exp, scale=-1.0, bias=nfbt[:, 0:1]),
                        reads=[psf], writes=[sp_], sreads=[nfbt])
                  kb.op("act", lambda e, sp_=sp_: e.activation(out=sp_[:, :], in_=sp_[:, :], func=AF.Ln, bias=1.0), reads=[sp_], writes=[sp_])
                  G = Gs[s % 2]
                  Gp = Gs[(s + 1) % 2]
                  if t0 == 0:
                      kb.op("dve", lambda e, sp_=sp_, G=G: e.tensor_tensor_scan(out=G[:, :], data0=onf[:, :], data1=sp_[:, :],
                                                                               initial=0.0, op0=ALU.mult, op1=ALU.add),
                            reads=[sp_, onf], writes=[G])
                  else:
                      kb.op("dve", lambda e, sp_=sp_, G=G, Gp=Gp: e.tensor_tensor_scan(
                          out=G[:, :], data0=onf[:, :], data1=sp_[:, :], initial=Gp[:, 511:512],
                          op0=ALU.mult, op1=ALU.add), reads=[sp_, onf], writes=[G], sreads=[Gp])
                  kb.op("dve", lambda e, G=G: e.tensor_scalar(out=g8[:, :], in0=G[:, :], scalar1=8.0, scalar2=None, op0=ALU.mult),
                        reads=[G], writes=[g8])
                  for j in range(3):
                      gj, ngj = gh[(s % 2) * 3 + j], ngh[(s % 2) * 3 + j]
                      kb.op("dve", lambda e, gj=gj: e.tensor_copy(out=gj[:, :], in_=g8[:, :]), reads=[g8], writes=[gj])
                      kb.op("dve", lambda e, gj=gj: e.tensor_copy(out=g32[:, :], in_=gj[:, :]), reads=[gj], writes=[g32])
                      if j < 2:
                          kb.op("dve", lambda e: e.tensor_sub(out=g8[:, :], in0=g8[:, :], in1=g32[:, :]), reads=[g8, g32], writes=[g8])
                      kb.op("dve", lambda e, ngj=ngj: e.tensor_scalar(out=ngj[:, :], in0=g32[:, :], scalar1=-1.0, scalar2=None,
                                                                    op0=ALU.mult), reads=[g32], writes=[ngj])
                      kb.dma("pool", qa[b_, :, 64 + j, t0:t0 + 512], ngj[:, :], ngj, reads=[ngj], writes=[B_qa])
                      kb.dma("pool", qa[b_, :, 67 + j, t0:t0 + 512], onl[:, :], onl, reads=[onl], writes=[B_qa])
                      kb.dma("pool", ka[b_, :, 64 + j, t0:t0 + 512], onl[:, :], onl, reads=[onl], writes=[B_ka])
                      kb.dma("pool", ka[b_, :, 67 + j, t0:t0 + 512], gj[:, :], gj, reads=[gj], writes=[B_ka])

          outer_in.close()
          chk("a2")
          with Phase(kb, "ssd") as ph:
              hpt = ph.sb("hpt", [128, 64], F32)
              abc = ph.sb("abc", [128, 16], F32)
              kb.dma("sp", hpt[:, :], hp[layer], hpt, reads=[B_in], writes=[hpt])
              kb.op("act", lambda e: e.activation(out=abc[:, :], in_=hpt[:, 16:32], func=AF.Exp), reads=[hpt], writes=[abc])
              kb.op("dve", lambda e: e.tensor_scalar(out=abc[:, :], in0=abc[:, :], scalar1=-1.0, scalar2=None, op0=ALU.mult),
                    reads=[abc], writes=[abc])
              def ssd_stream(si, slist):
                  sfx = "_%d" % si
                  xbt = [ph.sb("xbt%d" % i + sfx, [128, 16, 512], BF16) for i in range(1)]
                  zt_ = [ph.sb("zt%d" % i + sfx, [128, D], BF16) for i in range(2)]
                  dtb = [ph.sb("dtb%d" % i + sfx, [128, 16], F32) for i in range(2)]
                  b0 = ph.ps("b0" + sfx, [128, 512], F32)
                  b1 = ph.ps("b1" + sfx, [128, 512], F32)
                  b2 = ph.ps("b2" + sfx, [128, 512], F32)
                  b3 = ph.ps("b3" + sfx, [128, 512], F32)
                  pT = Alias(b0, b0.t[:, :].bitcast(BF16))
                  pR = b0
                  pA = b1
                  pG = b1
                  pY = b1
                  pB = Alias(b2, b2.t[:, :].bitcast(BF16))
                  pYo = b2
                  pS = b3
                  Gsb = ph.sb("Gsb" + sfx, [128, 512], F32)
                  xc = ph.sb("xc" + sfx, [128, NH, 64], BF16)
                  xcd = ph.sb("xcd" + sfx, [128, NH, 64], BF16)
                  xsd = ph.sb("xsd" + sfx, [128, NH, 64], F32)
                  btok = ph.sb("btok" + sfx, [128, 4, 128], BF16)
                  adt = ph.sb("adt" + sfx, [128, 16], F32)
                  acs = ph.sb("acs" + sfx, [128, 32], F32)
                  nacs = ph.sb("nacs" + sfx, [128, 16], F32)
                  dout = ph.sb("dout" + sfx, [128, 16], F32)
                  ea = ph.sb("ea" + sfx, [128, 16], F32)
                  cdec = ph.sb("cdec" + sfx, [128, 16], F32)
                  rr = [ph.sb("rr%d" % i + sfx, [128, 4, 128], F32) for i in range(2)]
                  Es = [ph.sb("Es%d" % i + sfx, [128, 4, 128], F32) for i in range(2)]
                  Mt = [ph.sb("Mt%d" % i + sfx, [128, 4, 128], BF16) for i in range(2)]
                  prev = ph.sb("prev" + sfx, [128, D], F32)
                  prevb = ph.sb("prevb" + sfx, [128, D], BF16)
                  tmp = ph.sb("tmp" + sfx, [128, 512], F32)
                  ysb = ph.sb("ysb" + sfx, [128, D], F32)
                  gz = ph.sb("gz" + sfx, [128, D], F32)
                  ssq = ph.sb("ssq" + sfx, [128, 1], F32)
                  rs = ph.sb("rs" + sfx, [128, 1], F32)
                  junk = ph.sb("junk" + sfx, [128, D], BF16)
                  yb = ph.sb("yb" + sfx, [128, D], BF16)
                  yst = [ph.sb("yst%d" % i + sfx, [128, 8, 512], BF16) for i in range(1)]
                  for s in slist:
                      xt_ = xbt[0]
                      kb.dma("sp", xt_[:, :, :], xbcT.rearrange("(c p) t -> p c t", p=128)[:, :, s * 512:(s + 1) * 512], xt_,
                             reads=[B_xbcT], writes=[xt_])
                      yield
                      ys_ = yst[0]
                      for tt in range(4):
                          tile = s * 4 + tt
                          tk = slice(tt * 128, (tt + 1) * 128)
                          z_ = zt_[tile % 2]
                          d_ = dtb[tile % 2]
                          kb.dma("sp", z_[:, :], zs[tile * 128:(tile + 1) * 128, :], z_, reads=[B_zs], writes=[z_])
                          yield
                          kb.dma("sp", d_[:, :], dts[tile * 128:(tile + 1) * 128, :], d_, reads=[B_dts], writes=[d_])
                          yield
                          if (tile * 128) % L == 0:
                              kb.op("dve", lambda e: e.memset(prev[:, :], 0.0), writes=[prev], fence=True)
                              yield
                              kb.op("pool", lambda e: e.memset(prevb[:, :], 0.0), writes=[prevb], fence=True)
                              yield
                          def trx(e, tk=tk):
                              ins = None
                              for c in range(8):
                                  ins = e.transpose(pT[:, c * 128:(c + 1) * 128], xt_[:, c, tk], idb[:, :])
                              return ins
                          kb.op("pe", trx, reads=[xt_, idb], writes=[pT])
                          yield

                          def trb(e, tk=tk):
                              ins = None
                              for c in range(4):
                                  ins = e.transpose(pB[:, c * 128:(c + 1) * 128], xt_[:, 8 + c, tk], idb[:, :])
                              return ins
                          kb.op("pe", trb, reads=[xt_, idb], writes=[pB])
                          yield
                          kb.op("act", lambda e: e.copy(out=btok[:, :, :], in_=pB[:, 0:512].rearrange("p (g n) -> p g n", g=4)),
                                reads=[pB], writes=[btok])
                          yield
                          kb.op("dve", lambda e, d_=d_: e.tensor_tensor(out=adt[:, :], in0=d_[:, :], in1=abc[:, :], op=ALU.mult),
                                reads=[d_, abc], writes=[adt])
                          yield

                          def mma(e):
                              e.matmul(pA[:, 0:16], lhsT=tri[:, :], rhs=adt[:, :], start=True, stop=True)
                              return e.matmul(pA[:, 16:32], lhsT=ones[:, :], rhs=adt[:, :], start=True, stop=True)
                          kb.op("pe", mma, reads=[tri, ones, adt], writes=[pA])
                          yield
                          kb.op("dve", lambda e: e.tensor_copy(out=acs[:, :], in_=pA[:, 0:32]), reads=[pA], writes=[acs])
                          yield
                          kb.op("dve", lambda e: e.tensor_scalar(out=nacs[:, :], in0=acs[:, 0:16], scalar1=-1.0, scalar2=None, op0=ALU.mult),
                                reads=[acs], writes=[nacs])
                          yield
                          kb.op("dve", lambda e: e.tensor_sub(out=dout[:, :], in0=acs[:, 16:32], in1=acs[:, 0:16]), reads=[acs], writes=[dout])
                          yield
                          kb.op("act", lambda e: e.activation(out=dout[:, :], in_=dout[:, :], func=AF.Exp), reads=[dout], writes=[dout])
                          yield
                          kb.op("act", lambda e: e.activation(out=ea[:, :], in_=acs[:, 0:16], func=AF.Exp), reads=[acs], writes=[ea])
                          yield
                          kb.op("act", lambda e: e.activation(out=cdec[:, :], in_=acs[:, 16:32], func=AF.Exp), reads=[acs], writes=[cdec])
                          yield
                          pT3 = pT[:, :].rearrange("p (h d) -> p h d", d=64)
                          kb.op("dve", lambda e, d_=d_: e.tensor_tensor(out=xc[:, :, :], in0=pT3, in1=bc(d_[:, 0:16].unsqueeze(2), [128, 16, 64]),
                                                                       op=ALU.mult), reads=[pT, d_], writes=[xc])
                          yield
                          kb.op("pool", lambda e: e.tensor_tensor(out=xcd[:, :, :], in0=xc[:, :, :], in1=bc(dout[:, 0:16].unsqueeze(2), [128, 16, 64]),
                                                                  op=ALU.mult), reads=[xc, dout], writes=[xcd])
                          yield
                          kb.op("dve", lambda e: e.tensor_tensor(out=xsd[:, :, :], in0=pT3, in1=bc(hpt[:, 32:48].unsqueeze(2), [128, 16, 64]),
                                                                 op=ALU.mult), reads=[pT, hpt], writes=[xsd])
                          yield
                          def mmg(e, tk=tk):
                              ins = None
                              for g in range(4):
                                  ins = e.matmul(pG[:, g * 128:(g + 1) * 128], lhsT=xt_[:, 8 + g, tk], rhs=xt_[:, 12 + g, tk], start=True, stop=True)
                              return ins
                          kb.op("pe", mmg, reads=[xt_], writes=[pG])
                          yield
                          kb.op("act", lambda e: e.copy(out=Gsb[:, :], in_=pG[:, :]), reads=[pG], writes=[Gsb])
                          yield
                          kb.op("act", lambda e, z_=z_: e.activation(out=gz[:, :], in_=z_[:, :], func=AF.Silu), reads=[z_], writes=[gz])
                          yield
                          for half in range(2):
                              for gg in range(2):
                                  g = half * 2 + gg
                                  r_ = rr[g % 2]
                                  E_ = Es[g % 2]
                                  M_ = Mt[g % 2]
                                  kb.op("pool", lambda e, r_=r_, g=g: e.tensor_tensor(
                                      out=r_[:, :, :], in0=bc(tri[:, :].unsqueeze(1), [128, 4, 128]),
                                      in1=bc(adt[:, 4 * g:4 * g + 4].unsqueeze(2), [128, 4, 128]), op=ALU.mult),
                                      reads=[tri, adt], writes=[r_])
                                  yield

                                  def mmr(e, r_=r_):
                                      e.matmul(pR[:, :], lhsT=ones[:, :], rhs=r_[:, :, :].rearrange("p a b -> p (a b)"), start=True, stop=False)
                                      return e.matmul(pR[:, :], lhsT=idf[:, :], rhs=mb4[:, :], start=False, stop=True)
                                  kb.op("pe", mmr, reads=[ones, idf, mb4, r_], writes=[pR])
                                  yield
                                  for r in range(4):
                                      kb.op("act", lambda e, E_=E_, r=r, g=g: e.activation(
                                          out=E_[:, r, :], in_=pR[:, r * 128:(r + 1) * 128], func=AF.Exp, bias=nacs[:, 4 * g + r:4 * g + r + 1]),
                                          reads=[pR], writes=[E_], sreads=[nacs])
                                      yield
                                  kb.op("dve", lambda e, E_=E_, M_=M_, g=g: e.tensor_tensor(
                                      out=M_[:, :, :], in0=E_[:, :, :], in1=bc(Gsb[:, g * 128:(g + 1) * 128].unsqueeze(1), [128, 4, 128]), op=ALU.mult),
                                      reads=[E_, Gsb], writes=[M_])
                                  yield

                                  def mmy(e, M_=M_, g=g, gg=gg):
                                      ins = None
                                      for r in range(4):
                                          h = 4 * g + r
                                          ins = e.matmul(pY[:, (gg * 4 + r) * 64:(gg * 4 + r + 1) * 64], lhsT=M_[:, r, :], rhs=xc[:, h, :],
                                                         start=True, stop=True)
                                      return ins
                                  kb.op("pe", mmy, reads=[M_, xc], writes=[pY])
                                  yield

                              def mmo(e, half=half, tk=tk):
                                  ins = None
                                  for gg in range(2):
                                      g = half * 2 + gg
                                      ins = e.matmul(pYo[:, gg * 256:(gg + 1) * 256], lhsT=xt_[:, 12 + g, tk], rhs=prevb[:, g * 256:(g + 1) * 256],
                                                     start=True, stop=True)
                                  return ins
                              kb.op("pe", mmo, reads=[xt_, prevb], writes=[pYo])
                              yield

                              def mms(e, half=half):
                                  ins = None
                                  for gg in range(2):
                                      g = half * 2 + gg
                                      ins = e.matmul(pS[:, gg * 256:(gg + 1) * 256], lhsT=btok[:, g, :],
                                                     rhs=xcd[:, 4 * g:4 * g + 4, :].rearrange("p h d -> p (h d)"), start=True, stop=True)
                                  return ins
                              kb.op("pe", mms, reads=[btok, xcd], writes=[pS])
                              yield
                              hs_ = slice(half * 512, (half + 1) * 512)
                              h8 = slice(half * 8, (half + 1) * 8)
                              kb.op("dve", lambda e, h8=h8: e.tensor_tensor(
                                  out=tmp[:, :].rearrange("p (h d) -> p h d", d=64), in0=pYo[:, :].rearrange("p (h d) -> p h d", d=64),
                                  in1=bc(ea[:, h8].unsqueeze(2), [128, 8, 64]), op=ALU.mult), reads=[pYo, ea], writes=[tmp])
                              yield
                              kb.op("dve", lambda e: e.tensor_tensor(out=tmp[:, :], in0=pY[:, :], in1=tmp[:, :], op=ALU.add),
                                    reads=[pY, tmp], writes=[tmp])
                              yield
                              kb.op("pool", lambda e, hs_=hs_, h8=h8: e.tensor_tensor(
                                  out=ysb[:, hs_], in0=tmp[:, :], in1=xsd[:, h8, :].rearrange("p h d -> p (h d)"), op=ALU.add),
                                  reads=[tmp, xsd], writes=[ysb])
                              yield
                              kb.op("dve", lambda e, hs_=hs_, h8=h8: e.tensor_tensor(
                                  out=prev[:, hs_].rearrange("p (h d) -> p h d", d=64), in0=prev[:, hs_].rearrange("p (h d) -> p h d", d=64),
                                  in1=bc(cdec[:, h8].unsqueeze(2), [128, 8, 64]), op=ALU.mult), reads=[prev, cdec], writes=[prev])
                              yield
                              kb.op("dve", lambda e, hs_=hs_: e.tensor_tensor(out=prev[:, hs_], in0=prev[:, hs_], in1=pS[:, :], op=ALU.add),
                                    reads=[prev, pS], writes=[prev])
                              yield
                              kb.op("pool", lambda e, hs_=hs_: e.tensor_copy(out=prevb[:, hs_], in_=prev[:, hs_]), reads=[prev], writes=[prevb])
                              yield
                          kb.op("dve", lambda e: e.tensor_tensor(out=ysb[:, :], in0=ysb[:, :], in1=gz[:, :], op=ALU.mult),
                                reads=[ysb, gz], writes=[ysb])
                          yield
                          rstd_of(ysb, ysb[:, :], ssq, rs, junk)
                          yield
                          kb.op("dve", lambda e: e.tensor_scalar(out=yb[:, :], in0=ysb[:, :], scalar1=rs[:, 0:1], scalar2=None, op0=ALU.mult),
                                reads=[ysb], writes=[yb], sreads=[rs])
                          yield

                          def try_(e):
                              ins = None
                              for c in range(8):
                                  ins = e.transpose(pT[:, c * 128:(c + 1) * 128], yb[:, c * 128:(c + 1) * 128], idb[:, :])
                              return ins
                          kb.op("pe", try_, reads=[yb, idb], writes=[pT])
                          yield
                          kb.op("act", lambda e, tt=tt, ys_=ys_: e.copy(out=ys_[:, :, tt * 128:(tt + 1) * 128],
                                                                      in_=pT[:, :].rearrange("p (k t) -> p k t", k=8)), reads=[pT], writes=[ys_])
                          yield
                      kb.dma("pool", yT.rearrange("(k p) t -> p k t", p=128)[:, :, s * 512:(s + 1) * 512], ys_[:, :, :], ys_,
                             reads=[ys_], writes=[B_yT])
                      yield

              if NB == 2:
                  gens = [ssd_stream(si, list(range(si * (L // 512), (si + 1) * (L // 512)))) for si in range(2)]
              else:
                  gens = [ssd_stream(0, list(range(NS)))]
              if len(gens) == 2:
                  for _ in range(36):
                      next(gens[0], None)
              while gens:
                  for g_ in list(gens):
                      try:
                          next(g_)
                      except StopIteration:
                          gens.remove(g_)
          chk("ssd")
          outer_e = ExitStack()
          W_e = outer_sb(outer_e, "W_e", [128, 16, D], BF16)
          go_e = outer_sb(outer_e, "go_e", [128, 16], F32)
          kb.dma("sp", go_e[:, :], g_out[layer], go_e, reads=[B_in], writes=[go_e])
          wvout = w_out[layer].rearrange("(k p) n -> k p n", p=128)
          with Phase(kb, "fox") as ph:
              pstg = [ph.sb("pstg%d" % i, [128, 512], F32) for i in range(2)]
              pend_e = w_chunks(W_e, 16, D, lambda kc: wvout[kc], go_e, pstg)
              NQ = 3
              qt = [ph.sb("qt%d" % i, [70, L], BF16) for i in range(NQ)]
              kt = [ph.sb("kt%d" % i, [70, L], BF16) for i in range(NQ)]
              vt = [ph.sb("vt%d" % i, [128, TPS, 65], BF16) for i in range(NQ)]
              NR = 6
              LA = 3
              pS_ = [ph.ps("pS%d" % i, [128, 512], F32) for i in range(NR)]
              pO = [ph.ps("pO%d" % i, [65, 512], F32) for i in range(2)]
              Pt = [ph.sb("Pt%d" % i, [128, 512], BF16) for i in range(NR)]
              osb = [ph.sb("osb%d" % i, [65, 512], F32) for i in range(3)]
              heads = [(b_, h) for b_ in range(NB) for h in range(NH)]

              def load_head(n):
                  b_, h = heads[n]
                  q_, k_, v_ = qt[n % NQ], kt[n % NQ], vt[n % NQ]
                  kb.dma("sp", q_[:, :], qa[b_, h], q_, reads=[B_qa], writes=[q_])
                  kb.dma("sp", k_[:, :], ka[b_, h], k_, reads=[B_ka], writes=[k_])
                  kb.dma("sp", v_[:, :, :], va.rearrange("(b i p) h c -> b h p i c", b=NB, p=128)[b_, h], v_, reads=[B_va], writes=[v_])

              steps = []
              jn = 0
              for n, (b_, h) in enumerate(heads):
                  for j in range(L // 512):
                      nk = 4 * j + 4
                      for i in range(nk):
                          steps.append(dict(n=n, b=b_, h=h, j=j, i=i, nk=nk, jn=jn, first=(j == 0 and i == 0)))
                      jn += 1

              def front(t):
                  st = steps[t]
                  n, j, i = st["n"], st["j"], st["i"]
                  if st["first"]:
                      if n == 0:
                          load_head(0)
                      if n + 1 < len(heads):
                          load_head(n + 1)
                  q_, k_ = qt[n % NQ], kt[n % NQ]
                  dd = i - 4 * j
                  c0 = 128 * dd if dd > 0 else 0
                  S_, P_ = pS_[t % NR], Pt[t % NR]
                  kb.op("pe", lambda e: e.matmul(S_[:, c0:512], lhsT=k_[:, i * 128:(i + 1) * 128], rhs=q_[:, j * 512 + c0:(j + 1) * 512],
                                                 start=True, stop=True), reads=[k_, q_], writes=[S_])
                  kb.op("act", lambda e: e.activation(out=P_[:, c0:512], in_=S_[:, c0:512], func=AF.Exp, scale=0.125), reads=[S_], writes=[P_])
                  if dd >= 0:
                      me = ("dve", "pool")[dd % 2]
                      kb.op(me, lambda e: e.tensor_tensor(out=P_[:, c0:c0 + 128], in0=P_[:, c0:c0 + 128], in1=tri01[:, :], op=ALU.mult),
                            reads=[P_, tri01], writes=[P_])

              def back(t):
                  st = steps[t]
                  n, j, i, nk, b_, h = st["n"], st["j"], st["i"], st["nk"], st["b"], st["h"]
                  v_ = vt[n % NQ]
                  dd = i - 4 * j
                  c0 = 128 * dd if dd > 0 else 0
                  P_ = Pt[t % NR]
                  O = pO[st["jn"] % 2]
                  kb.op("pe", lambda e: e.matmul(O[:, c0:512], lhsT=v_[:, i, :], rhs=P_[:, c0:512], start=(i == 0), stop=(i == nk - 1),
                                                 skip_group_check=True), reads=[v_, P_], writes=[O])
                  if i == nk - 1:
                      o_ = osb[st["jn"] % 3]
                      kb.op("dve", lambda e: e.tensor_copy(out=o_[:, :], in_=O[:, :]), reads=[O], writes=[o_])
                      tcol = b_ * L + j * 512
                      kb.dma("pool", oT[h * 64:(h + 1) * 64, tcol:tcol + 512], o_[0:64, :], o_, reads=[o_], writes=[B_oT])
                      kb.dma("pool", rsum[h:h + 1, tcol:tcol + 512], o_[64:65, :], o_, reads=[o_], writes=[B_rs])
                      if pend_e:
                          pend_e.pop(0)()

              for t in range(len(steps) + LA):
                  if t < len(steps):
                      front(t)
                  if t - LA >= 0:
                      back(t - LA)
              while pend_e:
                  pend_e.pop(0)()
          chk("fox")
          moe = (layer % 2 == 1)
          with Phase(kb, "e") as ph:
              W, go = W_e, go_e
              if moe:
                  rwt = ph.sb("rwt", [128, 8, 8], F32)
                  gf = ph.sb("gf", [128, 8], F32)
                  kb.dma("sp", rwt[:, :, :], rw, rwt, reads=[B_in], writes=[rwt])
                  kb.dma("sp", gf[:, :], g_ffn[layer], gf, reads=[B_in], writes=[gf])
                  kb.op("dve", lambda e: e.tensor_tensor(out=rwt[:, :, :], in0=rwt[:, :, :], in1=bc(gf[:, :].unsqueeze(2), [128, 8, 8]),
                                                         op=ALU.mult), reads=[rwt, gf], writes=[rwt])

              def e_stream(si, slist):
                  sfx = "_%d" % si
                  bA = ph.ps("bA" + sfx, [128, 512], F32)
                  bB = ph.ps("bB" + sfx, [128, 512], F32)
                  po = [ph.ps("po%d" % i + sfx, [128, 512], F32) for i in range(2)]
                  osq = ph.sb("osq" + sfx, [128, 8, 512], BF16)
                  nt = NormT(ph, pT=Alias(bA, bA.t[:, :].bitcast(BF16)), hs=[osq])
                  ot = [ph.sb("ot" + sfx, [128, 8, 512], F32)]
                  rt = [ph.sb("rt" + sfx, [128, 8, 512], F32)]
                  ysT = [ph.sb("ysT" + sfx, [128, 8, 512], BF16)]
                  yfT = ph.sb("yfT" + sfx, [128, 8, 512], BF16)
                  rstd = ph.sb("rstd" + sfx, [128, 512], F32)
                  pss = bB
                  xts = [ph.sb("xt%d" % i + sfx, [128, D], F32) for i in range(2)]
                  if moe:
                      pTf = bB
                      pl = bA
                      xTf = ph.sb("xTf" + sfx, [128, D], F32)
                      lg = ph.sb("lg" + sfx, [128, 8], F32)
                      lg2 = ph.sb("lg2" + sfx, [128, 8], F32)
                      m1 = ph.sb("m1" + sfx, [128, 4], F32)
                      mk1 = ph.sb("mk1" + sfx, [128, 8], F32)
                      mk2 = ph.sb("mk2" + sfx, [128, 8], F32)
                      cm = [ph.sb("cm%d" % i + sfx, [128, 8], F32) for i in range(2)]
                  pi = 0
                  xi = 0
                  for s in slist:
                      o_, r_, ys_ = ot[0], rt[0], ysT[0]
                      cs = slice(s * 512, (s + 1) * 512)
                      kb.dma("sp", o_[:, :, :], oT.rearrange("(k p) t -> p k t", p=128)[:, :, cs], o_, reads=[B_oT], writes=[o_])
                      yield
                      for par in range(2):
                          src = rsum.rearrange("(k two) t -> two k t", two=2)[par:par + 1, :, cs]
                          kb.dma("act", r_[par * 64:(par + 1) * 64, :, :], bc(src, [64, 8, 512]), r_, reads=[B_rs], writes=[r_])
                          yield
                      kb.dma("sp", ys_[:, :, :], yT.rearrange("(k p) t -> p k t", p=128)[:, :, cs], ys_, reads=[B_yT], writes=[ys_])
                      yield
                      kb.op("dve", lambda e, r_=r_: e.reciprocal(out=r_[:, :, :], in_=r_[:, :, :]), reads=[r_], writes=[r_])
                      yield
                      kb.op("dve", lambda e, r_=r_, o_=o_: e.tensor_tensor(out=o_[:, :, :], in0=o_[:, :, :], in1=r_[:, :, :], op=ALU.mult),
                            reads=[o_, r_], writes=[o_])
                      yield
                      kb.op("act", lambda e, o_=o_: e.activation(out=osq[:, :, :], in_=o_[:, :, :], func=AF.Square), reads=[o_], writes=[osq])
                      yield

                      def mss(e):
                          ins = None
                          for kc in range(8):
                              ins = e.matmul(pss[:, :], lhsT=onesb[:, :], rhs=osq[:, kc, :], start=(kc == 0), stop=(kc == 7))
                          return ins
                      kb.op("pe", mss, reads=[onesb, osq], writes=[pss])
                      yield
                      kb.op("act", lambda e: e.activation(out=rstd[:, :], in_=pss[:, :], func=AF.Sqrt, scale=1.0 / D, bias=EPS), reads=[pss], writes=[rstd])
                      yield
                      kb.op("dve", lambda e: e.reciprocal(out=rstd[:, :], in_=rstd[:, :]), reads=[rstd], writes=[rstd])
                      yield
                      kb.op("dve", lambda e, o_=o_: e.tensor_tensor(out=yfT[:, :, :], in0=o_[:, :, :], in1=bc(rstd[:, :].unsqueeze(1), [128, 8, 512]),
                                                                   op=ALU.mult), reads=[o_, rstd], writes=[yfT])
                      yield
                      for tt in range(4):
                          tile = s * 4 + tt
                          xt = xts[xi % 2]
                          xi += 1
                          kb.dma("sp", xt[:, :], x_src[tile * 128:(tile + 1) * 128, :], xt, reads=[B_xsrc], writes=[xt])
                          yield
                          for half in range(2):
                              p = po[pi % 2]
                              pi += 1

                              def mm(e, p=p, tt=tt, half=half, ys_=ys_):
                                  ins = None
                                  for kc in range(16):
                                      src = ys_ if kc < 8 else yfT
                                      ins = e.matmul(p[:, :], lhsT=src[:, kc % 8, tt * 128:(tt + 1) * 128], rhs=W[:, kc, half * 512:(half + 1) * 512],
                                                     start=(kc == 0), stop=(kc == 15))
                                  return ins
                              kb.op("pe", mm, reads=[ys_, yfT, W], writes=[p])
                              yield
                              kb.op("dve", lambda e, p=p, half=half, xt=xt: e.tensor_tensor(
                                  out=xt[:, half * 512:(half + 1) * 512], in0=p[:, :], in1=xt[:, half * 512:(half + 1) * 512], op=ALU.add),
                                  reads=[p, xt], writes=[xt])
                              yield
                          kb.dma("pool", xa[tile * 128:(tile + 1) * 128, :], xt[:, :], xt, reads=[xt], writes=[B_xa])
                          yield
                          yield from nt.emit_gen(xt, tile)
                          rs = nt.last_rs
                          if moe:
                              for hh in range(2):
                                  def trf(e, hh=hh, xt=xt):
                                      ins = None
                                      for c in range(4):
                                          kc = hh * 4 + c
                                          ins = e.transpose(pTf[:, c * 128:(c + 1) * 128], xt[:, kc * 128:(kc + 1) * 128], idf[:, :])
                                      return ins
                                  kb.op("pe", trf, reads=[xt, idf], writes=[pTf])
                                  yield
                                  kb.op("act", lambda e, hh=hh: e.copy(out=xTf[:, hh * 512:(hh + 1) * 512], in_=pTf[:, :]), reads=[pTf], writes=[xTf])
                                  yield

                              def mml(e):
                                  ins = None
                                  for kc in range(8):
                                      ins = e.matmul(pl[:, 0:8], lhsT=xTf[:, kc * 128:(kc + 1) * 128], rhs=rwt[:, kc, :], start=(kc == 0), stop=(kc == 7))
                                  return ins
                              kb.op("pe", mml, reads=[xTf, rwt], writes=[pl])
                              yield
                              c_ = cm[tile % 2]
                              kb.op("dve", lambda e, rs=rs: e.tensor_scalar(out=lg[:, :], in0=pl[:, 0:8], scalar1=rs[:, 0:1], scalar2=None, op0=ALU.mult),
                                    reads=[pl], writes=[lg], sreads=[rs])
                              yield
                              kb.op("dve", lambda e: e.reduce_max(out=m1[:, 0:1], in_=lg[:, :], axis=AX.X), reads=[lg], writes=[m1])
                              yield
                              kb.op("dve", lambda e: e.tensor_scalar(out=mk1[:, :], in0=lg[:, :], scalar1=m1[:, 0:1], scalar2=None, op0=ALU.is_ge),
                                    reads=[lg], writes=[mk1], sreads=[m1])
                              yield
                              kb.op("dve", lambda e: e.scalar_tensor_tensor(out=lg2[:, :], in0=mk1[:, :], scalar=NEG, in1=lg[:, :], op0=ALU.mult, op1=ALU.add),
                                    reads=[mk1, lg], writes=[lg2])
                              yield
                              kb.op("dve", lambda e: e.reduce_max(out=m1[:, 1:2], in_=lg2[:, :], axis=AX.X), reads=[lg2], writes=[m1])
                              yield
                              kb.op("dve", lambda e: e.tensor_scalar(out=mk2[:, :], in0=lg2[:, :], scalar1=m1[:, 1:2], scalar2=None, op0=ALU.is_ge),
                                    reads=[lg2], writes=[mk2], sreads=[m1])
                              yield
                              kb.op("dve", lambda e: e.tensor_sub(out=m1[:, 2:3], in0=m1[:, 1:2], in1=m1[:, 0:1]), reads=[m1], writes=[m1])
                              yield
                              kb.op("act", lambda e: e.activation(out=m1[:, 2:3], in_=m1[:, 2:3], func=AF.Sigmoid), reads=[m1], writes=[m1])
                              yield
                              kb.op("dve", lambda e: e.tensor_scalar(out=m1[:, 3:4], in0=m1[:, 2:3], scalar1=-1.0, scalar2=1.0, op0=ALU.mult, op1=ALU.add),
                                    reads=[m1], writes=[m1])
                              yield
                              kb.op("dve", lambda e, c_=c_: e.tensor_scalar(out=c_[:, :], in0=mk1[:, :], scalar1=m1[:, 3:4], scalar2=None, op0=ALU.mult),
                                    reads=[mk1], writes=[c_], sreads=[m1])
                              yield
                              kb.op("dve", lambda e, c_=c_: e.scalar_tensor_tensor(out=c_[:, :], in0=mk2[:, :], scalar=m1[:, 2:3], in1=c_[:, :],
                                                                                  op0=ALU.mult, op1=ALU.add), reads=[mk2, c_], writes=[c_], sreads=[m1])
                              yield
                              kb.dma("pool", cmb[tile * 128:(tile + 1) * 128, :], c_[:, :], c_, reads=[c_], writes=[B_cmb])
                              yield

              if NB == 2:
                  gens = [e_stream(si, list(range(si * (L // 512), (si + 1) * (L // 512)))) for si in range(2)]
              else:
                  gens = [e_stream(0, list(range(NS)))]
              if len(gens) == 2:
                  for _ in range(25 if moe else 17):
                      next(gens[0], None)
              while gens:
                  for g_ in list(gens):
                      try:
                          next(g_)
                      except StopIteration:
                          gens.remove(g_)
          outer_e.close()
          chk("e")
          npass = NE if moe else 1
          last_layer = (layer == depth - 1)
          HF = DFF // 2
          NFH = NFC // 2
          with Phase(kb, "f") as ph:
              WG = [ph.sb("WG%d" % i, [128, 8, HF], BF16) for i in range(2)]
              WU = [ph.sb("WU%d" % i, [128, 8, HF], BF16) for i in range(2)]
              WD = [ph.sb("WD%d" % i, [128, NFH, D], BF16) for i in range(2)]
              gf = ph.sb("gf", [128, 8], F32)
              CHW = 352
              stg = [ph.sb("stg%d" % i, [128, CHW], F32) for i in range(4)]
              kb.dma("sp", gf[:, :], g_ffn[layer], gf, reads=[B_in], writes=[gf])
              hts = [ph.sb("hts%d" % i, [128, 8, 512], BF16) for i in range(2)]
              aT = ph.sb("aT", [128, NFH, 512], BF16)
              sg = [ph.sb("sg%d" % i, [128, 512], F32) for i in range(2)]
              pg = [ph.ps("pg%d" % i, [128, 512], F32) for i in range(2)]
              pu = [ph.ps("pu%d" % i, [128, 512], F32) for i in range(2)]
              po = [ph.ps("po%d" % i, [128, 512], F32) for i in range(3)]
              xts = [ph.sb("xt%d" % i, [128, D], F32) for i in range(2)]
              cmt = [ph.sb("cmt%d" % i, [128, 8], F32) for i in range(2)]
              if last_layer:
                  gfin = ph.sb("gfin", [128, D], F32)
                  kb.dma("sp", gfin[:, :], g_fin, gfin, reads=[B_in], writes=[gfin])
                  ssq = ph.sb("ssq", [128, 1], F32)
                  rsf = ph.sb("rsf", [128, 1], F32)
                  junk = ph.sb("junk", [128, D], BF16)
              else:
                  nt = NormT(ph, nhs=1)
              units = [(e_, hf) for e_ in range(npass) for hf in range(2)]
              lc = [0]

              def wchunks(u, engs=("pool",)):
                  e_, hf = units[u]
                  if moe:
                      wgv = moe_wg[0, e_].rearrange("(k p) n -> k p n", p=128)
                      wuv = moe_wu[0, e_].rearrange("(k p) n -> k p n", p=128)
                      wdv = moe_wd[0, e_].rearrange("(k p) n -> k p n", p=128)
                  else:
                      wgv = ffn_wg[0].rearrange("(k p) n -> k p n", p=128)
                      wuv = ffn_wu[0].rearrange("(k p) n -> k p n", p=128)
                      wdv = ffn_wd[0].rearrange("(k p) n -> k p n", p=128)
                  out = []

                  def mk(dst, kc, c0, cn, src, gain):
                      def f():
                          s_ = stg[lc[0] % 4]
                          ce_ = engs[lc[0] % len(engs)]
                          lc[0] += 1
                          kb.dma("sp", s_[:, 0:cn], src, s_, reads=[B_in], writes=[s_])
                          if gain:
                              kb.op(ce_, lambda en: en.tensor_scalar(out=dst[:, kc, c0:c0 + cn], in0=s_[:, 0:cn], scalar1=gf[:, kc:kc + 1],
                                                                        scalar2=None, op0=ALU.mult), reads=[s_], writes=[dst], sreads=[gf])
                          else:
                              kb.op(ce_, lambda en: en.tensor_copy(out=dst[:, kc, c0:c0 + cn], in_=s_[:, 0:cn]), reads=[s_], writes=[dst])
                      return f
                  for dst, wv_ in ((WG[u % 2], wgv), (WU[u % 2], wuv)):
                      for kc in range(8):
                          for c0 in range(0, HF, CHW):
                              out.append(mk(dst, kc, c0, CHW, wv_[kc][:, hf * HF + c0:hf * HF + c0 + CHW], True))
                  for fc in range(NFH):
                      for c0 in range(0, D, CHW):
                          cn = min(CHW, D - c0)
                          out.append(mk(WD[u % 2], fc, c0, cn, wdv[hf * NFH + fc][:, c0:c0 + cn], False))
                  return out

              for f_ in wchunks(0, engs=("dve", "pool", "dve")):
                  f_()
              tile_tok = {}
              pi = 0
              for u, (e_, hf) in enumerate(units):
                  Wg, Wu, Wd = WG[u % 2], WU[u % 2], WD[u % 2]
                  pend = wchunks(u + 1) if u + 1 < len(units) else []
                  final_unit = (u == len(units) - 1)
                  acc_src = xa if u == 0 else xb
                  gi = [0]
                  gu_of = {}

                  def load_ht(s):
                      ht = hts[s % 2]
                      kb.dma("sp", ht[:, :, :], hT.rearrange("(k p) t -> p k t", p=128)[:, :, s * 512:(s + 1) * 512], ht,
                             reads=[B_hT], writes=[ht])

                  def pe_part(s, fc):
                      ht = hts[s % 2]
                      k_ = gi[0] % 2
                      gi[0] += 1
                      gu_of[(s, fc)] = k_
                      g_, u_ = pg[k_], pu[k_]

                      def mmg(e):
                          ins = None
                          for kc in range(8):
                              ins = e.matmul(g_[:, :], lhsT=Wg[:, kc, fc * 128:(fc + 1) * 128], rhs=ht[:, kc, :], start=(kc == 0), stop=(kc == 7))
                          return ins

                      def mmu(e):
                          ins = None
                          for kc in range(8):
                              ins = e.matmul(u_[:, :], lhsT=Wu[:, kc, fc * 128:(fc + 1) * 128], rhs=ht[:, kc, :], start=(kc == 0), stop=(kc == 7))
                          return ins
                      kb.op("pe", mmg, reads=[Wg, ht], writes=[g_])
                      kb.op("pe", mmu, reads=[Wu, ht], writes=[u_])

                  def ew_part(s, fc):
                      k_ = gu_of.pop((s, fc))
                      g_, u_, s_ = pg[k_], pu[k_], sg[k_]
                      kb.op("act", lambda e: e.activation(out=s_[:, :], in_=g_[:, :], func=AF.Silu), reads=[g_], writes=[s_])
                      kb.op("dve", lambda e: e.tensor_tensor(out=aT[:, fc, :], in0=u_[:, :], in1=s_[:, :], op=ALU.mult),
                            reads=[u_, s_], writes=[aT])

                  FR = 2
                  load_ht(0)
                  for fc in range(FR):
                      pe_part(0, fc)
                  for s in range(NS):
                      for fc in range(NFH):
                          if fc >= FR:
                              pe_part(s, fc)
                          ew_part(s, fc)
                          if pend:
                              pend.pop(0)()
                      if s + 1 < NS:
                          load_ht(s + 1)
                          for fc in range(FR):
                              pe_part(s + 1, fc)
                      for tt in range(4):
                          tile = s * 4 + tt
                          xt = xts[tile % 2]
                          if tile in tile_tok:
                              kb._wait("sp", {tile_tok[tile][0]: tile_tok[tile][1]})
                          kb.dma("sp", xt[:, :], acc_src[tile * 128:(tile + 1) * 128, :], xt, reads=[B_in], writes=[xt])
                          if moe:
                              c_ = cmt[tile % 2]
                              kb.dma("sp", c_[:, :], cmb[tile * 128:(tile + 1) * 128, :], c_, reads=[B_cmb], writes=[c_])
                          for half in range(2):
                              p = po[pi % 3]
                              pi += 1

                              def mmd(e, p=p, tt=tt, half=half):
                                  ins = None
                                  for fc in range(NFH):
                                      ins = e.matmul(p[:, :], lhsT=aT[:, fc, tt * 128:(tt + 1) * 128], rhs=Wd[:, fc, half * 512:(half + 1) * 512],
                                                     start=(fc == 0), stop=(fc == NFH - 1))
                                  return ins
                              kb.op("pe", mmd, reads=[aT, Wd], writes=[p])
                              hs_ = slice(half * 512, (half + 1) * 512)
                              if moe:
                                  kb.op("dve", lambda e, p=p, xt=xt, hs_=hs_, c_=c_: e.scalar_tensor_tensor(
                                      out=xt[:, hs_], in0=p[:, :], scalar=c_[:, e_:e_ + 1], in1=xt[:, hs_], op0=ALU.mult, op1=ALU.add),
                                      reads=[p, xt], writes=[xt], sreads=[c_])
                              else:
                                  kb.op("dve", lambda e, p=p, xt=xt, hs_=hs_: e.tensor_tensor(out=xt[:, hs_], in0=p[:, :], in1=xt[:, hs_], op=ALU.add),
                                        reads=[p, xt], writes=[xt])
                          if final_unit and last_layer:
                              rstd_of(xt, xt[:, :], ssq, rsf, junk)
                              kb.op("dve", lambda e, xt=xt: e.scalar_tensor_tensor(out=xt[:, :], in0=xt[:, :], scalar=rsf[:, 0:1], in1=gfin[:, :],
                                                                                  op0=ALU.mult, op1=ALU.mult), reads=[xt, gfin], writes=[xt], sreads=[rsf])
                              kb.dma("pool", out_d[tile * 128:(tile + 1) * 128, :], xt[:, :], xt, reads=[xt], writes=[B_out])
                          else:
                              kb.dma("pool", xb[tile * 128:(tile + 1) * 128, :], xt[:, :], xt, reads=[xt], writes=[B_xb])
                              tile_tok[tile] = (xt.sem, xt.sem.cnt)
                              if final_unit:
                                  nt.emit(xt, tile)
                          if pend:
                              pend.pop(0)()
                  while pend:
                      pend.pop(0)()
    except _Stop:
        pass
    kb.barrier()
    stack.close()
    return nc


def _consts():
    i = np.arange(128)
    tri = (i[:, None] <= i[None, :]).astype(np.float32)
    mb = np.where(i[:, None] > i[None, :], NEG, 0.0).astype(np.float32)
    return dict(
        c_id_b=np.eye(128, dtype=np.float32).astype(ml_dtypes.bfloat16),
        c_id_f=np.eye(128, dtype=np.float32),
        c_tri=tri,
        c_ones=np.ones((128, 128), np.float32),
        c_mb4=np.ascontiguousarray(np.tile(mb, (1, 4))),
        c_tri01=tri.astype(ml_dtypes.bfloat16),
    )


def _layout_params(inp):
    f = np.float32
    pm = {}
    pm["g_mix"] = np.ascontiguousarray(inp["mix_norm"].reshape(2, 8, 128).transpose(0, 2, 1)).astype(f)
    pm["g_ffn"] = np.ascontiguousarray(inp["ffn_norm"].reshape(2, 8, 128).transpose(0, 2, 1)).astype(f)
    go = np.concatenate([inp["ssd_norm"], inp["fox_norm"]], axis=1)
    pm["g_out"] = np.ascontiguousarray(go.reshape(2, 16, 128).transpose(0, 2, 1)).astype(f)
    pm["cw"] = np.ascontiguousarray(inp["conv_w"].reshape(2, 4, 16, 128).transpose(0, 3, 2, 1)).astype(f)
    pm["cb"] = np.ascontiguousarray(inp["conv_b"].reshape(2, 16, 128).transpose(0, 2, 1)).astype(f)
    hp = np.concatenate([inp["dt_bias"], inp["a_log"], inp["d_skip"], inp["fox_f_bias"]], axis=1)
    pm["hp"] = np.ascontiguousarray(np.broadcast_to(hp[:, None, :], (2, 128, 64))).astype(f)
    pm["nfb"] = np.ascontiguousarray(inp["fox_f_bias"].reshape(2, 16, 1)).astype(f)
    pm["rw"] = np.ascontiguousarray(inp["router_w"][0].reshape(8, 128, 8).transpose(1, 0, 2)).astype(f)
    pm["g_fin"] = np.ascontiguousarray(np.broadcast_to(inp["final_norm"][None, :], (128, D))).astype(f)
    return pm


_CACHE = {}


def run(inp, L, NB, ncores=8, depth=2, dbg=(), stop=None):
    key = (L, NB, depth, tuple(dbg), stop)
    if key not in _CACHE:
        _CACHE[key] = build(L, NB, depth, dbg, stop)
    nc = _CACHE[key]
    x = np.ascontiguousarray(np.asarray(inp["x"], dtype=np.float32))
    T = NB * L
    xs = x.reshape(-1, T, D)
    shared = dict(_consts())
    shared.update(_layout_params({k: np.asarray(v) for k, v in inp.items()}))
    for k in ("w_in", "w_out", "ffn_w_gate", "ffn_w_up", "ffn_w_down", "moe_w_gate", "moe_w_up", "moe_w_down"):
        shared[k] = np.ascontiguousarray(np.asarray(inp[k], dtype=np.float32))
    in_maps = []
    for c in range(ncores):
        m = dict(shared)
        m["x"] = np.ascontiguousarray(xs[c])
        in_maps.append(m)
    res = run_bass_kernel_spmd(nc, in_maps, core_ids=list(range(ncores)))
    return res


def kernel(**inputs):
    x = np.asarray(inputs["x"])
    Bsz, L, _ = x.shape
    NB = Bsz // 8
    res = run(inputs, L, NB)
    out = np.stack([r["out"] for r in res.results], axis=0).reshape(Bsz, L, D)
    return out.astype(np.float32)
```

```python
import numpy as np
from contextlib import ExitStack
import ml_dtypes
import concourse.bass as bass
import concourse.mybir as mybir
from concourse.bass_utils import run_bass_kernel_spmd

F32 = mybir.dt.float32
BF16 = mybir.dt.bfloat16
AF = mybir.ActivationFunctionType
ALU = mybir.AluOpType
AX = mybir.AxisListType

D = 1024
DP = 6176
DFF = 2816
NFC = DFF // 128
NE = 8
NH = 16
EPS = 1e-5
NEG = -30000.0
NDS = 88


class Sem:
    def __init__(self, h):
        self.h = h
        self.cnt = 0


class Buf:
    def __init__(self, t, dram=False):
        self.t = t
        self.dram = dram
        self.w = {}
        self.r = {}
        self.sem = None

    def __getitem__(self, k):
        return self.t[k]


class Alias:
    def __init__(self, parent, ap):
        self.p = parent
        self.ap_ = ap
        self.dram = False

    def __getitem__(self, k):
        return self.ap_[k]

    w = property(lambda s: s.p.w, lambda s, v: setattr(s.p, "w", v))
    r = property(lambda s: s.p.r, lambda s, v: setattr(s.p, "r", v))
    sem = property(lambda s: s.p.sem, lambda s, v: setattr(s.p, "sem", v))


class KB:
    def __init__(self, nc, stack):
        self.nc = nc
        self.stack = stack
        self.eng = dict(pe=nc.tensor, act=nc.scalar, dve=nc.vector, pool=nc.gpsimd, sp=nc.sync)
        self.esem = {e: Sem(stack.enter_context(nc.semaphore("se_" + e))) for e in self.eng}
        self.seen = {e: {} for e in self.eng}
        self.dsems = [Sem(stack.enter_context(nc.semaphore("sd%d" % i))) for i in range(NDS)]
        self.free = list(self.dsems)
        self.bar = Sem(stack.enter_context(nc.semaphore("sbar")))
        self.uid = 0

    def _wait(self, e, toks, skip=None):
        for s, val in list(toks.items()):
            if s is skip:
                continue
            if self.seen[e].get(s, 0) < val:
                self.eng[e].wait_ge(s.h, val)
                self.seen[e][s] = val

    def op(self, e, fn, reads=(), writes=(), sreads=(), fence=False):
        own = self.esem[e]
        for b in reads:
            self._wait(e, b.w, own if e == "pe" else None)
        for b in sreads:
            self._wait(e, b.w)
        for b in writes:
            self._wait(e, b.w, own)
            self._wait(e, b.r, own)
        ins = fn(self.eng[e])
        own.cnt += 1
        ins.then_inc(own.h, 1)
        for b in list(reads) + list(sreads):
            if b not in writes:
                b.r[own] = own.cnt
        for b in writes:
            b.w = {own: own.cnt}
            b.r = {}
        if fence:
            self._wait(e, {own: own.cnt})
        return ins

    def dma(self, q, out, in_, sb, reads=(), writes=()):
        if sb.sem is None:
            sb.sem = self.free.pop()
        s = sb.sem
        for b in reads:
            if not b.dram:
                self._wait(q, b.w)
        for b in writes:
            if not b.dram:
                self._wait(q, b.w, s)
                self._wait(q, b.r)
        ins = self.eng[q].dma_start(out=out, in_=in_)
        s.cnt += 16
        ins.then_inc(s.h, 16)
        for b in reads:
            if not b.dram:
                b.r[s] = s.cnt
        for b in writes:
            if b.dram:
                pass
            else:
                b.w = {s: s.cnt}
                b.r = {}
        return ins

    def release(self, bufs):
        for b in bufs:
            if b.sem is not None:
                self.free.append(b.sem)
                b.sem = None

    def barrier(self):
        for e, s in self.esem.items():
            if e != "sp" and s.cnt:
                self._wait("sp", {s: s.cnt})
        for s in self.dsems:
            if s.cnt:
                self._wait("sp", {s: s.cnt})
        self.nc.sync.sem_inc(self.bar.h, 1)
        self.bar.cnt += 1
        for e in self.eng:
            if e != "sp":
                self._wait(e, {self.bar: self.bar.cnt})


class Phase:
    def __init__(self, kb, name):
        self.kb = kb
        self.name = name
        self.stack = ExitStack()
        self.bufs = []

    def sb(self, name, shape, dt):
        self.kb.uid += 1
        t = self.stack.enter_context(self.kb.nc.sbuf_tensor("%s_%s_%d" % (self.name, name, self.kb.uid), list(shape), dt))
        b = Buf(t)
        self.bufs.append(b)
        return b

    def ps(self, name, shape, dt):
        self.kb.uid += 1
        t = self.stack.enter_context(self.kb.nc.psum_tensor("%s_%s_%d" % (self.name, name, self.kb.uid), list(shape), dt))
        b = Buf(t)
        self.bufs.append(b)
        return b

    def __enter__(self):
        return self

    def __exit__(self, *a):
        if a[0] is None:
            self.kb.barrier()
            self.kb.release(self.bufs)
        self.stack.close()
        return False


def bc(ap, shape):
    return ap.broadcast_to(list(shape))


class _Stop(Exception):
    pass


def build(L, NB, depth=2, dbg=(), stop=None):
    T = NB * L
    NT = T // 128
    NS = T // 512
    TPS = L // 128
    nc = bass.Bass("TRN2", target_bir_lowering=False)

    def din(name, shape, dt=F32):
        return nc.dram_tensor(name, list(shape), dt, kind="ExternalInput").ap()

    def dscr(name, shape, dt):
        kind = "ExternalOutput" if name in dbg else "Internal"
        return nc.dram_tensor(name, list(shape), dt, kind=kind).ap()

    x_in = din("x", [T, D])
    w_in = din("w_in", [2, D, DP])
    w_out = din("w_out", [2, 2 * D, D])
    ffn_wg = din("ffn_w_gate", [1, D, DFF])
    ffn_wu = din("ffn_w_up", [1, D, DFF])
    ffn_wd = din("ffn_w_down", [1, DFF, D])
    moe_wg = din("moe_w_gate", [1, NE, D, DFF])
    moe_wu = din("moe_w_up", [1, NE, D, DFF])
    moe_wd = din("moe_w_down", [1, NE, DFF, D])
    g_mix = din("g_mix", [2, 128, 8])
    g_ffn = din("g_ffn", [2, 128, 8])
    g_out = din("g_out", [2, 128, 16])
    cw = din("cw", [2, 128, 16, 4])
    cb = din("cb", [2, 128, 16])
    hp = din("hp", [2, 128, 64])
    nfb = din("nfb", [2, 16, 1])
    rw = din("rw", [128, 8, 8])
    g_fin = din("g_fin", [128, D])
    c_id_b = din("c_id_b", [128, 128], BF16)
    c_id_f = din("c_id_f", [128, 128])
    c_tri = din("c_tri", [128, 128])
    c_ones = din("c_ones", [128, 128])
    c_mb4 = din("c_mb4", [128, 512])
    c_tri01 = din("c_tri01", [128, 128], BF16)
    out_d = nc.dram_tensor("out", [T, D], F32, kind="ExternalOutput").ap()

    xa = dscr("xa", [T, D], F32)
    xb = dscr("xb", [T, D], F32)
    hT = dscr("hT", [D, T], BF16)
    zs = dscr("zs", [T, D], BF16)
    xbcT = dscr("xbcT", [2 * D, T], BF16)
    qa = dscr("qa", [NB, NH, 70, L], BF16)
    ka = dscr("ka", [NB, NH, 70, L], BF16)
    va = dscr("va", [T, NH, 65], BF16)
    oT = dscr("oT", [D, T], F32)
    rsum = dscr("rsum", [NH, T], F32)
    yT = dscr("yT", [D, T], BF16)
    dts = dscr("dts", [T, NH], F32)
    cmb = dscr("cmb", [T, NE], F32)

    stack = ExitStack()
    kb = KB(nc, stack)
    B_xa, B_xb, B_hT, B_zs, B_xbcT, B_qa, B_ka, B_va, B_oT, B_rs, B_yT, B_dts, B_cmb, B_out = [
        Buf(None, dram=True) for _ in range(14)]
    B_in = Buf(None, dram=True)

    def sbp(name, shape, dt):
        return Buf(stack.enter_context(nc.sbuf_tensor(name, list(shape), dt)))

    idb = sbp("idb", [128, 128], BF16)
    idf = sbp("idf", [128, 128], F32)
    tri = sbp("tri", [128, 128], F32)
    ones = sbp("ones", [128, 128], F32)
    mb4 = sbp("mb4", [128, 512], F32)
    tri01 = sbp("tri01", [128, 128], BF16)
    onesb = sbp("onesb", [128, 128], BF16)
    for b, src in ((idb, c_id_b), (idf, c_id_f), (tri, c_tri), (ones, c_ones), (mb4, c_mb4), (tri01, c_tri01)):
        kb.dma("sp", b[:], src, b, reads=[B_in], writes=[b])
    kb.op("dve", lambda e: e.tensor_copy(out=onesb[:], in_=ones[:]), reads=[ones], writes=[onesb])
    kb.barrier()

    def load_w(ph, dst, kcs, cols, src_fn, gain, stg, col0=0):
        i = 0
        CH = stg[0].t.shape[1]
        for kc in range(kcs):
            for c0 in range(0, cols, CH):
                cn = min(CH, cols - c0)
                s = stg[i % len(stg)]
                q = "sp" if i % 2 == 0 else "act"
                kb.dma(q, s[:, 0:cn], src_fn(kc)[:, c0:c0 + cn], s, reads=[B_in], writes=[s])
                e = ("dve", "pool")[i % 2]
                if gain is None:
                    kb.op(e, lambda en, s=s, kc=kc, c0=c0, cn=cn: en.tensor_copy(
                        out=dst[:, kc, col0 + c0:col0 + c0 + cn], in_=s[:, 0:cn]), reads=[s], writes=[dst])
                else:
                    kb.op(e, lambda en, s=s, kc=kc, c0=c0, cn=cn: en.tensor_scalar(
                        out=dst[:, kc, col0 + c0:col0 + c0 + cn], in0=s[:, 0:cn], scalar1=gain[:, kc:kc + 1],
                        scalar2=None, op0=ALU.mult), reads=[s], writes=[dst], sreads=[gain])
                i += 1

    def w_chunks(dst, kcs, cols, src_fn, gain, stg, eng="pool", q="pool"):
        CH = stg[0].t.shape[1]
        items = [(kc, c0, min(CH, cols - c0)) for kc in range(kcs) for c0 in range(0, cols, CH)]

        def dma_k(k):
            kc, c0, cn = items[k]
            s_ = stg[k % len(stg)]
            kb.dma(q, s_[:, 0:cn], src_fn(kc)[:, c0:c0 + cn], s_, reads=[B_in], writes=[s_])

        def cast_k(k):
            kc, c0, cn = items[k]
            s_ = stg[k % len(stg)]
            if gain is None:
                kb.op(eng, lambda en: en.tensor_copy(out=dst[:, kc, c0:c0 + cn], in_=s_[:, 0:cn]), reads=[s_], writes=[dst])
            else:
                kb.op(eng, lambda en: en.tensor_scalar(out=dst[:, kc, c0:c0 + cn], in0=s_[:, 0:cn], scalar1=gain[:, kc:kc + 1],
                                                       scalar2=None, op0=ALU.mult), reads=[s_], writes=[dst], sreads=[gain])
        out = [lambda: dma_k(0)]
        for k in range(len(items)):
            def f(k=k):
                if k + 1 < len(items):
                    dma_k(k + 1)
                cast_k(k)
            out.append(f)
        return out

    def outer_sb(st_, name, shape, dt):
        kb.uid += 1
        return Buf(st_.enter_context(nc.sbuf_tensor("%s_%d" % (name, kb.uid), list(shape), dt)))

    def rstd_gen(ph_tmp, src, ssq, rs, junk, n=D):
        kb.op("act", lambda e: e.activation(out=junk[:, 0:n], in_=src, func=AF.Square, accum_out=ssq[:, 0:1]),
              reads=[ph_tmp], writes=[junk, ssq])
        yield
        kb.op("act", lambda e: e.activation(out=rs[:, 0:1], in_=ssq[:, 0:1], func=AF.Sqrt, scale=1.0 / n, bias=EPS),
              reads=[ssq], writes=[rs])
        yield
        kb.op("dve", lambda e: e.reciprocal(out=rs[:, 0:1], in_=rs[:, 0:1]), reads=[rs], writes=[rs])
        yield

    def rstd_of(*a, **k):
        for _ in rstd_gen(*a, **k):
            pass

    class NormT:
        def __init__(self, ph, nhs=2, pT=None, hs=None):
            self.ph = ph
            self.nhs = nhs if hs is None else len(hs)
            self.ssq = [ph.sb("n_ssq%d" % i, [128, 1], F32) for i in range(2)]
            self.rs = [ph.sb("n_rs%d" % i, [128, 1], F32) for i in range(2)]
            self.junk = ph.sb("n_junk", [128, D], BF16)
            self.hb = [ph.sb("n_hb%d" % i, [128, D], BF16) for i in range(2)]
            self.pT = ph.ps("n_pT", [128, D], BF16) if pT is None else pT
            self.hs = [ph.sb("n_hs%d" % i, [128, 8, 512], BF16) for i in range(nhs)] if hs is None else hs
            self.n = 0

        def emit(self, xt, tile):
            for _ in self.emit_gen(xt, tile):
                pass
            return self.last_rs

        def emit_gen(self, xt, tile):
            i = self.n % 2
            self.n += 1
            ssq, rs, hb = self.ssq[i], self.rs[i], self.hb[i]
            self.last_rs = rs
            yield from rstd_gen(xt, xt[:, :], ssq, rs, self.junk)
            kb.op("dve", lambda e: e.tensor_scalar(out=hb[:, :], in0=xt[:, :], scalar1=rs[:, 0:1], scalar2=None,
                                                   op0=ALU.mult), reads=[xt], writes=[hb], sreads=[rs])
            yield
            pT = self.pT

            def tr(e):
                ins = None
                for kc in range(8):
                    ins = e.transpose(pT[:, kc * 128:(kc + 1) * 128], hb[:, kc * 128:(kc + 1) * 128], idb[:, :])
                return ins
            kb.op("pe", tr, reads=[hb, idb], writes=[pT])
            yield
            s, tt = tile // 4, tile % 4
            hs = self.hs[s % self.nhs]
            kb.op("act", lambda e: e.copy(out=hs[:, :, tt * 128:(tt + 1) * 128],
                                          in_=pT[:, :].rearrange("p (k t) -> p k t", k=8)), reads=[pT], writes=[hs])
            yield
            if tt == 3:
                kb.dma("pool", hT.rearrange("(k p) t -> p k t", p=128)[:, :, s * 512:(s + 1) * 512], hs[:, :, :], hs,
                       reads=[hs], writes=[B_hT])
            yield

    def chk(name):
        if stop == name:
            raise _Stop()

    try:
      for layer in range(depth):
          x_src, B_xsrc = (x_in, B_in) if layer == 0 else (xb, B_xb)

          wvin = w_in[layer].rearrange("(k p) n -> k p n", p=128)
          outer_in = ExitStack()
          preA1 = None
          if layer == 0:
              W_a1 = outer_sb(outer_in, "W_a1", [128, 8, 3072], BF16)
              gm_a1 = outer_sb(outer_in, "gm_a1", [128, 8], F32)
              kb.dma("sp", gm_a1[:, :], g_mix[layer], gm_a1, reads=[B_in], writes=[gm_a1])
              preA1 = (W_a1, gm_a1)
              with Phase(kb, "n1") as ph:
                  nt = NormT(ph)
                  xts = [ph.sb("xt%d" % i, [128, D], F32) for i in range(3)]
                  pstg = [ph.sb("pstg%d" % i, [128, 1536], F32) for i in range(2)]
                  pend = w_chunks(W_a1, 8, 3072, lambda kc: wvin[kc, :, 0:3072], gm_a1, pstg)
                  for t in range(NT):
                      xt = xts[t % 3]
                      kb.dma("sp", xt[:, :], x_src[t * 128:(t + 1) * 128, :], xt, reads=[B_xsrc], writes=[xt])
                      nt.emit(xt, t)
                      if pend and t % 2 == 1:
                          pend.pop(0)()
                  while pend:
                      pend.pop(0)()
          NW = DP - 3072
          W_a2 = outer_sb(outer_in, "W_a2", [128, 8, NW], BF16)
          gm_a2 = outer_sb(outer_in, "gm_a2", [128, 8], F32)
          kb.dma("sp", gm_a2[:, :], g_mix[layer], gm_a2, reads=[B_in], writes=[gm_a2])

          chk("n1")
          with Phase(kb, "a1") as ph:
              cwt = ph.sb("cwt", [128, 16, 4], F32)
              cbt = ph.sb("cbt", [128, 16], F32)
              kb.dma("sp", cwt[:, :, :], cw[layer], cwt, reads=[B_in], writes=[cwt])
              kb.dma("sp", cbt[:, :], cb[layer], cbt, reads=[B_in], writes=[cbt])
              if preA1 is not None:
                  W, gm = preA1
              else:
                  W = ph.sb("W", [128, 8, 3072], BF16)
                  gm = ph.sb("gm", [128, 8], F32)
                  stg = [ph.sb("stg%d" % i, [128, 1536], F32) for i in range(2)]
                  kb.dma("sp", gm[:, :], g_mix[layer], gm, reads=[B_in], writes=[gm])
                  load_w(ph, W, 8, 3072, lambda kc: wvin[kc, :, 0:3072], gm, stg)
              pstg = [ph.sb("pstg%d" % i, [128, 1552], F32) for i in range(2)]
              pend = w_chunks(W_a2, 8, NW, lambda kc: wvin[kc, :, 3072:DP], gm_a2, pstg)
              hts = [ph.sb("hts%d" % i, [128, 8, 512], BF16) for i in range(2)]
              pss = [ph.ps("ps%d" % i, [128, 512], F32) for i in range(6)]
              zst = [ph.sb("zst%d" % i, [128, D], BF16) for i in range(2)]
              xr = [ph.sb("xr%d" % i, [128, 515], F32) for i in range(4)]
              halos = [ph.sb("halo%d" % i, [128, 8, 3], F32) for i in range(2)]
              acc = [ph.sb("acc%d" % i, [128, 512], F32) for i in range(4)]
              xst = [ph.sb("xst%d" % i, [128, 16, 512], BF16) for i in range(2)]
              pi = 0
              for s in range(NS):
                  ht = hts[s % 2]
                  kb.dma("sp", ht[:, :, :], hT.rearrange("(k p) t -> p k t", p=128)[:, :, s * 512:(s + 1) * 512], ht,
                         reads=[B_hT], writes=[ht])
                  if (s * 512) % L == 0:
                      for hi_, he_ in enumerate(("dve", "pool")):
                          kb.op(he_, lambda e, hi_=hi_: e.memset(halos[hi_][:, :, :], 0.0), writes=[halos[hi_]], fence=True)
                  for tt in range(4):
                      zt = zst[tt % 2]
                      for half in range(2):
                          p = pss[pi % 6]
                          pi += 1

                          def mm(e, p=p, tt=tt, half=half):
                              ins = None
                              for kc in range(8):
                                  ins = e.matmul(p[:, :], lhsT=ht[:, kc, tt * 128:(tt + 1) * 128],
                                                 rhs=W[:, kc, half * 512:(half + 1) * 512], start=(kc == 0), stop=(kc == 7))
                              return ins
                          kb.op("pe", mm, reads=[ht, W], writes=[p])
                          kb.op("act", lambda e, p=p, half=half, zt=zt: e.copy(out=zt[:, half * 512:(half + 1) * 512], in_=p[:, :]),
                                reads=[p], writes=[zt])
                      tile = s * 4 + tt
                      kb.dma("pool", zs[tile * 128:(tile + 1) * 128, :], zt[:, :], zt, reads=[zt], writes=[B_zs])
                  xs_t = xst[s % 2]
                  for c in range(16):
                      p = pss[pi % 6]
                      pi += 1

                      def mm(e, p=p, c=c):
                          ins = None
                          for kc in range(8):
                              ins = e.matmul(p[:, :], lhsT=W[:, kc, 1024 + c * 128:1024 + (c + 1) * 128], rhs=ht[:, kc, :],
                                             start=(kc == 0), stop=(kc == 7))
                          return ins
                      kb.op("pe", mm, reads=[ht, W], writes=[p])
                      r = xr[c % 4]
                      a = acc[c % 4]
                      ce = "dve"
                      halo = halos[c % 2]
                      kb.op("act", lambda e, p=p, r=r: e.copy(out=r[:, 3:515], in_=p[:, :]), reads=[p], writes=[r])
                      kb.op(ce, lambda e, r=r, c=c, halo=halo: e.tensor_copy(out=r[:, 0:3], in_=halo[:, c // 2, :]), reads=[halo], writes=[r])
                      kb.op(ce, lambda e, r=r, a=a, c=c: e.tensor_scalar(out=a[:, :], in0=r[:, 0:512], scalar1=cwt[:, c, 0:1],
                                                                     scalar2=None, op0=ALU.mult),
                            reads=[r], writes=[a], sreads=[cwt])
                      for k in range(1, 4):
                          kb.op(ce, lambda e, r=r, a=a, c=c, k=k: e.scalar_tensor_tensor(
                              out=a[:, :], in0=r[:, k:k + 512], scalar=cwt[:, c, k:k + 1], in1=a[:, :], op0=ALU.mult, op1=ALU.add),
                              reads=[r, a], writes=[a], sreads=[cwt])
                      kb.op(ce, lambda e, r=r, c=c, halo=halo: e.tensor_copy(out=halo[:, c // 2, :], in_=r[:, 512:515]), reads=[r], writes=[halo])
                      kb.op("act", lambda e, a=a, c=c, xs_t=xs_t: e.activation(out=xs_t[:, c, :], in_=a[:, :], func=AF.Silu,
                                                                            bias=cbt[:, c:c + 1]),
                            reads=[a], writes=[xs_t], sreads=[cbt])
                  kb.dma("pool", xbcT.rearrange("(c p) t -> p c t", p=128)[:, :, s * 512:(s + 1) * 512], xs_t[:, :, :], xs_t,
                         reads=[xs_t], writes=[B_xbcT])
                  for _ in range(2):
                      if pend:
                          pend.pop(0)()
              while pend:
                  pend.pop(0)()

          chk("a1")
          with Phase(kb, "a2") as ph:
              W, gm = W_a2, gm_a2
              hpt = ph.sb("hpt", [128, 64], F32)
              nfbt = ph.sb("nfbt", [16, 1], F32)
              kb.dma("sp", hpt[:, :], hp[layer], hpt, reads=[B_in], writes=[hpt])
              kb.dma("sp", nfbt[:, :], nfb[layer], nfbt, reads=[B_in], writes=[nfbt])
              hts = [ph.sb("hts%d" % i, [128, 8, 512], BF16) for i in range(2)]
              pss = [ph.ps("ps%d" % i, [128, 512], F32) for i in range(6)]
              psd = ph.ps("psd", [128, 512], F32)
              psf = ph.ps("psf", [128, 512], F32)
              qst = [ph.sb("qst%d" % i, [128, 8, 512], BF16) for i in range(2)]
              kst = [ph.sb("kst%d" % i, [128, 8, 512], BF16) for i in range(2)]
              vst = [ph.sb("vst%d" % i, [128, NH, 65], BF16) for i in range(2)]
              dtt = [ph.sb("dtt%d" % i, [128, 16], F32) for i in range(2)]
              Gs = [ph.sb("G%d" % i, [16, 512], F32) for i in range(2)]
              spf = [ph.sb("spf%d" % i, [16, 512], F32) for i in range(2)]
              g8 = ph.sb("g8", [16, 512], F32)
              gh = [ph.sb("gh%d" % i, [16, 512], BF16) for i in range(6)]
              ngh = [ph.sb("ngh%d" % i, [16, 512], BF16) for i in range(6)]
              g32 = ph.sb("g32", [16, 512], F32)
              onl = ph.sb("onl", [16, 512], BF16)
              onf = ph.sb("onf", [16, 512], F32)
              kb.op("pool", lambda e: e.memset(onl[:, :], 1.0), writes=[onl], fence=True)
              kb.op("pool", lambda e: e.memset(onf[:, :], 1.0), writes=[onf], fence=True)
              kb.op("dve", lambda e: e.tensor_scalar(out=nfbt[:, :], in0=nfbt[:, :], scalar1=-1.0, scalar2=None, op0=ALU.mult),
                    reads=[nfbt], writes=[nfbt])
              for v_ in vst:
                  kb.op("pool", lambda e, v_=v_: e.memset(v_[:, :, :], 1.0), writes=[v_], fence=True)
              pi = 0
              for s in range(NS):
                  b_, t0 = (s * 512) // L, (s * 512) % L
                  ht = hts[s % 2]
                  kb.dma("sp", ht[:, :, :], hT.rearrange("(k p) t -> p k t", p=128)[:, :, s * 512:(s + 1) * 512], ht,
                         reads=[B_hT], writes=[ht])
                  for which, st_, dd, Bd, off in (("q", qst[s % 2], qa, B_qa, 16), ("k", kst[s % 2], ka, B_ka, 16 + 1024)):
                      for c in range(8):
                          p = pss[pi % 6]
                          pi += 1

                          def mm(e, p=p, c=c, off=off):
                              ins = None
                              for kc in range(8):
                                  ins = e.matmul(p[:, :], lhsT=W[:, kc, off + c * 128:off + (c + 1) * 128], rhs=ht[:, kc, :],
                                                 start=(kc == 0), stop=(kc == 7))
                              return ins
                          kb.op("pe", mm, reads=[ht, W], writes=[p])
                          ce = ("act", "dve")[c % 2]
                          if ce == "act":
                              kb.op("act", lambda e, p=p, c=c, st_=st_: e.copy(out=st_[:, c, :], in_=p[:, :]), reads=[p], writes=[st_])
                          else:
                              kb.op("dve", lambda e, p=p, c=c, st_=st_: e.tensor_copy(out=st_[:, c, :], in_=p[:, :]), reads=[p], writes=[st_])
                      for par in range(2):
                          dst = dd[b_, :, 0:64, t0:t0 + 512].rearrange("(c two) d t -> two d c t", two=2)[par]
                          kb.dma("pool", dst, st_[par * 64:(par + 1) * 64, :, :], st_, reads=[st_], writes=[Bd])
                  for tt in range(4):
                      tile = s * 4 + tt
                      vt = vst[tt % 2]
                      for half in range(2):
                          p = pss[pi % 6]
                          pi += 1

                          def mm(e, p=p, tt=tt, half=half):
                              ins = None
                              for kc in range(8):
                                  ins = e.matmul(p[:, :], lhsT=ht[:, kc, tt * 128:(tt + 1) * 128],
                                                 rhs=W[:, kc, 2064 + half * 512:2064 + (half + 1) * 512], start=(kc == 0), stop=(kc == 7))
                              return ins
                          kb.op("pe", mm, reads=[ht, W], writes=[p])
                          kb.op("act", lambda e, p=p, half=half, vt=vt: e.copy(
                              out=vt[:, half * 8:(half + 1) * 8, 0:64], in_=p[:, :].rearrange("p (h d) -> p h d", d=64)),
                              reads=[p], writes=[vt])
                      kb.dma("pool", va[tile * 128:(tile + 1) * 128, :, :], vt[:, :, :], vt, reads=[vt], writes=[B_va])

                      def mmd(e, tt=tt):
                          ins = None
                          for kc in range(8):
                              ins = e.matmul(psd[:, 0:16], lhsT=ht[:, kc, tt * 128:(tt + 1) * 128], rhs=W[:, kc, 0:16],
                                             start=(kc == 0), stop=(kc == 7))
                          return ins
                      kb.op("pe", mmd, reads=[ht, W], writes=[psd])
                      d_ = dtt[tt % 2]
                      kb.op("dve", lambda e, d_=d_: e.tensor_tensor(out=d_[:, :], in0=psd[:, 0:16], in1=hpt[:, 0:16], op=ALU.add),
                            reads=[psd, hpt], writes=[d_])
                      kb.op("act", lambda e, d_=d_: e.activation(out=d_[:, :], in_=d_[:, :], func=AF.Exp), reads=[d_], writes=[d_])
                      kb.op("act", lambda e, d_=d_: e.activation(out=d_[:, :], in_=d_[:, :], func=AF.Ln, bias=1.0), reads=[d_], writes=[d_])
                      kb.dma("pool", dts[tile * 128:(tile + 1) * 128, :], d_[:, :], d_, reads=[d_], writes=[B_dts])

                  def mmf(e):
                      ins = None
                      for kc in range(8):
                          ins = e.matmul(psf[0:16, :], lhsT=W[:, kc, 3088:3104], rhs=ht[:, kc, :], start=(kc == 0), stop=(kc == 7))
                      return ins
                  kb.op("pe", mmf, reads=[ht, W], writes=[psf])
                  sp_ = spf[s % 2]
                  kb.op("act", lambda e, sp_=sp_: e.activation(out=sp_[:, :], in_=psf[0:16, :], func=AF.Exp, scale=-1.0, bias=nfbt[:, 0:1]),
                        reads=[psf], writes=[sp_], sreads=[nfbt])
                  kb.op("act", lambda e, sp_=sp_: e.activation(out=sp_[:, :], in_=sp_[:, :], func=AF.Ln, bias=1.0), reads=[sp_], writes=[sp_])
                  G = Gs[s % 2]
                  Gp = Gs[(s + 1) % 2]
                  if t0 == 0:
                      kb.op("dve", lambda e, sp_=sp_, G=G: e.tensor_tensor_scan(out=G[:, :], data0=onf[:, :], data1=sp_[:, :],
                                                                               initial=0.0, op0=ALU.mult, op1=ALU.add),
                            reads=[sp_, onf], writes=[G])
                  else:
                      kb.op("dve", lambda e, sp_=sp_, G=G, Gp=Gp: e.tensor_tensor_scan(
                          out=G[:, :], data0=onf[:, :], data1=sp_[:, :], initial=Gp[:, 511:512],
                          op0=ALU.mult, op1=ALU.add), reads=[sp_, onf], writes=[G], sreads=[Gp])
                  kb.op("dve", lambda e, G=G: e.tensor_scalar(out=g8[:, :], in0=G[:, :], scalar1=8.0, scalar2=None, op0=ALU.mult),
                        reads=[G], writes=[g8])
                  for j in range(3):
                      gj, ngj = gh[(s % 2) * 3 + j], ngh[(s % 2) * 3 + j]
                      kb.op("dve", lambda e, gj=gj: e.tensor_copy(out=gj[:, :], in_=g8[:, :]), reads=[g8], writes=[gj])
                      kb.op("dve", lambda e, gj=gj: e.tensor_copy(out=g32[:, :], in_=gj[:, :]), reads=[gj], writes=[g32])
                      if j < 2:
                          kb.op("dve", lambda e: e.tensor_sub(out=g8[:, :], in0=g8[:, :], in1=g32[:, :]), reads=[g8, g32], writes=[g8])
                      kb.op("dve", lambda e, ngj=ngj: e.tensor_scalar(out=ngj[:, :], in0=g32[:, :], scalar1=-1.0, scalar2=None,
                                                                    op0=ALU.mult), reads=[g32], writes=[ngj])
                      kb.dma("pool", qa[b_, :, 64 + j, t0:t0 + 512], ngj[:, :], ngj, reads=[ngj], writes=[B_qa])
                      kb.dma("pool", qa[b_, :, 67 + j, t0:t0 + 512], onl[:, :], onl, reads=[onl], writes=[B_qa])
                      kb.dma("pool", ka[b_, :, 64 + j, t0:t0 + 512], onl[:, :], onl, reads=[onl], writes=[B_ka])
                      kb.dma("pool", ka[b_, :, 67 + j, t0:t0 + 512], gj[:, :], gj, reads=[gj], writes=[B_ka])

          outer_in.close()
          chk("a2")
          with Phase(kb, "ssd") as ph:
              hpt = ph.sb("hpt", [128, 64], F32)
              abc = ph.sb("abc", [128, 16], F32)
              kb.dma("sp", hpt[:, :], hp[layer], hpt, reads=[B_in], writes=[hpt])
              kb.op("act", lambda e: e.activation(out=abc[:, :], in_=hpt[:, 16:32], func=AF.Exp), reads=[hpt], writes=[abc])
              kb.op("dve", lambda e: e.tensor_scalar(out=abc[:, :], in0=abc[:, :], scalar1=-1.0, scalar2=None, op0=ALU.mult),
                    reads=[abc], writes=[abc])
              def ssd_stream(si, slist):
                  sfx = "_%d" % si
                  xbt = [ph.sb("xbt%d" % i + sfx, [128, 16, 512], BF16) for i in range(1)]
                  zt_ = [ph.sb("zt%d" % i + sfx, [128, D], BF16) for i in range(2)]
                  dtb = [ph.sb("dtb%d" % i + sfx, [128, 16], F32) for i in range(2)]
                  b0 = ph.ps("b0" + sfx, [128, 512], F32)
                  b1 = ph.ps("b1" + sfx, [128, 512], F32)
                  b2 = ph.ps("b2" + sfx, [128, 512], F32)
                  b3 = ph.ps("b3" + sfx, [128, 512], F32)
                  pT = Alias(b0, b0.t[:, :].bitcast(BF16))
                  pR = b0
                  pA = b1
                  pG = b1
                  pY = b1
                  pB = Alias(b2, b2.t[:, :].bitcast(BF16))
                  pYo = b2
                  pS = b3
                  Gsb = ph.sb("Gsb" + sfx, [128, 512], F32)
                  xc = ph.sb("xc" + sfx, [128, NH, 64], BF16)
                  xcd = ph.sb("xcd" + sfx, [128, NH, 64], BF16)
                  xsd = ph.sb("xsd" + sfx, [128, NH, 64], F32)
                  btok = ph.sb("btok" + sfx, [128, 4, 128], BF16)
                  adt = ph.sb("adt" + sfx, [128, 16], F32)
                  acs = ph.sb("acs" + sfx, [128, 32], F32)
                  nacs = ph.sb("nacs" + sfx, [128, 16], F32)
                  dout = ph.sb("dout" + sfx, [128, 16], F32)
                  ea = ph.sb("ea" + sfx, [128, 16], F32)
                  cdec = ph.sb("cdec" + sfx, [128, 16], F32)
                  rr = [ph.sb("rr%d" % i + sfx, [128, 4, 128], F32) for i in range(2)]
                  Es = [ph.sb("Es%d" % i + sfx, [128, 4, 128], F32) for i in range(2)]
                  Mt = [ph.sb("Mt%d" % i + sfx, [128, 4, 128], BF16) for i in range(2)]
                  prev = ph.sb("prev" + sfx, [128, D], F32)
                  prevb = ph.sb("prevb" + sfx, [128, D], BF16)
                  tmp = ph.sb("tmp" + sfx, [128, 512], F32)
                  ysb = ph.sb("ysb" + sfx, [128, D], F32)
                  gz = ph.sb("gz" + sfx, [128, D], F32)
                  ssq = ph.sb("ssq" + sfx, [128, 1], F32)
                  rs = ph.sb("rs" + sfx, [128, 1], F32)
                  junk = ph.sb("junk" + sfx, [128, D], BF16)
                  yb = ph.sb("yb" + sfx, [128, D], BF16)
                  yst = [ph.sb("yst%d" % i + sfx, [128, 8, 512], BF16) for i in range(1)]
                  for s in slist:
                      xt_ = xbt[0]
                      kb.dma("sp", xt_[:, :, :], xbcT.rearrange("(c p) t -> p c t", p=128)[:, :, s * 512:(s + 1) * 512], xt_,
                             reads=[B_xbcT], writes=[xt_])
                      yield
                      ys_ = yst[0]
                      for tt in range(4):
                          tile = s * 4 + tt
                          tk = slice(tt * 128, (tt + 1) * 128)
                          z_ = zt_[tile % 2]
                          d_ = dtb[tile % 2]
                          kb.dma("sp", z_[:, :], zs[tile * 128:(tile + 1) * 128, :], z_, reads=[B_zs], writes=[z_])
                          yield
                          kb.dma("sp", d_[:, :], dts[tile * 128:(tile + 1) * 128, :], d_, reads=[B_dts], writes=[d_])
                          yield
                          if (tile * 128) % L == 0:
                              kb.op("dve", lambda e: e.memset(prev[:, :], 0.0), writes=[prev], fence=True)
                              yield
                              kb.op("pool", lambda e: e.memset(prevb[:, :], 0.0), writes=[prevb], fence=True)
                              yield
                          def trx(e, tk=tk):
                              ins = None
                              for c in range(8):
                                  ins = e.transpose(pT[:, c * 128:(c + 1) * 128], xt_[:, c, tk], idb[:, :])
                              return ins
                          kb.op("pe", trx, reads=[xt_, idb], writes=[pT])
                          yield

                          def trb(e, tk=tk):
                              ins = None
                              for c in range(4):
                                  ins = e.transpose(pB[:, c * 128:(c + 1) * 128], xt_[:, 8 + c, tk], idb[:, :])
                              return ins
                          kb.op("pe", trb, reads=[xt_, idb], writes=[pB])
                          yield
                          kb.op("act", lambda e: e.copy(out=btok[:, :, :], in_=pB[:, 0:512].rearrange("p (g n) -> p g n", g=4)),
                                reads=[pB], writes=[btok])
                          yield
                          kb.op("dve", lambda e, d_=d_: e.tensor_tensor(out=adt[:, :], in0=d_[:, :], in1=abc[:, :], op=ALU.mult),
                                reads=[d_, abc], writes=[adt])
                          yield

                          def mma(e):
                              e.matmul(pA[:, 0:16], lhsT=tri[:, :], rhs=adt[:, :], start=True, stop=True)
                              return e.matmul(pA[:, 16:32], lhsT=ones[:, :], rhs=adt[:, :], start=True, stop=True)
                          kb.op("pe", mma, reads=[tri, ones, adt], writes=[pA])
                          yield
                          kb.op("dve", lambda e: e.tensor_copy(out=acs[:, :], in_=pA[:, 0:32]), reads=[pA], writes=[acs])
                          yield
                          kb.op("dve", lambda e: e.tensor_scalar(out=nacs[:, :], in0=acs[:, 0:16], scalar1=-1.0, scalar2=None, op0=ALU.mult),
                                reads=[acs], writes=[nacs])
                          yield
                          kb.op("dve", lambda e: e.tensor_sub(out=dout[:, :], in0=acs[:, 16:32], in1=acs[:, 0:16]), reads=[acs], writes=[dout])
                          yield
                          kb.op("act", lambda e: e.activation(out=dout[:, :], in_=dout[:, :], func=AF.Exp), reads=[dout], writes=[dout])
                          yield
                          kb.op("act", lambda e: e.activation(out=ea[:, :], in_=acs[:, 0:16], func=AF.Exp), reads=[acs], writes=[ea])
                          yield
                          kb.op("act", lambda e: e.activation(out=cdec[:, :], in_=acs[:, 16:32], func=AF.Exp), reads=[acs], writes=[cdec])
                          yield
                          pT3 = pT[:, :].rearrange("p (h d) -> p h d", d=64)
                          kb.op("dve", lambda e, d_=d_: e.tensor_tensor(out=xc[:, :, :], in0=pT3, in1=bc(d_[:, 0:16].unsqueeze(2), [128, 16, 64]),
                                                                       op=ALU.mult), reads=[pT, d_], writes=[xc])
                          yield
                          kb.op("pool", lambda e: e.tensor_tensor(out=xcd[:, :, :], in0=xc[:, :, :], in1=bc(dout[:, 0:16].unsqueeze(2), [128, 16, 64]),
                                                                  op=ALU.mult), reads=[xc, dout], writes=[xcd])
                          yield
                          kb.op("dve", lambda e: e.tensor_tensor(out=xsd[:, :, :], in0=pT3, in1=bc(hpt[:, 32:48].unsqueeze(2), [128, 16, 64]),
                                                                 op=ALU.mult), reads=[pT, hpt], writes=[xsd])
                          yield
                          def mmg(e, tk=tk):
                              ins = None
                              for g in range(4):
                                  ins = e.matmul(pG[:, g * 128:(g + 1) * 128], lhsT=xt_[:, 8 + g, tk], rhs=xt_[:, 12 + g, tk], start=True, stop=True)
                              return ins
                          kb.op("pe", mmg, reads=[xt_], writes=[pG])
                          yield
                          kb.op("act", lambda e: e.copy(out=Gsb[:, :], in_=pG[:, :]), reads=[pG], writes=[Gsb])
                          yield
                          kb.op("act", lambda e, z_=z_: e.activation(out=gz[:, :], in_=z_[:, :], func=AF.Silu), reads=[z_], writes=[gz])
                          yield
                          for half in range(2):
                              for gg in range(2):
                                  g = half * 2 + gg
                                  r_ = rr[g % 2]
                                  E_ = Es[g % 2]
                                  M_ = Mt[g % 2]
                                  kb.op("pool", lambda e, r_=r_, g=g: e.tensor_tensor(
                                      out=r_[:, :, :], in0=bc(tri[:, :].unsqueeze(1), [128, 4, 128]),
                                      in1=bc(adt[:, 4 * g:4 * g + 4].unsqueeze(2), [128, 4, 128]), op=ALU.mult),
                                      reads=[tri, adt], writes=[r_])
                                  yield

                                  def mmr(e, r_=r_):
                                      e.matmul(pR[:, :], lhsT=ones[:, :], rhs=r_[:, :, :].rearrange("p a b -> p (a b)"), start=True, stop=False)
                                      return e.matmul(pR[:, :], lhsT=idf[:, :], rhs=mb4[:, :], start=False, stop=True)
                                  kb.op("pe", mmr, reads=[ones, idf, mb4, r_], writes=[pR])
                                  yield
                                  for r in range(4):
                                      kb.op("act", lambda e, E_=E_, r=r, g=g: e.activation(
                                          out=E_[:, r, :], in_=pR[:, r * 128:(r + 1) * 128], func=AF.Exp, bias=nacs[:, 4 * g + r:4 * g + r + 1]),
                                          reads=[pR], writes=[E_], sreads=[nacs])
                                      yield
                                  kb.op("dve", lambda e, E_=E_, M_=M_, g=g: e.tensor_tensor(
                                      out=M_[:, :, :], in0=E_[:, :, :], in1=bc(Gsb[:, g * 128:(g + 1) * 128].unsqueeze(1), [128, 4, 128]), op=ALU.mult),
                                      reads=[E_, Gsb], writes=[M_])
                                  yield

                                  def mmy(e, M_=M_, g=g, gg=gg):
                                      ins = None
                                      for r in range(4):
                                          h = 4 * g + r
                                          ins = e.matmul(pY[:, (gg * 4 + r) * 64:(gg * 4 + r + 1) * 64], lhsT=M_[:, r, :], rhs=xc[:, h, :],
                                                         start=True, stop=True)
                                      return ins
                                  kb.op("pe", mmy, reads=[M_, xc], writes=[pY])
                                  yield

                              def mmo(e, half=half, tk=tk):
                                  ins = None
                                  for gg in range(2):
                                      g = half * 2 + gg
                                      ins = e.matmul(pYo[:, gg * 256:(gg + 1) * 256], lhsT=xt_[:, 12 + g, tk], rhs=prevb[:, g * 256:(g + 1) * 256],
                                                     start=True, stop=True)
                                  return ins
                              kb.op("pe", mmo, reads=[xt_, prevb], writes=[pYo])
                              yield

                              def mms(e, half=half):
                                  ins = None
                                  for gg in range(2):
                                      g = half * 2 + gg
                                      ins = e.matmul(pS[:, gg * 256:(gg + 1) * 256], lhsT=btok[:, g, :],
                                                     rhs=xcd[:, 4 * g:4 * g + 4, :].rearrange("p h d -> p (h d)"), start=True, stop=True)
                                  return ins
                              kb.op("pe", mms, reads=[btok, xcd], writes=[pS])
                              yield
                              hs_ = slice(half * 512, (half + 1) * 512)
                              h8 = slice(half * 8, (half + 1) * 8)
                              kb.op("dve", lambda e, h8=h8: e.tensor_tensor(
                                  out=tmp[:, :].rearrange("p (h d) -> p h d", d=64), in0=pYo[:, :].rearrange("p (h d) -> p h d", d=64),
                                  in1=bc(ea[:, h8].unsqueeze(2), [128, 8, 64]), op=ALU.mult), reads=[pYo, ea], writes=[tmp])
                              yield
                              kb.op("dve", lambda e: e.tensor_tensor(out=tmp[:, :], in0=pY[:, :], in1=tmp[:, :], op=ALU.add),
                                    reads=[pY, tmp], writes=[tmp])
                              yield
                              kb.op("pool", lambda e, hs_=hs_, h8=h8: e.tensor_tensor(
                                  out=ysb[:, hs_], in0=tmp[:, :], in1=xsd[:, h8, :].rearrange("p h d -> p (h d)"), op=ALU.add),
                                  reads=[tmp, xsd], writes=[ysb])
                              yield
                              kb.op("dve", lambda e, hs_=hs_, h8=h8: e.tensor_tensor(
                                  out=prev[:, hs_].rearrange("p (h d) -> p h d", d=64), in0=prev[:, hs_].rearrange("p (h d) -> p h d", d=64),
                                  in1=bc(cdec[:, h8].unsqueeze(2), [128, 8, 64]), op=ALU.mult), reads=[prev, cdec], writes=[prev])
                              yield
                              kb.op("dve", lambda e, hs_=hs_: e.tensor_tensor(out=prev[:, hs_], in0=prev[:, hs_], in1=pS[:, :], op=ALU.add),
                                    reads=[prev, pS], writes=[prev])
                              yield
                              kb.op("pool", lambda e, hs_=hs_: e.tensor_copy(out=prevb[:, hs_], in_=prev[:, hs_]), reads=[prev], writes=[prevb])
                              yield
                          kb.op("dve", lambda e: e.tensor_tensor(out=ysb[:, :], in0=ysb[:, :], in1=gz[:, :], op=ALU.mult),
                                reads=[ysb, gz], writes=[ysb])
                          yield
                          rstd_of(ysb, ysb[:, :], ssq, rs, junk)
                          yield
                          kb.op("dve", lambda e: e.tensor_scalar(out=yb[:, :], in0=ysb[:, :], scalar1=rs[:, 0:1], scalar2=None, op0=ALU.mult),
                                reads=[ysb], writes=[yb], sreads=[rs])
                          yield

                          def try_(e):
                              ins = None
                              for c in range(8):
                                  ins = e.transpose(pT[:, c * 128:(c + 1) * 128], yb[:, c * 128:(c + 1) * 128], idb[:, :])
                              return ins
                          kb.op("pe", try_, reads=[yb, idb], writes=[pT])
                          yield
                          kb.op("act", lambda e, tt=tt, ys_=ys_: e.copy(out=ys_[:, :, tt * 128:(tt + 1) * 128],
                                                                      in_=pT[:, :].rearrange("p (k t) -> p k t", k=8)), reads=[pT], writes=[ys_])
                          yield
                      kb.dma("pool", yT.rearrange("(k p) t -> p k t", p=128)[:, :, s * 512:(s + 1) * 512], ys_[:, :, :], ys_,
                             reads=[ys_], writes=[B_yT])
                      yield

              if NB == 2:
                  gens = [ssd_stream(si, list(range(si * (L // 512), (si + 1) * (L // 512)))) for si in range(2)]
              else:
                  gens = [ssd_stream(0, list(range(NS)))]
              if len(gens) == 2:
                  for _ in range(36):
                      next(gens[0], None)
              while gens:
                  for g_ in list(gens):
                      try:
                          next(g_)
                      except StopIteration:
                          gens.remove(g_)
          chk("ssd")
          outer_e = ExitStack()
          W_e = outer_sb(outer_e, "W_e", [128, 16, D], BF16)
          go_e = outer_sb(outer_e, "go_e", [128, 16], F32)
          kb.dma("sp", go_e[:, :], g_out[layer], go_e, reads=[B_in], writes=[go_e])
          wvout = w_out[layer].rearrange("(k p) n -> k p n", p=128)
          with Phase(kb, "fox") as ph:
              pstg = [ph.sb("pstg%d" % i, [128, 512], F32) for i in range(2)]
              pend_e = w_chunks(W_e, 16, D, lambda kc: wvout[kc], go_e, pstg)
              NQ = 3
              qt = [ph.sb("qt%d" % i, [70, L], BF16) for i in range(NQ)]
              kt = [ph.sb("kt%d" % i, [70, L], BF16) for i in range(NQ)]
              vt = [ph.sb("vt%d" % i, [128, TPS, 65], BF16) for i in range(NQ)]
              NR = 6
              LA = 3
              pS_ = [ph.ps("pS%d" % i, [128, 512], F32) for i in range(NR)]
              pO = [ph.ps("pO%d" % i, [65, 512], F32) for i in range(2)]
              Pt = [ph.sb("Pt%d" % i, [128, 512], BF16) for i in range(NR)]
              osb = [ph.sb("osb%d" % i, [65, 512], F32) for i in range(3)]
              heads = [(b_, h) for b_ in range(NB) for h in range(NH)]

              def load_head(n):
                  b_, h = heads[n]
                  q_, k_, v_ = qt[n % NQ], kt[n % NQ], vt[n % NQ]
                  kb.dma("sp", q_[:, :], qa[b_, h], q_, reads=[B_qa], writes=[q_])
                  kb.dma("sp", k_[:, :], ka[b_, h], k_, reads=[B_ka], writes=[k_])
                  kb.dma("sp", v_[:, :, :], va.rearrange("(b i p) h c -> b h p i c", b=NB, p=128)[b_, h], v_, reads=[B_va], writes=[v_])

              steps = []
              jn = 0
              for n, (b_, h) in enumerate(heads):
                  for j in range(L // 512):
                      nk = 4 * j + 4
                      for i in range(nk):
                          steps.append(dict(n=n, b=b_, h=h, j=j, i=i, nk=nk, jn=jn, first=(j == 0 and i == 0)))
                      jn += 1

              def front(t):
                  st = steps[t]
                  n, j, i = st["n"], st["j"], st["i"]
                  if st["first"]:
                      if n == 0:
                          load_head(0)
                      if n + 1 < len(heads):
                          load_head(n + 1)
                  q_, k_ = qt[n % NQ], kt[n % NQ]
                  dd = i - 4 * j
                  c0 = 128 * dd if dd > 0 else 0
                  S_, P_ = pS_[t % NR], Pt[t % NR]
                  kb.op("pe", lambda e: e.matmul(S_[:, c0:512], lhsT=k_[:, i * 128:(i + 1) * 128], rhs=q_[:, j * 512 + c0:(j + 1) * 512],
                                                 start=True, stop=True), reads=[k_, q_], writes=[S_])
                  kb.op("act", lambda e: e.activation(out=P_[:, c0:512], in_=S_[:, c0:512], func=AF.Exp, scale=0.125), reads=[S_], writes=[P_])
                  if dd >= 0:
                      me = ("dve", "pool")[dd % 2]
                      kb.op(me, lambda e: e.tensor_tensor(out=P_[:, c0:c0 + 128], in0=P_[:, c0:c0 + 128], in1=tri01[:, :], op=ALU.mult),
                            reads=[P_, tri01], writes=[P_])

              def back(t):
                  st = steps[t]
                  n, j, i, nk, b_, h = st["n"], st["j"], st["i"], st["nk"], st["b"], st["h"]
                  v_ = vt[n % NQ]
                  dd = i - 4 * j
                  c0 = 128 * dd if dd > 0 else 0
                  P_ = Pt[t % NR]
                  O = pO[st["jn"] % 2]
                  kb.op("pe", lambda e: e.matmul(O[:, c0:512], lhsT=v_[:, i, :], rhs=P_[:, c0:512], start=(i == 0), stop=(i == nk - 1),
                                                 skip_group_check=True), reads=[v_, P_], writes=[O])
                  if i == nk - 1:
                      o_ = osb[st["jn"] % 3]
                      kb.op("dve", lambda e: e.tensor_copy(out=o_[:, :], in_=O[:, :]), reads=[O], writes=[o_])
                      tcol = b_ * L + j * 512
                      kb.dma("pool", oT[h * 64:(h + 1) * 64, tcol:tcol + 512], o_[0:64, :], o_, reads=[o_], writes=[B_oT])
                      kb.dma("pool", rsum[h:h + 1, tcol:tcol + 512], o_[64:65, :], o_, reads=[o_], writes=[B_rs])
                      if pend_e:
                          pend_e.pop(0)()

              for t in range(len(steps) + LA):
                  if t < len(steps):
                      front(t)
                  if t - LA >= 0:
                      back(t - LA)
              while pend_e:
                  pend_e.pop(0)()
          chk("fox")
          moe = (layer % 2 == 1)
          with Phase(kb, "e") as ph:
              W, go = W_e, go_e
              if moe:
                  rwt = ph.sb("rwt", [128, 8, 8], F32)
                  gf = ph.sb("gf", [128, 8], F32)
                  kb.dma("sp", rwt[:, :, :], rw, rwt, reads=[B_in], writes=[rwt])
                  kb.dma("sp", gf[:, :], g_ffn[layer], gf, reads=[B_in], writes=[gf])
                  kb.op("dve", lambda e: e.tensor_tensor(out=rwt[:, :, :], in0=rwt[:, :, :], in1=bc(gf[:, :].unsqueeze(2), [128, 8, 8]),
                                                         op=ALU.mult), reads=[rwt, gf], writes=[rwt])

              def e_stream(si, slist):
                  sfx = "_%d" % si
                  bA = ph.ps("bA" + sfx, [128, 512], F32)
                  bB = ph.ps("bB" + sfx, [128, 512], F32)
                  po = [ph.ps("po%d" % i + sfx, [128, 512], F32) for i in range(2)]
                  osq = ph.sb("osq" + sfx, [128, 8, 512], BF16)
                  nt = NormT(ph, pT=Alias(bA, bA.t[:, :].bitcast(BF16)), hs=[osq])
                  ot = [ph.sb("ot" + sfx, [128, 8, 512], F32)]
                  rt = [ph.sb("rt" + sfx, [128, 8, 512], F32)]
                  ysT = [ph.sb("ysT" + sfx, [128, 8, 512], BF16)]
                  yfT = ph.sb("yfT" + sfx, [128, 8, 512], BF16)
                  rstd = ph.sb("rstd" + sfx, [128, 512], F32)
                  pss = bB
                  xts = [ph.sb("xt%d" % i + sfx, [128, D], F32) for i in range(2)]
                  if moe:
                      pTf = bB
                      pl = bA
                      xTf = ph.sb("xTf" + sfx, [128, D], F32)
                      lg = ph.sb("lg" + sfx, [128, 8], F32)
                      lg2 = ph.sb("lg2" + sfx, [128, 8], F32)
                      m1 = ph.sb("m1" + sfx, [128, 4], F32)
                      mk1 = ph.sb("mk1" + sfx, [128, 8], F32)
                      mk2 = ph.sb("mk2" + sfx, [128, 8], F32)
                      cm = [ph.sb("cm%d" % i + sfx, [128, 8], F32) for i in range(2)]
                  pi = 0
                  xi = 0
                  for s in slist:
                      o_, r_, ys_ = ot[0], rt[0], ysT[0]
                      cs = slice(s * 512, (s + 1) * 512)
                      kb.dma("sp", o_[:, :, :], oT.rearrange("(k p) t -> p k t", p=128)[:, :, cs], o_, reads=[B_oT], writes=[o_])
                      yield
                      for par in range(2):
                          src = rsum.rearrange("(k two) t -> two k t", two=2)[par:par + 1, :, cs]
                          kb.dma("act", r_[par * 64:(par + 1) * 64, :, :], bc(src, [64, 8, 512]), r_, reads=[B_rs], writes=[r_])
                          yield
                      kb.dma("sp", ys_[:, :, :], yT.rearrange("(k p) t -> p k t", p=128)[:, :, cs], ys_, reads=[B_yT], writes=[ys_])
                      yield
                      kb.op("dve", lambda e, r_=r_: e.reciprocal(out=r_[:, :, :], in_=r_[:, :, :]), reads=[r_], writes=[r_])
                      yield
                      kb.op("dve", lambda e, r_=r_, o_=o_: e.tensor_tensor(out=o_[:, :, :], in0=o_[:, :, :], in1=r_[:, :, :], op=ALU.mult),
                            reads=[o_, r_], writes=[o_])
                      yield
                      kb.op("act", lambda e, o_=o_: e.activation(out=osq[:, :, :], in_=o_[:, :, :], func=AF.Square), reads=[o_], writes=[osq])
                      yield

                      def mss(e):
                          ins = None
                          for kc in range(8):
                              ins = e.matmul(pss[:, :], lhsT=onesb[:, :], rhs=osq[:, kc, :], start=(kc == 0), stop=(kc == 7))
                          return ins
                      kb.op("pe", mss, reads=[onesb, osq], writes=[pss])
                      yield
                      kb.op("act", lambda e: e.activation(out=rstd[:, :], in_=pss[:, :], func=AF.Sqrt, scale=1.0 / D, bias=EPS), reads=[pss], writes=[rstd])
                      yield
                      kb.op("dve", lambda e: e.reciprocal(out=rstd[:, :], in_=rstd[:, :]), reads=[rstd], writes=[rstd])
                      yield
                      kb.op("dve", lambda e, o_=o_: e.tensor_tensor(out=yfT[:, :, :], in0=o_[:, :, :], in1=bc(rstd[:, :].unsqueeze(1), [128, 8, 512]),
                                                                   op=ALU.mult), reads=[o_, rstd], writes=[yfT])
                      yield
                      for tt in range(4):
                          tile = s * 4 + tt
                          xt = xts[xi % 2]
                          xi += 1
                          kb.dma("sp", xt[:, :], x_src[tile * 128:(tile + 1) * 128, :], xt, reads=[B_xsrc], writes=[xt])
                          yield
                          for half in range(2):
                              p = po[pi % 2]
                              pi += 1

                              def mm(e, p=p, tt=tt, half=half, ys_=ys_):
                                  ins = None
                                  for kc in range(16):
                                      src = ys_ if kc < 8 else yfT
                                      ins = e.matmul(p[:, :], lhsT=src[:, kc % 8, tt * 128:(tt + 1) * 128], rhs=W[:, kc, half * 512:(half + 1) * 512],
                                                     start=(kc == 0), stop=(kc == 15))
                                  return ins
                              kb.op("pe", mm, reads=[ys_, yfT, W], writes=[p])
                              yield
                              kb.op("dve", lambda e, p=p, half=half, xt=xt: e.tensor_tensor(
                                  out=xt[:, half * 512:(half + 1) * 512], in0=p[:, :], in1=xt[:, half * 512:(half + 1) * 512], op=ALU.add),
                                  reads=[p, xt], writes=[xt])
                              yield
                          kb.dma("pool", xa[tile * 128:(tile + 1) * 128, :], xt[:, :], xt, reads=[xt], writes=[B_xa])
                          yield
                          yield from nt.emit_gen(xt, tile)
                          rs = nt.last_rs
                          if moe:
                              for hh in range(2):
                                  def trf(e, hh=hh, xt=xt):
                                      ins = None
                                      for c in range(4):
                                          kc = hh * 4 + c
                                          ins = e.transpose(pTf[:, c * 128:(c + 1) * 128], xt[:, kc * 128:(kc + 1) * 128], idf[:, :])
                                      return ins
                                  kb.op("pe", trf, reads=[xt, idf], writes=[pTf])
                                  yield
                                  kb.op("act", lambda e, hh=hh: e.copy(out=xTf[:, hh * 512:(hh + 1) * 512], in_=pTf[:, :]), reads=[pTf], writes=[xTf])
                                  yield

                              def mml(e):
                                  ins = None
                                  for kc in range(8):
                                      ins = e.matmul(pl[:, 0:8], lhsT=xTf[:, kc * 128:(kc + 1) * 128], rhs=rwt[:, kc, :], start=(kc == 0), stop=(kc == 7))
                                  return ins
                              kb.op("pe", mml, reads=[xTf, rwt], writes=[pl])
                              yield
                              c_ = cm[tile % 2]
                              kb.op("dve", lambda e, rs=rs: e.tensor_scalar(out=lg[:, :], in0=pl[:, 0:8], scalar1=rs[:, 0:1], scalar2=None, op0=ALU.mult),
                                    reads=[pl], writes=[lg], sreads=[rs])
                              yield
                              kb.op("dve", lambda e: e.reduce_max(out=m1[:, 0:1], in_=lg[:, :], axis=AX.X), reads=[lg], writes=[m1])
                              yield
                              kb.op("dve", lambda e: e.tensor_scalar(out=mk1[:, :], in0=lg[:, :], scalar1=m1[:, 0:1], scalar2=None, op0=ALU.is_ge),
                                    reads=[lg], writes=[mk1], sreads=[m1])
                              yield
                              kb.op("dve", lambda e: e.scalar_tensor_tensor(out=lg2[:, :], in0=mk1[:, :], scalar=NEG, in1=lg[:, :], op0=ALU.mult, op1=ALU.add),
                                    reads=[mk1, lg], writes=[lg2])
                              yield
                              kb.op("dve", lambda e: e.reduce_max(out=m1[:, 1:2], in_=lg2[:, :], axis=AX.X), reads=[lg2], writes=[m1])
                              yield
                              kb.op("dve", lambda e: e.tensor_scalar(out=mk2[:, :], in0=lg2[:, :], scalar1=m1[:, 1:2], scalar2=None, op0=ALU.is_ge),
                                    reads=[lg2], writes=[mk2], sreads=[m1])
                              yield
                              kb.op("dve", lambda e: e.tensor_sub(out=m1[:, 2:3], in0=m1[:, 1:2], in1=m1[:, 0:1]), reads=[m1], writes=[m1])
                              yield
                              kb.op("act", lambda e: e.activation(out=m1[:, 2:3], in_=m1[:, 2:3], func=AF.Sigmoid), reads=[m1], writes=[m1])
                              yield
                              kb.op("dve", lambda e: e.tensor_scalar(out=m1[:, 3:4], in0=m1[:, 2:3], scalar1=-1.0, scalar2=1.0, op0=ALU.mult, op1=ALU.add),
                                    reads=[m1], writes=[m1])
                              yield
                              kb.op("dve", lambda e, c_=c_: e.tensor_scalar(out=c_[:, :], in0=mk1[:, :], scalar1=m1[:, 3:4], scalar2=None, op0=ALU.mult),
                                    reads=[mk1], writes=[c_], sreads=[m1])
                              yield
                              kb.op("dve", lambda e, c_=c_: e.scalar_tensor_tensor(out=c_[:, :], in0=mk2[:, :], scalar=m1[:, 2:3], in1=c_[:, :],
                                                                                  op0=ALU.mult, op1=ALU.add), reads=[mk2, c_], writes=[c_], sreads=[m1])
                              yield
                              kb.dma("pool", cmb[tile * 128:(tile + 1) * 128, :], c_[:, :], c_, reads=[c_], writes=[B_cmb])
                              yield

              if NB == 2:
                  gens = [e_stream(si, list(range(si * (L // 512), (si + 1) * (L // 512)))) for si in range(2)]
              else:
                  gens = [e_stream(0, list(range(NS)))]
              if len(gens) == 2:
                  for _ in range(25 if moe else 17):
                      next(gens[0], None)
              while gens:
                  for g_ in list(gens):
                      try:
                          next(g_)
                      except StopIteration:
                          gens.remove(g_)
          outer_e.close()
          chk("e")
          npass = NE if moe else 1
          last_layer = (layer == depth - 1)
          HF = DFF // 2
          NFH = NFC // 2
          with Phase(kb, "f") as ph:
              WG = [ph.sb("WG%d" % i, [128, 8, HF], BF16) for i in range(2)]
              WU = [ph.sb("WU%d" % i, [128, 8, HF], BF16) for i in range(2)]
              WD = [ph.sb("WD%d" % i, [128, NFH, D], BF16) for i in range(2)]
              gf = ph.sb("gf", [128, 8], F32)
              CHW = 352
              stg = [ph.sb("stg%d" % i, [128, CHW], F32) for i in range(4)]
              kb.dma("sp", gf[:, :], g_ffn[layer], gf, reads=[B_in], writes=[gf])
              hts = [ph.sb("hts%d" % i, [128, 8, 512], BF16) for i in range(2)]
              aT = ph.sb("aT", [128, NFH, 512], BF16)
              sg = [ph.sb("sg%d" % i, [128, 512], F32) for i in range(2)]
              pg = [ph.ps("pg%d" % i, [128, 512], F32) for i in range(2)]
              pu = [ph.ps("pu%d" % i, [128, 512], F32) for i in range(2)]
              po = [ph.ps("po%d" % i, [128, 512], F32) for i in range(3)]
              xts = [ph.sb("xt%d" % i, [128, D], F32) for i in range(2)]
              cmt = [ph.sb("cmt%d" % i, [128, 8], F32) for i in range(2)]
              if last_layer:
                  gfin = ph.sb("gfin", [128, D], F32)
                  kb.dma("sp", gfin[:, :], g_fin, gfin, reads=[B_in], writes=[gfin])
                  ssq = ph.sb("ssq", [128, 1], F32)
                  rsf = ph.sb("rsf", [128, 1], F32)
                  junk = ph.sb("junk", [128, D], BF16)
              else:
                  nt = NormT(ph, nhs=1)
              units = [(e_, hf) for e_ in range(npass) for hf in range(2)]
              lc = [0]

              def wchunks(u, engs=("pool",)):
                  e_, hf = units[u]
                  if moe:
                      wgv = moe_wg[0, e_].rearrange("(k p) n -> k p n", p=128)
                      wuv = moe_wu[0, e_].rearrange("(k p) n -> k p n", p=128)
                      wdv = moe_wd[0, e_].rearrange("(k p) n -> k p n", p=128)
                  else:
                      wgv = ffn_wg[0].rearrange("(k p) n -> k p n", p=128)
                      wuv = ffn_wu[0].rearrange("(k p) n -> k p n", p=128)
                      wdv = ffn_wd[0].rearrange("(k p) n -> k p n", p=128)
                  out = []

                  def mk(dst, kc, c0, cn, src, gain):
                      def f():
                          s_ = stg[lc[0] % 4]
                          ce_ = engs[lc[0] % len(engs)]
                          lc[0] += 1
                          kb.dma("sp", s_[:, 0:cn], src, s_, reads=[B_in], writes=[s_])
                          if gain:
                              kb.op(ce_, lambda en: en.tensor_scalar(out=dst[:, kc, c0:c0 + cn], in0=s_[:, 0:cn], scalar1=gf[:, kc:kc + 1],
                                                                        scalar2=None, op0=ALU.mult), reads=[s_], writes=[dst], sreads=[gf])
                          else:
                              kb.op(ce_, lambda en: en.tensor_copy(out=dst[:, kc, c0:c0 + cn], in_=s_[:, 0:cn]), reads=[s_], writes=[dst])
                      return f
                  for dst, wv_ in ((WG[u % 2], wgv), (WU[u % 2], wuv)):
                      for kc in range(8):
                          for c0 in range(0, HF, CHW):
                              out.append(mk(dst, kc, c0, CHW, wv_[kc][:, hf * HF + c0:hf * HF + c0 + CHW], True))
                  for fc in range(NFH):
                      for c0 in range(0, D, CHW):
                          cn = min(CHW, D - c0)
                          out.append(mk(WD[u % 2], fc, c0, cn, wdv[hf * NFH + fc][:, c0:c0 + cn], False))
                  return out

              for f_ in wchunks(0, engs=("dve", "pool", "dve")):
                  f_()
              tile_tok = {}
              pi = 0
              for u, (e_, hf) in enumerate(units):
                  Wg, Wu, Wd = WG[u % 2], WU[u % 2], WD[u % 2]
                  pend = wchunks(u + 1) if u + 1 < len(units) else []
                  final_unit = (u == len(units) - 1)
                  acc_src = xa if u == 0 else xb
                  gi = [0]
                  gu_of = {}

                  def load_ht(s):
                      ht = hts[s % 2]
                      kb.dma("sp", ht[:, :, :], hT.rearrange("(k p) t -> p k t", p=128)[:, :, s * 512:(s + 1) * 512], ht,
                             reads=[B_hT], writes=[ht])

                  def pe_part(s, fc):
                      ht = hts[s % 2]
                      k_ = gi[0] % 2
                      gi[0] += 1
                      gu_of[(s, fc)] = k_
                      g_, u_ = pg[k_], pu[k_]

                      def mmg(e):
                          ins = None
                          for kc in range(8):
                              ins = e.matmul(g_[:, :], lhsT=Wg[:, kc, fc * 128:(fc + 1) * 128], rhs=ht[:, kc, :], start=(kc == 0), stop=(kc == 7))
                          return ins

                      def mmu(e):
                          ins = None
                          for kc in range(8):
                              ins = e.matmul(u_[:, :], lhsT=Wu[:, kc, fc * 128:(fc + 1) * 128], rhs=ht[:, kc, :], start=(kc == 0), stop=(kc == 7))
                          return ins
                      kb.op("pe", mmg, reads=[Wg, ht], writes=[g_])
                      kb.op("pe", mmu, reads=[Wu, ht], writes=[u_])

                  def ew_part(s, fc):
                      k_ = gu_of.pop((s, fc))
                      g_, u_, s_ = pg[k_], pu[k_], sg[k_]
                      kb.op("act", lambda e: e.activation(out=s_[:, :], in_=g_[:, :], func=AF.Silu), reads=[g_], writes=[s_])
                      kb.op("dve", lambda e: e.tensor_tensor(out=aT[:, fc, :], in0=u_[:, :], in1=s_[:, :], op=ALU.mult),
                            reads=[u_, s_], writes=[aT])

                  FR = 2
                  load_ht(0)
                  for fc in range(FR):
                      pe_part(0, fc)
                  for s in range(NS):
                      for fc in range(NFH):
                          if fc >= FR:
                              pe_part(s, fc)
                          ew_part(s, fc)
                          if pend:
                              pend.pop(0)()
                      if s + 1 < NS:
                          load_ht(s + 1)
                          for fc in range(FR):
                              pe_part(s + 1, fc)
                      for tt in range(4):
                          tile = s * 4 + tt
                          xt = xts[tile % 2]
                          if tile in tile_tok:
                              kb._wait("sp", {tile_tok[tile][0]: tile_tok[tile][1]})
                          kb.dma("sp", xt[:, :], acc_src[tile * 128:(tile + 1) * 128, :], xt, reads=[B_in], writes=[xt])
                          if moe:
                              c_ = cmt[tile % 2]
                              kb.dma("sp", c_[:, :], cmb[tile * 128:(tile + 1) * 128, :], c_, reads=[B_cmb], writes=[c_])
                          for half in range(2):
                              p = po[pi % 3]
                              pi += 1

                              def mmd(e, p=p, tt=tt, half=half):
                                  ins = None
                                  for fc in range(NFH):
                                      ins = e.matmul(p[:, :], lhsT=aT[:, fc, tt * 128:(tt + 1) * 128], rhs=Wd[:, fc, half * 512:(half + 1) * 512],
                                                     start=(fc == 0), stop=(fc == NFH - 1))
                                  return ins
                              kb.op("pe", mmd, reads=[aT, Wd], writes=[p])
                              hs_ = slice(half * 512, (half + 1) * 512)
                              if moe:
                                  kb.op("dve", lambda e, p=p, xt=xt, hs_=hs_, c_=c_: e.scalar_tensor_tensor(
                                      out=xt[:, hs_], in0=p[:, :], scalar=c_[:, e_:e_ + 1], in1=xt[:, hs_], op0=ALU.mult, op1=ALU.add),
                                      reads=[p, xt], writes=[xt], sreads=[c_])
                              else:
                                  kb.op("dve", lambda e, p=p, xt=xt, hs_=hs_: e.tensor_tensor(out=xt[:, hs_], in0=p[:, :], in1=xt[:, hs_], op=ALU.add),
                                        reads=[p, xt], writes=[xt])
                          if final_unit and last_layer:
                              rstd_of(xt, xt[:, :], ssq, rsf, junk)
                              kb.op("dve", lambda e, xt=xt: e.scalar_tensor_tensor(out=xt[:, :], in0=xt[:, :], scalar=rsf[:, 0:1], in1=gfin[:, :],
                                                                                  op0=ALU.mult, op1=ALU.mult), reads=[xt, gfin], writes=[xt], sreads=[rsf])
                              kb.dma("pool", out_d[tile * 128:(tile + 1) * 128, :], xt[:, :], xt, reads=[xt], writes=[B_out])
                          else:
                              kb.dma("pool", xb[tile * 128:(tile + 1) * 128, :], xt[:, :], xt, reads=[xt], writes=[B_xb])
                              tile_tok[tile] = (xt.sem, xt.sem.cnt)
                              if final_unit:
                                  nt.emit(xt, tile)
                          if pend:
                              pend.pop(0)()
                  while pend:
                      pend.pop(0)()
    except _Stop:
        pass
    kb.barrier()
    stack.close()
    return nc


def _consts():
    i = np.arange(128)
    tri = (i[:, None] <= i[None, :]).astype(np.float32)
    mb = np.where(i[:, None] > i[None, :], NEG, 0.0).astype(np.float32)
    return dict(
        c_id_b=np.eye(128, dtype=np.float32).astype(ml_dtypes.bfloat16),
        c_id_f=np.eye(128, dtype=np.float32),
        c_tri=tri,
        c_ones=np.ones((128, 128), np.float32),
        c_mb4=np.ascontiguousarray(np.tile(mb, (1, 4))),
        c_tri01=tri.astype(ml_dtypes.bfloat16),
    )


def _layout_params(inp):
    f = np.float32
    pm = {}
    pm["g_mix"] = np.ascontiguousarray(inp["mix_norm"].reshape(2, 8, 128).transpose(0, 2, 1)).astype(f)
    pm["g_ffn"] = np.ascontiguousarray(inp["ffn_norm"].reshape(2, 8, 128).transpose(0, 2, 1)).astype(f)
    go = np.concatenate([inp["ssd_norm"], inp["fox_norm"]], axis=1)
    pm["g_out"] = np.ascontiguousarray(go.reshape(2, 16, 128).transpose(0, 2, 1)).astype(f)
    pm["cw"] = np.ascontiguousarray(inp["conv_w"].reshape(2, 4, 16, 128).transpose(0, 3, 2, 1)).astype(f)
    pm["cb"] = np.ascontiguousarray(inp["conv_b"].reshape(2, 16, 128).transpose(0, 2, 1)).astype(f)
    hp = np.concatenate([inp["dt_bias"], inp["a_log"], inp["d_skip"], inp["fox_f_bias"]], axis=1)
    pm["hp"] = np.ascontiguousarray(np.broadcast_to(hp[:, None, :], (2, 128, 64))).astype(f)
    pm["nfb"] = np.ascontiguousarray(inp["fox_f_bias"].reshape(2, 16, 1)).astype(f)
    pm["rw"] = np.ascontiguousarray(inp["router_w"][0].reshape(8, 128, 8).transpose(1, 0, 2)).astype(f)
    pm["g_fin"] = np.ascontiguousarray(np.broadcast_to(inp["final_norm"][None, :], (128, D))).astype(f)
    return pm


_CACHE = {}


def run(inp, L, NB, ncores=8, depth=2, dbg=(), stop=None):
    key = (L, NB, depth, tuple(dbg), stop)
    if key not in _CACHE:
        _CACHE[key] = build(L, NB, depth, dbg, stop)
    nc = _CACHE[key]
    x = np.ascontiguousarray(np.asarray(inp["x"], dtype=np.float32))
    T = NB * L
    xs = x.reshape(-1, T, D)
    shared = dict(_consts())
    shared.update(_layout_params({k: np.asarray(v) for k, v in inp.items()}))
    for k in ("w_in", "w_out", "ffn_w_gate", "ffn_w_up", "ffn_w_down", "moe_w_gate", "moe_w_up", "moe_w_down"):
        shared[k] = np.ascontiguousarray(np.asarray(inp[k], dtype=np.float32))
    in_maps = []
    for c in range(ncores):
        m = dict(shared)
        m["x"] = np.ascontiguousarray(xs[c])
        in_maps.append(m)
    res = run_bass_kernel_spmd(nc, in_maps, core_ids=list(range(ncores)))
    return res


def kernel(**inputs):
    x = np.asarray(inputs["x"])
    Bsz, L, _ = x.shape
    NB = Bsz // 8
    res = run(inputs, L, NB)
    out = np.stack([r["out"] for r in res.results], axis=0).reshape(Bsz, L, D)
    return out.astype(np.float32)
```
